# Optimizing a Trainium2 kernel written in Bass

```python
import jax, jax.numpy as jnp
from jax import lax
import numpy as np

D_MODEL = 1024
BATCH = 16
SEQ = 2048
DEPTH = 4

GRID_W = 64
CTX_LEN = 256
N_MIXERS = 2
N_A_LAYERS = (DEPTH + 1) // 2
N_B_LAYERS = DEPTH // 2
CHUNK = 64
EPS = 1e-6
N_MOD = 6
A_HEADS = 8
A_DK = D_MODEL // 16
A_DV = D_MODEL // 8
A_PROJ = 2 * A_HEADS * A_DK + 2 * A_HEADS * A_DV + 4 * A_HEADS
CONV_W = 5
B_HEADS = 8
B_DK = D_MODEL // B_HEADS
B_DV = D_MODEL // B_HEADS
B_PROJ = 2 * B_HEADS * B_DK + 2 * B_HEADS * B_DV
ROPE_BASE = 10000.0
N_EXPERTS = 16
EXPERT_FF = D_MODEL
CAPACITY_FACTOR = 2

kernel_name = "hybrid_mlstm_retention_ec_moe_diffusion"


def rmsnorm(x, g):
    xf = x.astype(jnp.float32)
    y = xf * lax.rsqrt(jnp.mean(xf * xf, axis=-1, keepdims=True) + EPS)
    return (y * g.astype(jnp.float32)).astype(x.dtype)


def modulate(h, shift, scale):
    return h * (1 + scale) + shift


def heads(a, n_heads):
    b, t, _ = a.shape
    return a.reshape(b, t, n_heads, -1).transpose(0, 2, 1, 3)


def dwconv_centred(x, w, b):
    pad = CONV_W // 2
    y = lax.conv_general_dilated(x, w[:, None, :].astype(x.dtype), window_strides=(1,),
                                 padding=[(pad, pad)], dimension_numbers=('NWC', 'WIO', 'NWC'),
                                 feature_group_count=x.shape[-1])
    return y + b.astype(x.dtype)


def to_chunks(a):
    b, h, t = a.shape[:3]
    return jnp.moveaxis(a.reshape(b, h, t // CHUNK, CHUNK, *a.shape[3:]), 2, 0)


def from_chunks(a):
    nc, b, h, l = a.shape[:4]
    return jnp.moveaxis(a, 0, 2).reshape(b, h, nc * l, *a.shape[4:])


def mlstm_scan(q, k, v, ig, lf, state):
    tril = jnp.tril(jnp.ones((CHUNK, CHUNK), bool))

    def step(carry, inp):
        C, n, m = carry
        qc, kc, vc, ic, fc = inp
        b = jnp.cumsum(fc, axis=-1)
        dmat = jnp.where(tril, b[..., :, None] - b[..., None, :] + ic[..., None, :], -jnp.inf)
        inter = b + m[..., None]
        m_row = jnp.maximum(inter, dmat.max(-1))
        s = jnp.einsum('bhid,bhjd->bhij', qc, kc) * jnp.exp(dmat - m_row[..., None])
        w_inter = jnp.exp(inter - m_row)
        num = jnp.einsum('bhij,bhjv->bhiv', s, vc) + w_inter[..., None] * jnp.einsum('bhid,bhdv->bhiv', qc, C)
        den = s.sum(-1) + w_inter * jnp.einsum('bhid,bhd->bhi', qc, n)
        h = num / jnp.maximum(jnp.abs(den), jnp.exp(-m_row))[..., None]
        b_last = b[..., -1]
        gk = b_last[..., None] - b + ic
        m_new = jnp.maximum(b_last + m, gk.max(-1))
        wk = jnp.exp(gk - m_new[..., None])
        wc = jnp.exp(b_last + m - m_new)
        C_new = wc[..., None, None] * C + jnp.einsum('bhj,bhjd,bhjv->bhdv', wk, kc, vc)
        n_new = wc[..., None] * n + jnp.einsum('bhj,bhjd->bhd', wk, kc)
        return (C_new, n_new, m_new), h

    xs = tuple(to_chunks(a) for a in (q, k, v, ig, lf))
    state, h = lax.scan(step, state, xs)
    return from_chunks(h), state


def retention_scan(q, k, v, logg, S):
    idx = jnp.arange(CHUNK, dtype=jnp.float32)
    tril = jnp.tril(jnp.ones((CHUNK, CHUNK), bool))
    diff = jnp.where(tril, idx[:, None] - idx[None, :], 0.0)
    dmat = jnp.where(tril, jnp.exp(diff * logg[:, None, None]), 0.0)
    inter = jnp.exp((idx + 1.0) * logg[:, None])
    kdec = jnp.exp((CHUNK - 1.0 - idx) * logg[:, None])
    sdec = jnp.exp(CHUNK * logg)

    def step(S, inp):
        qc, kc, vc = inp
        o = jnp.einsum('bhij,bhjv->bhiv', jnp.einsum('bhid,bhjd->bhij', qc, kc) * dmat, vc) \
            + inter[..., None] * jnp.einsum('bhid,bhdv->bhiv', qc, S)
        S_new = sdec[:, None, None] * S + jnp.einsum('hj,bhjd,bhjv->bhdv', kdec, kc, vc)
        return S_new, o

    xs = tuple(to_chunks(a) for a in (q, k, v))
    S, o = lax.scan(step, S, xs)
    return from_chunks(o), S


def run_bidirectional(scan_f, scan_b, ctx_f, lat_f, ctx_b, lat_b, state0):
    flip = lambda t: tuple(jnp.flip(a, 2) for a in t)
    oc_f, s = scan_f(ctx_f, state0)
    ol_f, _ = scan_f(lat_f, s)
    oc_b, s = scan_b(flip(ctx_b), state0)
    ol_b, _ = scan_b(flip(lat_b), s)
    return oc_f + jnp.flip(oc_b, 2), ol_f + jnp.flip(ol_b, 2)


def mlstm_mixer(h_ctx, h_lat, w_in, conv_w, conv_b, ig_b, fg_b, norm_g, w_out, need_ctx_out):
    nq = A_HEADS * A_DK
    nv = A_HEADS * A_DV
    f32 = jnp.float32

    def project(h):
        bn, t, _ = h.shape
        p = h @ w_in
        qk = jax.nn.silu(dwconv_centred(p[..., :2 * nq], conv_w, conv_b)).astype(f32)
        q = heads(qk[..., :nq], A_HEADS) * (A_DK ** -0.5)
        k = heads(qk[..., nq:], A_HEADS)
        v = heads(p[..., 2 * nq:2 * nq + nv].astype(f32), A_HEADS)
        o = p[..., 2 * nq + nv:2 * nq + 2 * nv]
        g = p[..., 2 * nq + 2 * nv:].astype(f32)
        ig = g[..., :2 * A_HEADS].reshape(bn, t, 2, A_HEADS) + ig_b.astype(f32)
        lf = jax.nn.log_sigmoid(g[..., 2 * A_HEADS:].reshape(bn, t, 2, A_HEADS) + fg_b.astype(f32))
        ig = jnp.transpose(ig, (0, 2, 3, 1))
        lf = jnp.transpose(lf, (0, 2, 3, 1))
        fwd = (q, k, v, ig[:, 0], lf[:, 0])
        bwd = (q, k, v, ig[:, 1], lf[:, 1])
        return fwd, bwd, o

    cf, cb, o_ctx = project(h_ctx)
    lf_, lb, o_lat = project(h_lat)
    bn = h_lat.shape[0]
    state0 = (jnp.zeros((bn, A_HEADS, A_DK, A_DV), f32), jnp.zeros((bn, A_HEADS, A_DK), f32),
              jnp.zeros((bn, A_HEADS), f32))
    scan = lambda args, s: mlstm_scan(*args, s)
    y_ctx, y_lat = run_bidirectional(scan, scan, cf, lf_, cb, lb, state0)

    def finish(y, o):
        b_, h_, t_, d_ = y.shape
        y = jnp.transpose(y, (0, 2, 1, 3))
        y = y * lax.rsqrt(jnp.mean(y * y, axis=-1, keepdims=True) + EPS)
        y = y.reshape(b_, t_, h_ * d_) * norm_g.astype(f32) * jax.nn.sigmoid(o.astype(f32))
        return y.astype(o.dtype) @ w_out

    out_ctx = finish(y_ctx, o_ctx) if need_ctx_out else None
    return out_ctx, finish(y_lat, o_lat)


def rope_angles(t):
    rows = t // GRID_W
    r = jnp.broadcast_to(jnp.arange(rows, dtype=jnp.float32)[:, None], (rows, GRID_W)).reshape(-1)
    col = jnp.broadcast_to(jnp.arange(GRID_W, dtype=jnp.float32)[None, :], (rows, GRID_W)).reshape(-1)
    nf = B_DK // 4
    inv = ROPE_BASE ** (-jnp.arange(nf, dtype=jnp.float32) / nf)
    return r[:, None] * inv, col[:, None] * inv


def rope(x, ang):
    x1, x2 = jnp.split(x, 2, axis=-1)
    cos, sin = jnp.cos(ang), jnp.sin(ang)
    return jnp.concatenate([x1 * cos - x2 * sin, x1 * sin + x2 * cos], axis=-1)


def rope2d(x, ang_r, ang_c):
    half = B_DK // 2
    return jnp.concatenate([rope(x[..., :half], ang_r), rope(x[..., half:], ang_c)], axis=-1)


def retention_mixer(h_ctx, h_lat, w_in, decay_logit, gn_g, w_out, need_ctx_out):
    n = B_HEADS * B_DK
    f32 = jnp.float32

    def project(h, angles):
        p = h @ w_in
        q = heads(p[..., :n].astype(f32), B_HEADS)
        k = heads(p[..., n:2 * n].astype(f32), B_HEADS) * (B_DK ** -0.5)
        v = heads(p[..., 2 * n:3 * n].astype(f32), B_HEADS)
        g = p[..., 3 * n:]
        if angles is not None:
            q = rope2d(q, *angles)
            k = rope2d(k, *angles)
        return (q, k, v), g

    c_args, g_ctx = project(h_ctx, None)
    l_args, g_lat = project(h_lat, rope_angles(h_lat.shape[1]))
    logg = jax.nn.log_sigmoid(decay_logit.astype(f32))
    bn = h_lat.shape[0]
    state0 = jnp.zeros((bn, B_HEADS, B_DK, B_DV), f32)
    scan_f = lambda args, s: retention_scan(*args, logg[0], s)
    scan_b = lambda args, s: retention_scan(*args, logg[1], s)
    y_ctx, y_lat = run_bidirectional(scan_f, scan_b, c_args, l_args, c_args, l_args, state0)

    def finish(y, g):
        b_, h_, t_, d_ = y.shape
        y = jnp.transpose(y, (0, 2, 1, 3))
        mu = jnp.mean(y, axis=-1, keepdims=True)
        var = jnp.mean(jnp.square(y - mu), axis=-1, keepdims=True)
        y = ((y - mu) * lax.rsqrt(var + EPS)).reshape(b_, t_, h_ * d_) * gn_g.astype(f32)
        y = y * jax.nn.silu(g.astype(f32))
        return y.astype(g.dtype) @ w_out

    out_ctx = finish(y_ctx, g_ctx) if need_ctx_out else None
    return out_ctx, finish(y_lat, g_lat)


def expert_choice_ffn(h, router, w_gate, w_up, w_down):
    bn, t, _ = h.shape
    cap = CAPACITY_FACTOR * t // N_EXPERTS
    aff = jax.nn.softmax(h.astype(jnp.float32) @ router.astype(jnp.float32), axis=-1)
    gate, idx = lax.top_k(jnp.swapaxes(aff, 1, 2), cap)
    bidx = jnp.arange(bn)[:, None, None]
    xs = h[bidx, idx]
    hid = jax.nn.silu(jnp.einsum('becd,edf->becf', xs, w_gate)) * jnp.einsum('becd,edf->becf', xs, w_up)
    y = jnp.einsum('becf,efd->becd', hid, w_down) * gate[..., None].astype(h.dtype)
    return jnp.zeros_like(h).at[bidx, idx].add(y)


def setup_inputs(seed: int = 0) -> dict:
    key = jax.random.key(seed)
    ks = jax.random.split(key, 24)
    f32 = jnp.float32
    D = D_MODEL
    nqk = 2 * A_HEADS * A_DK
    nrm = lambda k, shape, s: jax.random.normal(k, shape, f32) * s
    fbase = jnp.linspace(3.0, 6.0, A_HEADS, dtype=f32)
    dbase = jnp.log(2.0 ** (5.0 + jnp.arange(B_HEADS, dtype=f32)) - 1.0)
    return {
        "x": nrm(ks[0], (BATCH, SEQ, D), 1.0),
        "c": nrm(ks[1], (BATCH, D), 1.0),
        "ctx": nrm(ks[2], (BATCH, CTX_LEN, D), 1.0),
        "c_ctx": nrm(ks[3], (D,), 1.0),
        "ada_w": nrm(ks[4], (DEPTH, D, N_MOD * D), 0.5 * D ** -0.5),
        "ada_b": nrm(ks[5], (DEPTH, N_MOD * D), 0.02),
        "norm_mix_g": 1.0 + nrm(ks[6], (DEPTH, D), 0.02),
        "norm_ffn_g": 1.0 + nrm(ks[7], (DEPTH, D), 0.02),
        "final_norm_g": 1.0 + nrm(ks[8], (D,), 0.02),
        "mlstm_w_in": nrm(ks[9], (N_A_LAYERS, D, A_PROJ), D ** -0.5),
        "mlstm_conv_w": nrm(ks[10], (N_A_LAYERS, CONV_W, nqk), CONV_W ** -0.5),
        "mlstm_conv_b": nrm(ks[11], (N_A_LAYERS, nqk), 0.02),
        "mlstm_igate_b": nrm(ks[12], (N_A_LAYERS, 2, A_HEADS), 0.1),
        "mlstm_fgate_b": fbase + nrm(ks[13], (N_A_LAYERS, 2, A_HEADS), 0.1),
        "mlstm_head_norm_g": 1.0 + nrm(ks[14], (N_A_LAYERS, A_HEADS * A_DV), 0.02),
        "mlstm_w_out": nrm(ks[15], (N_A_LAYERS, A_HEADS * A_DV, D), (A_HEADS * A_DV) ** -0.5),
        "ret_w_in": nrm(ks[16], (N_B_LAYERS, D, B_PROJ), D ** -0.5),
        "ret_decay_logit": dbase + nrm(ks[17], (N_B_LAYERS, 2, B_HEADS), 0.1),
        "ret_group_norm_g": 1.0 + nrm(ks[18], (N_B_LAYERS, B_HEADS * B_DV), 0.02),
        "ret_w_out": nrm(ks[19], (N_B_LAYERS, B_HEADS * B_DV, D), (B_HEADS * B_DV) ** -0.5),
        "moe_router": nrm(ks[20], (DEPTH, D, N_EXPERTS), D ** -0.5),
        "moe_w_gate": nrm(ks[21], (DEPTH, N_EXPERTS, D, EXPERT_FF), D ** -0.5),
        "moe_w_up": nrm(ks[22], (DEPTH, N_EXPERTS, D, EXPERT_FF), D ** -0.5),
        "moe_w_down": nrm(ks[23], (DEPTH, N_EXPERTS, EXPERT_FF, D), EXPERT_FF ** -0.5),
    }


def reference(x, c, ctx, c_ctx, ada_w, ada_b, norm_mix_g, norm_ffn_g, final_norm_g,
              mlstm_w_in, mlstm_conv_w, mlstm_conv_b, mlstm_igate_b, mlstm_fgate_b,
              mlstm_head_norm_g, mlstm_w_out, ret_w_in, ret_decay_logit, ret_group_norm_g,
              ret_w_out, moe_router, moe_w_gate, moe_w_up, moe_w_down):
    c_lat = jax.nn.silu(c)[:, None, :]
    c_con = jax.nn.silu(c_ctx)[None, None, :]
    for i in range(DEPTH):
        last = i == DEPTH - 1
        mod_l = jnp.split(c_lat @ ada_w[i] + ada_b[i], N_MOD, axis=-1)
        mod_c = jnp.split(c_con @ ada_w[i] + ada_b[i], N_MOD, axis=-1)
        h_lat = modulate(rmsnorm(x, norm_mix_g[i]), mod_l[0], mod_l[1])
        h_ctx = modulate(rmsnorm(ctx, norm_mix_g[i]), mod_c[0], mod_c[1])
        j = i // N_MIXERS
        if i % N_MIXERS == 0:
            o_ctx, o_lat = mlstm_mixer(h_ctx, h_lat, mlstm_w_in[j], mlstm_conv_w[j], mlstm_conv_b[j],
                                       mlstm_igate_b[j], mlstm_fgate_b[j], mlstm_head_norm_g[j],
                                       mlstm_w_out[j], not last)
        else:
            o_ctx, o_lat = retention_mixer(h_ctx, h_lat, ret_w_in[j], ret_decay_logit[j],
                                           ret_group_norm_g[j], ret_w_out[j], not last)
        x = x + mod_l[2] * o_lat
        h_lat = modulate(rmsnorm(x, norm_ffn_g[i]), mod_l[3], mod_l[4])
        x = x + mod_l[5] * expert_choice_ffn(h_lat, moe_router[i], moe_w_gate[i], moe_w_up[i], moe_w_down[i])
        if not last:
            ctx = ctx + mod_c[2] * o_ctx
            h_ctx = modulate(rmsnorm(ctx, norm_ffn_g[i]), mod_c[3], mod_c[4])
            ctx = ctx + mod_c[5] * expert_choice_ffn(h_ctx, moe_router[i], moe_w_gate[i], moe_w_up[i], moe_w_down[i])
    return rmsnorm(x, final_norm_g)
```

```python
import numpy as np
from contextlib import ExitStack
import concourse.bass as bass
import concourse.mybir as mybir
from concourse.bass_utils import run_bass_kernel_spmd

F32 = mybir.dt.float32
BF16 = mybir.dt.bfloat16
I32 = mybir.dt.int32
ALU = mybir.AluOpType
AF = mybir.ActivationFunctionType
AX = mybir.AxisListType

ENGS = ("sp", "pe", "act", "dve", "pool")
NDMASEM = 24

D = 1024
SEQ = 2048
CTX = 256
DEPTH = 4
NE = 16
EPS = 1e-6
NCORES = 8
BPC = 2


class Tile:
    __slots__ = ("ap", "name", "last_w", "readers")

    def __init__(self, ap, name):
        self.ap = ap
        self.name = name
        self.last_w = None
        self.readers = []

    def __getitem__(self, k):
        return self.ap[k]


class Op:
    __slots__ = ("idx", "eng", "fn", "deps", "signal", "is_dma", "semi", "semval", "pos", "kind", "nconsumers")

    def __init__(self, idx, eng, fn, is_dma=False, kind="op"):
        self.idx = idx
        self.eng = eng
        self.fn = fn
        self.deps = []
        self.signal = False
        self.is_dma = is_dma
        self.semi = None
        self.semval = None
        self.pos = None
        self.kind = kind
        self.nconsumers = 0


class Prog:
    def __init__(self, nc):
        self.nc = nc
        self.ops = []
        self.streams = {e: [] for e in ENGS}
        self.waited = {e: {f: -1 for f in ENGS} for e in ENGS}
        self.waited_dma = {e: {} for e in ENGS}
        self.dma_rr = 0
        self.dma_last = [None] * NDMASEM
        self.dma_count = [0] * NDMASEM
        self.unconsumed_dma = []
        self.last_op = {e: None for e in ENGS}
        self.nbar = 0

    def _add_dep(self, op, prod):
        if prod is None or prod is op:
            return
        e = op.eng
        if prod.is_dma:
            w = self.waited_dma[e].get(prod.semi, 0)
            if w >= prod.semval:
                return
            self.waited_dma[e][prod.semi] = prod.semval
            op.deps.append(prod)
            prod.nconsumers += 1
        else:
            f = prod.eng
            if e == "pe" and f == "pe":
                return
            if self.waited[e][f] >= prod.pos:
                return
            self.waited[e][f] = prod.pos
            prod.signal = True
            op.deps.append(prod)

    def _track(self, op, reads, writes):
        for t in reads:
            self._add_dep(op, t.last_w)
        for t in writes:
            self._add_dep(op, t.last_w)
            for r in t.readers:
                self._add_dep(op, r)
        for t in reads:
            t.readers.append(op)
        for t in writes:
            t.last_w = op
            t.readers = []

    def _push(self, op):
        op.pos = len(self.streams[op.eng])
        self.streams[op.eng].append(op)
        self.ops.append(op)
        if not op.is_dma:
            self.last_op[op.eng] = op

    def op(self, eng, fn, reads=(), writes=()):
        o = Op(len(self.ops), eng, fn)
        self._push(o)
        self._track(o, reads, writes)
        return o

    def dma(self, queue, out_ap, in_ap, reads=(), writes=(), **kw):
        o = Op(len(self.ops), queue, None, is_dma=True)
        k = self.dma_rr
        self.dma_rr = (self.dma_rr + 1) % NDMASEM
        o.semi = k
        self.dma_count[k] += 1
        o.semval = 16 * self.dma_count[k]
        o.fn = lambda eng: eng.dma_start(out=out_ap, in_=in_ap, **kw)
        self._push(o)
        prev = self.dma_last[k]
        if prev is not None:
            self._add_dep(o, prev)
        self.dma_last[k] = o
        self._track(o, reads, writes)
        self.unconsumed_dma.append(o)
        return o

    def barrier(self):
        self.nbar += 1
        n = self.nbar
        b = Op(len(self.ops), "sp", None, kind="bar_inc")
        b.semval = n
        self._push(b)
        for e in ENGS:
            if e != "sp":
                self._add_dep(b, self.last_op[e])
        for d in self.unconsumed_dma:
            self._add_dep(b, d)
        self.unconsumed_dma = []
        for e in ENGS:
            if e == "sp":
                continue
            w = Op(len(self.ops), e, None, kind="bar_wait")
            w.semval = n
            self._push(w)

    def emit(self):
        nc = self.nc
        with ExitStack() as es:
            esem = {e: es.enter_context(nc.semaphore(f"es_{e}")) for e in ENGS}
            dsem = [es.enter_context(nc.semaphore(f"ds_{i}")) for i in range(NDMASEM)]
            bsem = es.enter_context(nc.semaphore("barsem"))
            for e in ENGS:
                c = 0
                for o in self.streams[e]:
                    if o.is_dma or o.kind != "op":
                        continue
                    if o.signal:
                        c += 1
                        o.semval = c
                        o.semi = e
            block = es.enter_context(nc.Block())

            def run(e, eng):
                for o in self.streams[e]:
                    for p in o.deps:
                        if p.is_dma:
                            eng.wait_ge(dsem[p.semi], p.semval)
                        else:
                            eng.wait_ge(esem[p.eng], p.semval)
                    if o.kind == "bar_inc":
                        eng.sem_inc(bsem, 1)
                        continue
                    if o.kind == "bar_wait":
                        eng.wait_ge(bsem, o.semval)
                        continue
                    ins = o.fn(eng)
                    if o.is_dma:
                        ins.then_inc(dsem[o.semi], 16)
                    elif o.signal:
                        ins.then_inc(esem[e], 1)

            @block.sync
            def _(eng):
                run("sp", eng)

            @block.tensor
            def _(eng):
                run("pe", eng)

            @block.scalar
            def _(eng):
                run("act", eng)

            @block.vector
            def _(eng):
                run("dve", eng)

            @block.gpsimd
            def _(eng):
                run("pool", eng)


class Arena:
    def __init__(self, t, ncols):
        self.t = t
        self.n = ncols
        self.off = 0

    def alloc(self, name, free, dt, parts=128):
        free = list(free)
        n = int(np.prod(free))
        cols = n * (2 if dt in (F32, I32) else 1)
        cols = (cols + 1) // 2 * 2
        assert self.off + cols <= self.n, f"arena overflow {name} {self.off}+{cols}>{self.n}"
        ap = self.t[0:parts, self.off:self.off + cols]
        self.off += cols
        if dt != BF16:
            ap = ap.bitcast(dt)
        if n * (2 if dt in (F32, I32) else 1) != cols:
            ap = ap[:, 0:n]
        if len(free) == 2:
            ap = ap.rearrange("p (a b) -> p a b", a=free[0])
        elif len(free) == 3:
            ap = ap.rearrange("p (a b c) -> p a b c", a=free[0], b=free[1])
        return Tile(ap, name)

    def reset(self, to=0):
        self.off = to


class Ring:
    def __init__(self, tiles):
        self.tiles = tiles
        self.i = 0

    def next(self):
        t = self.tiles[self.i % len(self.tiles)]
        self.i += 1
        return t


class K:
    def __init__(self, P):
        self.P = P
        self.flip = 0

    def mm(self, ps, out, lhsT, rhs, start, stop, rd):
        self.P.op("pe", lambda e: e.matmul(out, lhsT=lhsT, rhs=rhs, start=start, stop=stop), reads=rd, writes=[ps])

    def tr(self, ps, out, in_, ident, rd):
        self.P.op("pe", lambda e: e.transpose(out, in_, ident), reads=rd, writes=[ps])

    def act(self, out, in_, func, rd, wr, bias=None, scale=None, accum=None):
        kw = {}
        if bias is not None:
            kw["bias"] = bias
        if scale is not None:
            kw["scale"] = scale
        if accum is not None:
            kw["accum_out"] = accum
        self.P.op("act", lambda e: e.activation(out=out, in_=in_, func=func, **kw), reads=rd, writes=wr)

    def tt(self, eng, out, in0, in1, op, rd, wr):
        self.P.op(eng, lambda e: e.tensor_tensor(out=out, in0=in0, in1=in1, op=op), reads=rd, writes=wr)

    def ts(self, eng, out, in0, s1, op0, rd, wr, s2=None, op1=None, accum=None):
        kw = {}
        if op1 is not None:
            kw["op1"] = op1
        if accum is not None:
            kw["accum_out"] = accum
        self.P.op(eng, lambda e: e.tensor_scalar(out=out, in0=in0, scalar1=s1, scalar2=s2, op0=op0, **kw), reads=rd, writes=wr)

    def stt(self, eng, out, in0, scalar, in1, op0, op1, rd, wr):
        self.P.op("dve", lambda e: e.scalar_tensor_tensor(out=out, in0=in0, scalar=scalar, in1=in1, op0=op0, op1=op1), reads=rd, writes=wr)

    def copy(self, eng, out, in_, rd, wr):
        if eng == "act":
            self.P.op("act", lambda e: e.copy(out=out, in_=in_), reads=rd, writes=wr)
        else:
            self.P.op(eng, lambda e: e.tensor_copy(out=out, in_=in_), reads=rd, writes=wr)

    def evac(self, out, in_, rd, wr):
        self.flip ^= 1
        self.copy("act" if self.flip else "dve", out, in_, rd, wr)

    def memset(self, eng, ap, val, wr):
        self.P.op(eng, lambda e: e.memset(ap, val), writes=wr)


def build(dbg=False):
    nc = bass.Bass("TRN2", target_bir_lowering=False)

    def din(name, shape):
        return nc.dram_tensor(name, list(shape), F32, kind="ExternalInput").ap()

    x_in = din("x", [BPC, SEQ, D])
    c_in = din("c", [BPC, D])
    ctx_in = din("ctx", [BPC, CTX, D])
    cctx_in = din("c_ctx", [D])
    ada_w = din("ada_w", [DEPTH, D, 6 * D])
    ada_b = din("ada_b", [DEPTH, 6 * D])
    norm_mix_g = din("norm_mix_g", [DEPTH, D])
    norm_ffn_g = din("norm_ffn_g", [DEPTH, D])
    final_norm_g = din("final_norm_g", [D])
    ml_w_in = din("mlstm_w_in", [2, D, 3104])
    ml_conv_w = din("mlstm_conv_w", [2, 5, 1024])
    ml_conv_b = din("mlstm_conv_b", [2, 1024])
    ml_ig_b = din("mlstm_igate_b", [2, 16])
    ml_fg_b = din("mlstm_fgate_b", [2, 16])
    ml_hn_g = din("mlstm_head_norm_g", [2, 1024])
    ml_w_out = din("mlstm_w_out", [2, 1024, D])
    rt_w_in = din("ret_w_in", [2, D, 4096])
    rt_decay = din("ret_decay_logit", [2, 16])
    rt_gn_g = din("ret_group_norm_g", [2, 1024])
    rt_w_out = din("ret_w_out", [2, 1024, D])
    moe_router = din("moe_router", [DEPTH, D, NE])
    moe_wg = din("moe_w_gate", [DEPTH, NE, D, D])
    moe_wu = din("moe_w_up", [DEPTH, NE, D, D])
    moe_wd = din("moe_w_down", [DEPTH, NE, D, D])
    out_ap = nc.dram_tensor("out", [BPC, SEQ, D], F32, kind="ExternalOutput").ap()

    def scr(name, shape, dt=F32):
        return nc.dram_tensor(name, list(shape), dt, kind="Internal").ap()

    SEQS = []
    for b in range(BPC):
        SEQS.append(dict(name=f"c{b}", b=b, ctx=True, T=CTX, kind=2, xin=ctx_in[b]))
        SEQS.append(dict(name=f"l{b}", b=b, ctx=False, T=SEQ, kind=b, xin=x_in[b]))
    for s in SEQS:
        T = s["T"]
        n = s["name"]
        s["XS"] = scr(f"XS_{n}", [T, D])
        s["FM"] = scr(f"FM_{n}", [2048, T])
        s["TM"] = scr(f"TM_{n}", [T, 2080])
        s["QK"] = scr(f"QK_{n}", [2048, T], BF16)
        s["KT"] = scr(f"KT_{n}", [T, 1024], BF16)
        s["Y"] = scr(f"Y_{n}", [T, D])
        s["H2"] = scr(f"H2_{n}", [T, D], BF16)
        s["AFFT"] = scr(f"AFFT_{n}", [NE, T])
        s["YE"] = scr(f"YE_{n}", [NE, T // 8, D], BF16)
        s["nt"] = T // 128
        s["cap"] = T // 8
    MODS = scr("MODS", [DEPTH, 6, 3, D])

    dbg_outs = {}

    def dbg_tap(name, shape):
        dbg_outs[name] = nc.dram_tensor(name, list(shape), F32, kind="ExternalOutput").ap()
        return dbg_outs[name]

    P = Prog(nc)
    k = K(P)
    with ExitStack() as es:
        CA_COLS = 20 * 1024
        PA_COLS = 83 * 1024
        ca_t = es.enter_context(nc.sbuf_tensor("carena", [128, CA_COLS], BF16))
        pa_t = es.enter_context(nc.sbuf_tensor("parena", [128, PA_COLS], BF16))
        CA = Arena(ca_t, CA_COLS)
        PA = Arena(pa_t, PA_COLS)
        PS = [Tile(es.enter_context(nc.psum_tensor(f"psb{i}", [128, 512], F32)), f"psb{i}") for i in range(8)]

        def ps_bf(t):
            return t.ap.bitcast(BF16)

        iota_p = CA.alloc("iota_p", [1], F32)
        iota_p128 = CA.alloc("iota_p128", [1], F32)
        iota_f = CA.alloc("iota_f", [2048], F32)
        ident32 = CA.alloc("ident32", [128], F32)
        identbf = CA.alloc("identbf", [128], BF16)
        Mfwd = CA.alloc("Mfwd", [128], F32)
        Mbwd = CA.alloc("Mbwd", [128], F32)
        ones32 = CA.alloc("ones32", [128], F32)
        oh32 = CA.alloc("oh32", [32, 128], BF16, parts=32)
        Psw = CA.alloc("Psw", [128], F32)
        cosT = CA.alloc("cosT", [2048], F32)
        sinT = CA.alloc("sinT", [2048], F32)
        P.op("pool", lambda e: e.iota(iota_p[:], pattern=[[0, 1]], base=0, channel_multiplier=1, allow_small_or_imprecise_dtypes=True), writes=[iota_p])
        P.op("pool", lambda e: e.iota(iota_p128[:], pattern=[[0, 1]], base=128, channel_multiplier=1, allow_small_or_imprecise_dtypes=True), writes=[iota_p128])
        P.op("pool", lambda e: e.iota(iota_f[:], pattern=[[1, 2048]], base=0, channel_multiplier=0, allow_small_or_imprecise_dtypes=True), writes=[iota_f])
        k.ts("dve", ident32[:], iota_f[:, 0:128], iota_p[:, 0:1], ALU.is_equal, [iota_f, iota_p], [ident32])
        k.copy("dve", identbf[:], ident32[:], [ident32], [identbf])
        k.ts("dve", Mfwd[:], iota_f[:, 0:128], iota_p[:, 0:1], ALU.is_ge, [iota_f, iota_p], [Mfwd])
        k.ts("dve", Mbwd[:], iota_f[:, 0:128], iota_p[:, 0:1], ALU.is_le, [iota_f, iota_p], [Mbwd])
        k.memset("dve", ones32[:], 1.0, [ones32])
        ohf = PA.alloc("ohf", [32, 128], F32, parts=32)
        for e_ in range(32):
            k.ts("dve", ohf[:, e_, :], ones32[0:32, :], float(e_), ALU.mult, [ones32], [ohf])
        k.ts("dve", oh32[:].rearrange("p a b -> p (a b)"), ohf[:].rearrange("p a b -> p (a b)"), iota_p[0:32, 0:1], ALU.is_equal, [ohf, iota_p], [oh32])
        tA = PA.alloc("tA", [128], F32)
        tB = PA.alloc("tB", [128], F32)
        tC = PA.alloc("tC", [128], F32)
        pm32 = PA.alloc("pm32", [2], F32)
        k.ts("dve", pm32[:, 0:1], iota_p[:, 0:1], -32.0, ALU.add, [iota_p], [pm32])
        k.ts("dve", pm32[:, 1:2], iota_p[:, 0:1], 32.0, ALU.add, [iota_p], [pm32])
        k.ts("dve", tA[:], iota_f[:, 0:128], pm32[:, 0:1], ALU.is_equal, [iota_f, pm32], [tA])
        k.ts("dve", tB[:], iota_f[:, 0:128], pm32[:, 1:2], ALU.is_equal, [iota_f, pm32], [tB])
        tD = PA.alloc("tD", [128], F32)
        k.ts("dve", tC[:], iota_f[:, 0:128], 64.0, ALU.is_ge, [iota_f], [tC], s2=None)
        k.ts("dve", tD[:], iota_f[:, 0:128], 96.0, ALU.is_lt, [iota_f], [tD])
        k.tt("dve", tC[:], tC[:], tD[:], ALU.mult, [tC, tD], [tC])
        k.ts("dve", tD[:], iota_f[:, 0:128], 32.0, ALU.is_lt, [iota_f], [tD])
        k.tt("dve", tC[:], tC[:], tD[:], ALU.add, [tC, tD], [tC])
        k.tt("dve", tA[:], tA[:], tC[:], ALU.mult, [tA, tC], [tA])
        k.ts("dve", tC[:], tC[:], -1.0, ALU.mult, [tC], [tC], s2=1.0, op1=ALU.add)
        k.tt("dve", tB[:], tB[:], tC[:], ALU.mult, [tB, tC], [tB])
        k.tt("dve", Psw[:], tB[:], tA[:], ALU.subtract, [tA, tB], [Psw])
        inv = PA.alloc("inv", [1], F32)
        posT = PA.alloc("posT", [2048], F32)
        ang = PA.alloc("ang", [2048], F32)
        inv2 = PA.alloc("inv2", [1], F32)
        k.copy("dve", inv[:], iota_p[:, 0:1], [iota_p], [inv])
        for thr in (32.0, 64.0, 96.0):
            k.ts("dve", inv2[:], iota_p[:, 0:1], thr, ALU.is_ge, [iota_p], [inv2], s2=-32.0, op1=ALU.mult)
            k.tt("dve", inv[:], inv[:], inv2[:], ALU.add, [inv, inv2], [inv])
        k.act(inv[:], inv[:], AF.Exp, [inv], [inv], scale=-float(np.log(10000.0) / 32.0))
        P.op("pool", lambda e: e.iota(posT[64:128, :], pattern=[[0, 32], [1, 64]], base=0, channel_multiplier=0, allow_small_or_imprecise_dtypes=True), writes=[posT])
        P.op("pool", lambda e: e.iota(posT[0:64, :], pattern=[[1, 32], [0, 64]], base=0, channel_multiplier=0, allow_small_or_imprecise_dtypes=True), writes=[posT])
        k.ts("dve", ang[:], posT[:], inv[:, 0:1], ALU.mult, [posT, inv], [ang])
        PI = float(np.pi)
        red = PA.alloc("red", [2048], F32)
        cmpt = PA.alloc("cmpt", [2048], F32)
        for (dst, shift) in ((sinT, 0.0), (cosT, 0.5 * PI)):
            k.ts("dve", red[:], ang[:], shift, ALU.add, [ang], [red])
            for kk in range(1, 12):
                k.ts("dve", cmpt[:], ang[:], 2 * PI * kk - PI - shift, ALU.is_ge, [ang], [cmpt], s2=-2 * PI, op1=ALU.mult)
                k.tt("dve", red[:], red[:], cmpt[:], ALU.add, [red, cmpt], [red])
            k.ts("dve", red[:], red[:], -3.141592, ALU.max, [red], [red], s2=3.141592, op1=ALU.min)
            k.act(dst[:], red[:], AF.Sin, [red], [dst])

        cT = CA.alloc("cT", [8, 3], F32)
        for b in range(BPC):
            P.dma("sp", cT[:, :, b], c_in[b].rearrange("(kc p) -> p kc", p=128), writes=[cT], allow_slow_non_contiguous=True)
        P.dma("sp", cT[:, :, 2], cctx_in.rearrange("(kc p) -> p kc", p=128), writes=[cT], allow_slow_non_contiguous=True)
        k.act(cT[:], cT[:], AF.Silu, [cT], [cT])

        def ada_alloc():
            return dict(awr=Ring([PA.alloc(f"aw{i}", [8, 512], F32) for i in range(2)]),
                        abr=Ring([PA.alloc(f"ab{i}", [512], F32, parts=3) for i in range(2)]),
                        gr=Ring([PA.alloc(f"gg{i}", [512], F32, parts=3) for i in range(2)]),
                        mrow=Ring([PA.alloc(f"mrow{i}", [512], F32, parts=3) for i in range(3)]))

        def ada_block(R_, l_, nb, psr):
            m, hf = nb // 2, nb % 2
            aw = R_["awr"].next()
            P.dma("sp", aw[:], ada_w[l_].rearrange("(kc p) n -> p kc n", p=128)[:, :, nb * 512:(nb + 1) * 512], writes=[aw])
            ab = R_["abr"].next()
            P.dma("sp", ab[:], ada_b[l_, nb * 512:(nb + 1) * 512].partition_broadcast(3), writes=[ab])
            ps = psr.next()
            for kc in range(8):
                k.mm(ps, ps[0:3, :], cT[:, kc, :], aw[:, kc, :], kc == 0, kc == 7, [cT, aw])
            mr = R_["mrow"].next()
            k.tt("dve", mr[:], ps[0:3, :], ab[:], ALU.add, [ps, ab], [mr])
            if m in (1, 4):
                g = R_["gr"].next()
                gsrc = norm_mix_g if m == 1 else norm_ffn_g
                P.dma("sp", g[:], gsrc[l_, hf * 512:(hf + 1) * 512].partition_broadcast(3), writes=[g])
                k.stt("dve", mr[:], mr[:], 1.0, g[:], ALU.add, ALU.mult, [mr, g], [mr])
            slot = {1: 0, 0: 1, 2: 2, 4: 3, 3: 4, 5: 5}[m]
            P.dma("pool", MODS[l_, slot, :, hf * 512:(hf + 1) * 512], mr[:], reads=[mr])

        R0 = ada_alloc()
        psr0 = Ring(PS[0:2])
        for nb in range(12):
            ada_block(R0, 0, nb, psr0)
        P.barrier()
        PA.reset()

        def load_bc(name, src_row):
            t = PA.alloc(name, [D], F32)
            P.dma("sp", t[:], src_row.partition_broadcast(128), writes=[t])
            return t

        def rstd_from_ssq(ssq, n, scr2):
            raise NotImplementedError

        for l in range(DEPTH):
            last = l == DEPTH - 1
            is_ml = (l % 2 == 0)
            j = l // 2
            if is_ml:
                w_in, NP, NFM, NHDK = ml_w_in[j], 3104, 8, 512
                w_out, hn_g = ml_w_out[j], ml_hn_g[j]
                dk, VC0, OC0 = 64, 0, 1024
            else:
                w_in, NP, NFM, NHDK = rt_w_in[j], 4096, 16, 1024
                w_out, hn_g = rt_w_out[j], rt_gn_g[j]
                dk, VC0, OC0 = 128, 0, 1024
            NTM = NP - NFM * 128

            def xsrc(s):
                return s["xin"] if l == 0 else s["XS"]

            Win = PA.alloc("Win", [8, NP], BF16)
            for kc in range(8):
                P.dma("pool", Win[:, kc, :], w_in[kc * 128:(kc + 1) * 128, :], writes=[Win])
            A1 = [load_bc(f"A1_{kd}", MODS[l, 0, kd]) for kd in range(3)]
            B1 = [load_bc(f"B1_{kd}", MODS[l, 1, kd]) for kd in range(3)]
            xr = Ring([PA.alloc(f"xt{i}", [D], F32) for i in range(3)])
            junk = PA.alloc("junk", [D], F32)
            ssr = Ring([PA.alloc(f"ss{i}", [2], F32) for i in range(3)])
            tmpr = Ring([PA.alloc(f"tmp{i}", [D], F32) for i in range(2)])
            hbr = Ring([PA.alloc(f"hb{i}", [D], BF16) for i in range(2)])
            hTr = Ring([PA.alloc(f"hT{i}", [8, 512], BF16) for i in range(2)])
            fmr = Ring([PA.alloc(f"fms{i}", [512], F32) for i in range(3)])
            tmr = Ring([PA.alloc(f"tms{i}", [NTM], F32) for i in range(2)])
            ps_tr = Ring(PS[0:2])
            ps_mm = Ring(PS[2:8])
            for s in SEQS:
                T, kd = s["T"], s["kind"]
                NB = min(512, T)
                for blk in range(T // NB):
                    hT = hTr.next()
                    for ti in range(NB // 128):
                        t0 = blk * NB + ti * 128
                        xt = xr.next()
                        P.dma("sp", xt[:], xsrc(s)[t0:t0 + 128, :], writes=[xt])
                        ss = ssr.next()
                        k.act(junk[:], xt[:], AF.Square, [xt], [junk, ss], accum=ss[:, 0:1])
                        k.act(ss[:, 1:2], ss[:, 0:1], AF.Ln, [ss], [ss], scale=1.0 / D, bias=EPS)
                        k.act(ss[:, 1:2], ss[:, 1:2], AF.Exp, [ss], [ss], scale=-0.5)
                        tmp = tmpr.next()
                        k.stt("dve", tmp[:], xt[:], ss[:, 1:2], A1[kd][:], ALU.mult, ALU.mult, [xt, ss, A1[kd]], [tmp])
                        hb = hbr.next()
                        k.tt("dve", hb[:], tmp[:], B1[kd][:], ALU.add, [tmp, B1[kd]], [hb])
                        pt = ps_tr.next()
                        for kc in range(8):
                            k.tr(pt, ps_bf(pt)[:, kc * 128:(kc + 1) * 128], hb[:, kc * 128:(kc + 1) * 128], identbf[:], [hb, identbf])
                        k.evac(hT[:, :, ti * 128:(ti + 1) * 128], ps_bf(pt).rearrange("p (a b) -> p a b", a=8), [pt], [hT])
                    for fc in range(NFM):
                        ps = ps_mm.next()
                        for kc in range(8):
                            k.mm(ps, ps[:, 0:NB], Win[:, kc, fc * 128:(fc + 1) * 128], hT[:, kc, 0:NB], kc == 0, kc == 7, [Win, hT])
                        st = fmr.next()
                        k.evac(st[:, 0:NB], ps[:, 0:NB], [ps], [st])
                        P.dma("pool", s["FM"][fc * 128:(fc + 1) * 128, blk * NB:(blk + 1) * NB], st[:, 0:NB], reads=[st])
                    for ti in range(NB // 128):
                        t0 = blk * NB + ti * 128
                        st = tmr.next()
                        c0 = 0
                        while c0 < NTM:
                            cw = min(512, NTM - c0)
                            ps = ps_mm.next()
                            for kc in range(8):
                                k.mm(ps, ps[:, 0:cw], hT[:, kc, ti * 128:(ti + 1) * 128], Win[:, kc, NFM * 128 + c0:NFM * 128 + c0 + cw], kc == 0, kc == 7, [Win, hT])
                            k.evac(st[:, c0:c0 + cw], ps[:, 0:cw], [ps], [st])
                            c0 += cw
                        P.dma("pool", s["TM"][t0:t0 + 128, 0:NTM], st[:], reads=[st])
            P.barrier()
            PA.reset()

            QOFF, KOFF = 0, NHDK
            if is_ml:
                cwt = PA.alloc("cwt", [8, 5], F32)
                cbt = PA.alloc("cbt", [8], F32)
                for jj in range(5):
                    P.dma("sp", cwt[:, :, jj], ml_conv_w[j, jj].rearrange("(fc p) -> p fc", p=128), writes=[cwt], allow_slow_non_contiguous=True)
                P.dma("sp", cbt[:], ml_conv_b[j].rearrange("(fc p) -> p fc", p=128), writes=[cbt], allow_slow_non_contiguous=True)
            xpr = {T_: Ring([PA.alloc(f"xp{T_}_{i}", [T_ + 4], F32) for i in range(2)]) for T_ in (CTX, SEQ)}
            for T_ in (CTX, SEQ):
                for t_ in xpr[T_].tiles:
                    k.memset("pool", t_[:, 0:2], 0.0, [t_])
                    k.memset("pool", t_[:, T_ + 2:T_ + 4], 0.0, [t_])
            accr = Ring([PA.alloc(f"acc{i}", [SEQ], F32) for i in range(2)])
            t2r = Ring([PA.alloc(f"t2{i}", [SEQ], F32) for i in range(2)])
            qbr = Ring([PA.alloc(f"qb{i}", [SEQ], BF16) for i in range(3)])
            ktr = Ring([PA.alloc(f"kts{i}", [16, 128], BF16) for i in range(2)])
            ps_tr = Ring(PS[0:2])
            ps_sw = Ring(PS[2:6])
            kscale = float(128.0 ** -0.5)
            ada_todo = []
            if l + 1 < DEPTH:
                R_ada = ada_alloc()
                ps_ada = Ring(PS[6:8])
                ada_todo = list(range(12))
            for s in SEQS:
                T, nt = s["T"], s["nt"]
                for fc in range(NFM):
                    if ada_todo:
                        ada_block(R_ada, l + 1, ada_todo.pop(0), ps_ada)
                    is_k = fc >= NFM // 2
                    xp = xpr[T].next()
                    P.dma("sp", xp[:, 2:T + 2], s["FM"][fc * 128:(fc + 1) * 128, 0:T], writes=[xp])
                    qb = qbr.next()
                    if is_ml:
                        acc = accr.next()
                        k.ts("dve", acc[:, 0:T], xp[:, 0:T], cwt[:, fc, 0:1], ALU.mult, [xp, cwt, cbt], [acc], s2=cbt[:, fc:fc + 1], op1=ALU.add)
                        for jj in range(1, 5):
                            eng = "pool" if jj in (2, 4) else "dve"
                            k.stt(eng, acc[:, 0:T], xp[:, jj:jj + T], cwt[:, fc, jj:jj + 1], acc[:, 0:T], ALU.mult, ALU.add, [xp, cwt, acc], [acc])
                        if is_k:
                            k.act(qb[:, 0:T], acc[:, 0:T], AF.Silu, [acc], [qb])
                        else:
                            t2 = t2r.next()
                            k.act(t2[:, 0:T], acc[:, 0:T], AF.Silu, [acc], [t2])
                            k.ts("dve", qb[:, 0:T], t2[:, 0:T], 0.125, ALU.mult, [t2], [qb])
                    else:
                        sc_ = kscale if is_k else 1.0
                        if s["ctx"]:
                            k.ts("dve", qb[:, 0:T], xp[:, 2:T + 2], sc_, ALU.mult, [xp], [qb])
                        else:
                            acc = accr.next()
                            t2 = t2r.next()
                            for b4 in range(T // 512):
                                ps = ps_sw.next()
                                k.mm(ps, ps[:], Psw[:], xp[:, 2 + b4 * 512:2 + (b4 + 1) * 512], True, True, [Psw, xp])
                                k.stt("dve", t2[:, b4 * 512:(b4 + 1) * 512], ps[:], sc_, sinT[:, b4 * 512:(b4 + 1) * 512], ALU.mult, ALU.mult, [ps, sinT], [t2])
                            k.stt("pool", acc[:, 0:T], xp[:, 2:T + 2], sc_, cosT[:, 0:T], ALU.mult, ALU.mult, [xp, cosT], [acc])
                            k.tt("dve", qb[:, 0:T], acc[:, 0:T], t2[:, 0:T], ALU.add, [acc, t2], [qb])
                    P.dma("pool", s["QK"][fc * 128:(fc + 1) * 128, 0:T], qb[:, 0:T], reads=[qb])
                    if is_k:
                        kt = ktr.next()
                        for g0 in range(0, nt, 8):
                            pt = ps_tr.next()
                            ng = min(8, nt - g0)
                            for ti in range(ng):
                                k.tr(pt, ps_bf(pt)[:, ti * 128:(ti + 1) * 128], qb[:, (g0 + ti) * 128:(g0 + ti + 1) * 128], identbf[:], [qb, identbf])
                            k.evac(kt[:, g0:g0 + ng, :], ps_bf(pt)[:, 0:ng * 128].rearrange("p (a b) -> p a b", a=ng), [pt], [kt])
                        kc0 = (fc - NFM // 2) * 128
                        P.dma("pool", s["KT"].rearrange("(n p) f -> p n f", p=128)[:, :, kc0:kc0 + 128], kt[:, 0:nt, :], reads=[kt])
            P.barrier()
            PA.reset()

            NTT = 18
            NS = NTT - 1
            ORD = [list(range(NTT)), [1, 0] + list(range(17, 1, -1))]
            gbias = PA.alloc("gbias", [32], F32)
            if is_ml:
                P.dma("sp", gbias[:, 0:16], ml_ig_b[j].partition_broadcast(128), writes=[gbias])
                P.dma("sp", gbias[:, 16:32], ml_fg_b[j].partition_broadcast(128), writes=[gbias])
            else:
                P.dma("sp", gbias[:, 16:32], rt_decay[j].partition_broadcast(128), writes=[gbias])
            Gt = PA.alloc("Gt", [NTT, 32], F32)
            igb = PA.alloc("igb", [NTT, 16], F32)
            spt = PA.alloc("spt", [NTT, 16], F32)
            chf = PA.alloc("chf", [NTT, 16], F32)
            gsets = [dict(wt=PA.alloc(f"wt{i}", [NTT, 16], F32), rowf=PA.alloc(f"rowf{i}", [NTT, 16], F32),
                          fS=[PA.alloc(f"fS{i}_{d}", [8, NTT + 1], F32) for d in range(2)]) for i in range(2)]
            hdr = [dict(
                qT=PA.alloc(f"qT{i}", [NTT * 128], BF16),
                kT=PA.alloc(f"kT{i}", [NTT * 128], BF16),
                ktok=PA.alloc(f"ktok{i}", [NTT, dk], BF16),
                vaug=PA.alloc(f"vaug{i}", [NTT, 129], BF16),
                vp=[PA.alloc(f"vp{i}_{d}", [NTT, 129], BF16) for d in range(2)],
                atm=[PA.alloc(f"atm{i}_{d}", [NTT, 128], BF16) for d in range(2)],
                cbf=[PA.alloc(f"cbf{i}_{d}", [NS, 129], BF16) for d in range(2)],
                Y=PA.alloc(f"Y{i}", [NTT, 128], F32),
            ) for i in range(2)]
            for hb_ in hdr:
                k.memset("pool", hb_["vaug"][:, :, 128:129], 1.0, [hb_["vaug"]])
            dCall = [PA.alloc(f"dCall{d}", [129, NS], F32) for d in range(2)]
            Fb = [PA.alloc(f"Fb{d}", [129, NS], F32) for d in range(2)]
            ats = Ring([PA.alloc(f"ats{i}", [128], F32) for i in range(3)])
            resr = Ring([PA.alloc(f"res{i}", [129], F32) for i in range(6)])
            ddr = Ring([PA.alloc(f"dd{i}", [2], F32) for i in range(6)])
            ps_g = PS[0]
            ps_at = Ring(PS[1:3])
            ps_dc = Ring(PS[3:5])
            ps_out = Ring(PS[5:8])

            def gate_prep(b, GS):
                sc, sl = SEQS[2 * b], SEQS[2 * b + 1]
                parts = [(sc, 0, 2), (sl, 2, 16)]
                wt, rowf, fS = GS["wt"], GS["rowf"], GS["fS"]
                if is_ml:
                    for (s, o0, n_) in parts:
                        P.dma("sp", Gt[:, o0:o0 + n_, :], s["TM"].rearrange("(n p) c -> p n c", p=128)[:, :, 2048:2080], writes=[Gt])
                    gb_ig = gbias[:, 0:16].unsqueeze(1).to_broadcast([128, NTT, 16])
                    gb_fg = gbias[:, 16:32].unsqueeze(1).to_broadcast([128, NTT, 16])
                    k.tt("dve", igb[:], Gt[:, :, 0:16], gb_ig, ALU.add, [Gt, gbias], [igb])
                    k.tt("dve", spt[:], Gt[:, :, 16:32], gb_fg, ALU.add, [Gt, gbias], [spt])
                else:
                    k.memset("dve", igb[:], 0.0, [igb])
                    k.copy("dve", spt[:], gbias[:, 16:32].unsqueeze(1).to_broadcast([128, NTT, 16]), [gbias], [spt])
                k.act(spt[:], spt[:], AF.Exp, [spt], [spt], scale=-1.0)
                k.act(spt[:], spt[:], AF.Ln, [spt], [spt], bias=1.0)
                for t_ in range(NTT):
                    k.mm(ps_g, ps_g[:, t_ * 16:t_ * 16 + 8], Mfwd[:], spt[:, t_, 0:8], True, True, [Mfwd, spt])
                    k.mm(ps_g, ps_g[:, t_ * 16 + 8:t_ * 16 + 16], Mbwd[:], spt[:, t_, 8:16], True, True, [Mbwd, spt])
                pt_ = ps_at.next()
                k.mm(pt_, pt_[:, 0:NTT * 16], ones32[:], spt[:].rearrange("p a b -> p (a b)"), True, True, [ones32, spt])
                cum = ps_g[:, 0:NTT * 16]
                tot = pt_[:, 0:NTT * 16]
                k.tt("dve", wt[:].rearrange("p a b -> p (a b)"), igb[:].rearrange("p a b -> p (a b)"), cum, ALU.add, [igb, ps_g], [wt])
                k.act(wt[:], wt[:], AF.Exp, [wt], [wt])
                k.act(rowf[:].rearrange("p a b -> p (a b)"), cum, AF.Exp, [ps_g], [rowf], scale=-1.0)
                k.act(chf[:].rearrange("p a b -> p (a b)"), tot, AF.Exp, [pt_], [chf], scale=-1.0)
                for d in range(2):
                    k.memset("pool", fS[d][:, :, 0:1], 0.0, [fS[d]])
                k.copy("pool", fS[0][:, :, 1:NTT + 1], chf[:, :, 0:8].rearrange("p t h -> p h t"), [chf], [fS[0]])
                for s_ in range(NTT):
                    k.copy("pool", fS[1][:, :, s_ + 1], chf[:, ORD[1][s_], 8:16], [chf], [fS[1]])

            def head_pre(b, h, H, GS):
                sc, sl = SEQS[2 * b], SEQS[2 * b + 1]
                parts = [(sc, 0, 2), (sl, 2, 16)]
                wt, fS = GS["wt"], GS["fS"]
                qT, kT, ktok, vaug = H["qT"], H["kT"], H["ktok"], H["vaug"]
                for (s, o0, n_) in parts:
                    P.dma("sp", qT[0:dk, o0 * 128:(o0 + n_) * 128], s["QK"][QOFF + h * dk:QOFF + (h + 1) * dk, :], writes=[qT])
                    P.dma("sp", kT[0:dk, o0 * 128:(o0 + n_) * 128], s["QK"][KOFF + h * dk:KOFF + (h + 1) * dk, :], writes=[kT])
                    P.dma("sp", ktok[:, o0:o0 + n_, :], s["KT"].rearrange("(n p) f -> p n f", p=128)[:, :, h * dk:(h + 1) * dk], writes=[ktok])
                    P.dma("pool", vaug[:, o0:o0 + n_, 0:128], s["TM"].rearrange("(n p) c -> p n c", p=128)[:, :, VC0 + h * 128:VC0 + (h + 1) * 128], writes=[vaug])
                for d in range(2):
                    col = d * 8 + h
                    k.tt("pool", H["vp"][d][:], vaug[:], wt[:, :, col:col + 1].to_broadcast([128, NTT, 129]), ALU.mult, [vaug, wt], [H["vp"][d]])
                for t_ in range(NTT):
                    pa = ps_at.next()
                    k.mm(pa, pa[:, 0:128], kT[0:dk, t_ * 128:(t_ + 1) * 128], qT[0:dk, t_ * 128:(t_ + 1) * 128], True, True, [kT, qT])
                    a_ = ats.next()
                    k.copy("act", a_[:], pa[:, 0:128], [pa], [a_])
                    k.tt("dve", H["atm"][0][:, t_, :], a_[:], Mfwd[:], ALU.mult, [a_, Mfwd], [H["atm"][0]])
                    k.tt("pool", H["atm"][1][:, t_, :], a_[:], Mbwd[:], ALU.mult, [a_, Mbwd], [H["atm"][1]])
                for d in range(2):
                    vp = H["vp"][d]
                    for s0 in range(0, NS, 3):
                        ng = min(3, NS - s0)
                        pd = ps_dc.next()
                        for i_ in range(ng):
                            t_ = ORD[d][s0 + i_]
                            k.mm(pd, pd[0:dk, i_ * 129:(i_ + 1) * 129], ktok[:, t_, :], vp[:, t_, :], True, True, [ktok, vp])
                        k.evac(dCall[d][0:dk, :, s0:s0 + ng], pd[0:dk, 0:ng * 129].rearrange("p (s c) -> p c s", s=ng), [pd], [dCall[d]])
                    k.copy("pool", Fb[d][0:dk, :, :], fS[d][0:dk, h, 0:NS].unsqueeze(1).to_broadcast([dk, 129, NS]), [fS[d]], [Fb[d]])
                    dflat = dCall[d][0:dk].rearrange("p c s -> p (c s)")
                    fflat = Fb[d][0:dk].rearrange("p c s -> p (c s)")
                    P.op("dve", lambda e, dflat=dflat, fflat=fflat: e.tensor_tensor_scan(out=dflat, data0=fflat, data1=dflat, initial=0.0, op0=ALU.mult, op1=ALU.add),
                         reads=[Fb[d], dCall[d]], writes=[dCall[d]])
                    k.tt("dve", H["cbf"][d][0:dk].rearrange("p s c -> p c s"), dCall[d][0:dk],
                         fS[d][0:dk, h, 1:NS + 1].unsqueeze(1).to_broadcast([dk, 129, NS]), ALU.mult, [dCall[d], fS[d]], [H["cbf"][d]])

            def head_out(b, h, H, GS):
                sc, sl = SEQS[2 * b], SEQS[2 * b + 1]
                parts = [(sc, 0, 2), (sl, 2, 16)]
                rowf = GS["rowf"]
                qT = H["qT"]
                for st_ in range(NTT):
                    for d in range(2):
                        t_ = ORD[d][st_]
                        col = d * 8 + h
                        vp, atm, Yd, cbf = H["vp"][d], H["atm"][d], H["Y"], H["cbf"][d]
                        first = (d == 0) == (ORD[0].index(t_) <= ORD[1].index(t_))
                        po = ps_out.next()
                        k.mm(po, po[:, 0:129], atm[:, t_, :], vp[:, t_, :], True, st_ == 0, [atm, vp])
                        if st_ > 0:
                            k.mm(po, po[:, 0:129], qT[0:dk, t_ * 128:(t_ + 1) * 128], cbf[0:dk, st_ - 1, :], False, True, [qT, cbf])
                        if is_ml:
                            res = resr.next()
                            k.act(res[:], po[:, 0:129], AF.Copy, [po, rowf], [res], scale=rowf[:, t_, col:col + 1])
                            dd = ddr.next()
                            k.act(dd[:, 0:1], res[:, 128:129], AF.Abs, [res], [dd])
                            k.ts("pool", dd[:, 0:1], dd[:, 0:1], 1.0, ALU.max, [dd], [dd])
                            P.op("dve", lambda e, dd=dd: e.reciprocal(out=dd[:, 1:2], in_=dd[:, 0:1]), reads=[dd], writes=[dd])
                            if first:
                                k.act(Yd[:, t_, :], res[:, 0:128], AF.Copy, [res, dd], [Yd], scale=dd[:, 1:2])
                            else:
                                k.stt("dve", Yd[:, t_, :], res[:, 0:128], dd[:, 1:2], Yd[:, t_, :], ALU.mult, ALU.add, [res, dd, Yd], [Yd])
                        else:
                            if first:
                                k.act(Yd[:, t_, :], po[:, 0:128], AF.Copy, [po, rowf], [Yd], scale=rowf[:, t_, col:col + 1])
                            else:
                                k.stt("dve", Yd[:, t_, :], po[:, 0:128], rowf[:, t_, col:col + 1], Yd[:, t_, :], ALU.mult, ALU.add, [po, rowf, Yd], [Yd])
                for (s, o0, n_) in parts:
                    if s["ctx"] and last:
                        continue
                    P.dma("sp", s["Y"].rearrange("(n p) f -> p n f", p=128)[:, :, h * 128:(h + 1) * 128], H["Y"][:, o0:o0 + n_, :], reads=[H["Y"]])

            heads = [(b, h) for b in range(BPC) for h in range(8)]
            for i_h in range(len(heads) + 1):
                if i_h < len(heads):
                    b, h = heads[i_h]
                    if h == 0:
                        gate_prep(b, gsets[b % 2])
                    head_pre(b, h, hdr[i_h % 2], gsets[b % 2])
                if i_h >= 1:
                    b, h = heads[i_h - 1]
                    head_out(b, h, hdr[(i_h - 1) % 2], gsets[b % 2])
            P.barrier()
            PA.reset()

            Wout = PA.alloc("Wout", [8, D], BF16)
            P.dma("pool", Wout[:], w_out.rearrange("(kc p) n -> p kc n", p=128), writes=[Wout])
            rtr = PA.alloc("rtr", [8, NE], F32)
            P.dma("sp", rtr[:], moe_router[l].rearrange("(kc p) e -> p kc e", p=128), writes=[rtr])
            hng = load_bc("hng", hn_g)
            G2 = [load_bc(f"G2_{kd}", MODS[l, 2, kd]) for kd in range(3)]
            A2 = [load_bc(f"A2_{kd}", MODS[l, 3, kd]) for kd in range(3)]
            B2 = [load_bc(f"B2_{kd}", MODS[l, 4, kd]) for kd in range(3)]
            yr = Ring([PA.alloc(f"yt{i}", [8, 128], F32) for i in range(3)])
            orr = Ring([PA.alloc(f"ot{i}", [D], F32) for i in range(3)])
            xr = Ring([PA.alloc(f"xt{i}", [D], F32) for i in range(3)])
            sqr = Ring([PA.alloc(f"sq{i}", [8, 128], F32) for i in range(2)])
            st8 = Ring([PA.alloc(f"st8{i}", [4, 8], F32) for i in range(2)])
            ybr = Ring([PA.alloc(f"yb{i}", [D], BF16) for i in range(2)])
            ybT = Ring([PA.alloc(f"ybT{i}", [8, 128], BF16) for i in range(2)])
            xnr = Ring([PA.alloc(f"xn{i}", [D], F32) for i in range(2)])
            tmpr = Ring([PA.alloc(f"tmp{i}", [D], F32) for i in range(2)])
            h2r = Ring([PA.alloc(f"h2{i}", [D], F32) for i in range(2)])
            h2br = Ring([PA.alloc(f"h2b{i}", [D], BF16) for i in range(2)])
            h2Tr = Ring([PA.alloc(f"h2T{i}", [8, 128], F32) for i in range(2)])
            junk = PA.alloc("junk", [D], F32)
            ssr = Ring([PA.alloc(f"ss{i}", [4], F32) for i in range(3)])
            smr = Ring([PA.alloc(f"sm{i}", [4], F32) for i in range(2)])
            affr = Ring([PA.alloc(f"aff{i}", [NE], F32) for i in range(2)])
            aftr = Ring([PA.alloc(f"aft{i}", [128], F32, parts=NE) for i in range(2)])
            ps_tr = Ring(PS[0:2])
            ps_o = Ring(PS[2:6])
            ps_t32 = Ring(PS[6:8])
            for s in SEQS:
                if s["ctx"] and last:
                    continue
                T, kd, nt = s["T"], s["kind"], s["nt"]
                for ti in range(nt):
                    t0 = ti * 128
                    yt = yr.next()
                    P.dma("sp", yt[:], s["Y"][t0:t0 + 128, :].rearrange("p (h f) -> p h f", h=8), writes=[yt])
                    ot = orr.next()
                    P.dma("sp", ot[:], s["TM"][t0:t0 + 128, OC0:OC0 + 1024], writes=[ot])
                    xt = xr.next()
                    P.dma("sp", xt[:], xsrc(s)[t0:t0 + 128, :], writes=[xt])
                    sq = sqr.next()
                    s8 = st8.next()
                    if not is_ml:
                        k.P.op("dve", lambda e, s8=s8, yt=yt: e.tensor_reduce(out=s8[:, 0, :], in_=yt[:], axis=AX.X, op=ALU.add), reads=[yt], writes=[s8])
                        k.ts("dve", s8[:, 0, :], s8[:, 0, :], -1.0 / 128.0, ALU.mult, [s8], [s8])
                        k.tt("dve", yt[:], yt[:], s8[:, 0, :].unsqueeze(2).to_broadcast([128, 8, 128]), ALU.add, [yt, s8], [yt])
                    k.tt("dve", sq[:], yt[:], yt[:], ALU.mult, [yt], [sq])
                    k.P.op("dve", lambda e, s8=s8, sq=sq: e.tensor_reduce(out=s8[:, 1, :], in_=sq[:], axis=AX.X, op=ALU.add), reads=[sq], writes=[s8])
                    k.act(s8[:, 2, :], s8[:, 1, :], AF.Ln, [s8], [s8], scale=1.0 / 128.0, bias=EPS)
                    k.act(s8[:, 3, :], s8[:, 2, :], AF.Exp, [s8], [s8], scale=-0.5)
                    k.tt("dve", sq[:], yt[:], s8[:, 3, :].unsqueeze(2).to_broadcast([128, 8, 128]), ALU.mult, [yt, s8], [sq])
                    k.act(ot[:], ot[:], AF.Sigmoid if is_ml else AF.Silu, [ot], [ot])
                    k.tt("dve", sq[:].rearrange("p a b -> p (a b)"), sq[:].rearrange("p a b -> p (a b)"), hng[:], ALU.mult, [sq, hng], [sq])
                    yb = ybr.next()
                    k.tt("dve", yb[:], sq[:].rearrange("p a b -> p (a b)"), ot[:], ALU.mult, [sq, ot], [yb])
                    pt = ps_tr.next()
                    for kc in range(8):
                        k.tr(pt, ps_bf(pt)[:, kc * 128:(kc + 1) * 128], yb[:, kc * 128:(kc + 1) * 128], identbf[:], [yb, identbf])
                    yT = ybT.next()
                    k.evac(yT[:], ps_bf(pt).rearrange("p (a b) -> p a b", a=8), [pt], [yT])
                    xn = xnr.next()
                    tmp = tmpr.next()
                    for hf in range(2):
                        po = ps_o.next()
                        for kc in range(8):
                            k.mm(po, po[:], yT[:, kc, :], Wout[:, kc, hf * 512:(hf + 1) * 512], kc == 0, kc == 7, [yT, Wout])
                        k.tt("dve", tmp[:, hf * 512:(hf + 1) * 512], po[:], G2[kd][:, hf * 512:(hf + 1) * 512], ALU.mult, [po, G2[kd]], [tmp])
                    k.tt("dve", xn[:], tmp[:], xt[:], ALU.add, [tmp, xt], [xn])
                    P.dma("pool", s["XS"][t0:t0 + 128, :], xn[:], reads=[xn])
                    ss = ssr.next()
                    k.act(junk[:], xn[:], AF.Square, [xn], [junk, ss], accum=ss[:, 0:1])
                    k.act(ss[:, 1:2], ss[:, 0:1], AF.Ln, [ss], [ss], scale=1.0 / D, bias=EPS)
                    k.act(ss[:, 1:2], ss[:, 1:2], AF.Exp, [ss], [ss], scale=-0.5)
                    tmp2 = tmpr.next()
                    k.stt("dve", tmp2[:], xn[:], ss[:, 1:2], A2[kd][:], ALU.mult, ALU.mult, [xn, ss, A2[kd]], [tmp2])
                    h2 = h2r.next()
                    k.tt("dve", h2[:], tmp2[:], B2[kd][:], ALU.add, [tmp2, B2[kd]], [h2])
                    h2b = h2br.next()
                    k.copy("act", h2b[:], h2[:], [h2], [h2b])
                    P.dma("pool", s["H2"][t0:t0 + 128, :], h2b[:], reads=[h2b])
                    h2T = h2Tr.next()
                    for half in range(2):
                        p32 = ps_t32.next()
                        for kc in range(4):
                            kk = half * 4 + kc
                            k.tr(p32, p32[:, kc * 128:(kc + 1) * 128], h2[:, kk * 128:(kk + 1) * 128], ident32[:], [h2, ident32])
                        k.evac(h2T[:, half * 4:(half + 1) * 4, :], p32[:].rearrange("p (a b) -> p a b", a=4), [p32], [h2T])
                    pl = ps_o.next()
                    for kc in range(8):
                        k.mm(pl, pl[:, 0:NE], h2T[:, kc, :], rtr[:, kc, :], kc == 0, kc == 7, [h2T, rtr])
                    sm = smr.next()
                    k.P.op("dve", lambda e, sm=sm, pl=pl: e.tensor_reduce(out=sm[:, 0:1], in_=pl[:, 0:NE], axis=AX.X, op=ALU.max), reads=[pl], writes=[sm])
                    k.ts("dve", sm[:, 1:2], sm[:, 0:1], -1.0, ALU.mult, [sm], [sm])
                    aff = affr.next()
                    k.act(aff[:], pl[:, 0:NE], AF.Exp, [pl, sm], [aff, sm], bias=sm[:, 1:2], accum=sm[:, 2:3])
                    P.op("dve", lambda e, sm=sm: e.reciprocal(out=sm[:, 3:4], in_=sm[:, 2:3]), reads=[sm], writes=[sm])
                    k.ts("dve", aff[:], aff[:], sm[:, 3:4], ALU.mult, [aff, sm], [aff])
                    pa_ = ps_t32.next()
                    k.tr(pa_, pa_[0:NE, 0:128], aff[:], ident32[:], [aff, ident32])
                    aft = aftr.next()
                    k.evac(aft[:], pa_[0:NE, 0:128], [pa_], [aft])
                    P.dma("pool", s["AFFT"][:, t0:t0 + 128], aft[:], reads=[aft])
            P.barrier()
            PA.reset()
            if dbg and l == 0:
                for s in SEQS:
                    tp = dbg_tap(f"dbg_xmix_{s['name']}", [s["T"], D])
                    P.dma("sp", tp, s["XS"], )
                    tp = dbg_tap(f"dbg_afft_{s['name']}", [NE, s["T"]])
                    P.dma("sp", tp, s["AFFT"])
                    tp = dbg_tap(f"dbg_y_{s['name']}", [s["T"], D])
                    P.dma("sp", tp, s["Y"])
                    tp = dbg_tap(f"dbg_tm_{s['name']}", [s["T"], 2080])
                    P.dma("sp", tp, s["TM"])
                    tp = dbg_tap(f"dbg_fm_{s['name']}", [2048, s["T"]])
                    P.dma("sp", tp, s["FM"])
                P.barrier()

            act_seqs = [s for s in SEQS if not (s["ctx"] and last)]
            groups = [[s for s in act_seqs if not s["ctx"]]]
            if not last:
                groups.append([s for s in act_seqs if s["ctx"]])
            for grp in groups:
                T, nt = grp[0]["T"], grp[0]["nt"]
                PMtokG = PA.alloc(f"PMtokG{T}", [nt, 32], F32)
                PGg = PA.alloc(f"PGg{T}", [2, T], BF16, parts=32)
                for gi, s in enumerate(grp):
                    s["gi"], s["PMtokG"], s["PGg"] = gi, PMtokG, PGg
            mark_E = PA.off
            aff_F = PA.alloc("aff_", [SEQ], F32, parts=32)
            work_F = PA.alloc("work", [SEQ], F32, parts=32)
            onesr_F = PA.alloc("onesr", [SEQ], F32, parts=32)
            cum_F = PA.alloc("cum_", [SEQ], F32, parts=32)
            m8 = PA.alloc("m8", [8], F32, parts=32)
            k.memset("pool", onesr_F[:], 1.0, [onesr_F])
            for grp in groups:
                T, nt, cap = grp[0]["T"], grp[0]["nt"], grp[0]["cap"]
                aff_ = Tile(aff_F[:, 0:T], "aff_v")
                work = Tile(work_F[:, 0:T], "work_v")
                onesr = Tile(onesr_F[:, 0:T], "ones_v")
                cum_ = Tile(cum_F[:, 0:T], "cum_v")
                P.barrier()
                for gi, s in enumerate(grp):
                    P.dma("sp", aff_[gi * NE:(gi + 1) * NE, :], s["AFFT"], writes=[aff_])
                k.copy("dve", work[:], aff_[:], [aff_], [work])
                for it in range(cap // 8):
                    P.op("dve", lambda e, m8=m8, work=work: e.max(out=m8[:], in_=work[:]), reads=[work], writes=[m8])
                    P.op("dve", lambda e, m8=m8, work=work: e.match_replace(out=work[:], in_to_replace=m8[:], in_values=work[:], imm_value=0.0), reads=[m8, work], writes=[work])
                k.tt("dve", work[:], aff_[:], work[:], ALU.subtract, [aff_, work], [work])
                k.ts("dve", aff_[:], work[:], 0.0, ALU.is_gt, [work], [aff_])
                P.op("dve", lambda e, cum_=cum_, onesr=onesr, aff_=aff_: e.tensor_tensor_scan(out=cum_[:], data0=onesr[:], data1=aff_[:], initial=0.0, op0=ALU.mult, op1=ALU.add), reads=[onesr, aff_], writes=[cum_])
                k.tt("dve", cum_[:], cum_[:], aff_[:], ALU.mult, [cum_, aff_], [cum_])
                k.ts("dve", cum_[:], cum_[:], -1.0, ALU.add, [cum_], [cum_])
                PGg = grp[0]["PGg"]
                k.copy("dve", PGg[:, 0, :], cum_[:], [cum_], [PGg])
                k.copy("dve", PGg[:, 1, :], work[:], [work], [PGg])
                pt = PS[0]
                for ti in range(nt):
                    k.tr(pt, pt[:, ti * 32:(ti + 1) * 32], cum_[:, ti * 128:(ti + 1) * 128], ident32[0:32, 0:32], [cum_, ident32])
                k.copy("dve", grp[0]["PMtokG"][:].rearrange("p a b -> p (a b)"), pt[:, 0:nt * 32], [pt], [grp[0]["PMtokG"]])
            P.barrier()
            PA.reset(mark_E)

            NRING = 16
            wring = Ring([PA.alloc(f"wr{i}", [8, 256], BF16) for i in range(NRING)])
            h2ring = Ring([PA.alloc(f"h2t{i}", [D], BF16) for i in range(6)])
            selr = Ring([PA.alloc(f"sel{i}", [256], BF16) for i in range(6)])
            for grp in groups:
                ncap = len(grp) * grp[0]["cap"]
                xs_g = PA.alloc(f"xsT_g{ncap}", [8, ncap], BF16)
                hid_g = PA.alloc(f"hidT_g{ncap}", [8, ncap], BF16)
                for s in grp:
                    s["xsT"], s["hidT"], s["ncap"] = xs_g, hid_g, ncap
            sgr = Ring([PA.alloc(f"sg{i}", [512], F32) for i in range(2)])
            ysr = Ring([PA.alloc(f"ys{i}", [2, D], BF16) for i in range(2)])
            pieces = []
            for e_ in range(NE):
                for fb in range(4):
                    pieces.append((e_, "g", fb))
                    pieces.append((e_, "u", fb))
                for i_ in range(4):
                    pieces.append((e_, "d", i_))
            loaded = {}
            nload = [0]

            def ensure(n_upto):
                while nload[0] < min(n_upto, len(pieces)):
                    e_, kind_, i_ = pieces[nload[0]]
                    wt_ = wring.next()
                    if kind_ == "d":
                        src = moe_wd[l, e_].rearrange("(fc p) d -> p fc d", p=128)[:, 2 * i_:2 * i_ + 2, :]
                        dst = wt_[:].rearrange("p a b -> p (a b)").rearrange("p (f d) -> p f d", f=2)
                    else:
                        wsrc = moe_wg if kind_ == "g" else moe_wu
                        src = wsrc[l, e_].rearrange("(kc p) n -> p kc n", p=128)[:, :, i_ * 256:(i_ + 1) * 256]
                        dst = wt_[:]
                    P.dma("pool", dst, src, writes=[wt_])
                    loaded[pieces[nload[0]]] = wt_
                    nload[0] += 1

            ps_ga = PS[0:4]
            ps_up = Ring(PS[4:8])
            ps_dn = Ring(PS[0:4])
            pidx = 0
            ensure(14)
            for e_ in range(NE):
                for s in act_seqs:
                    nt, cap = s["nt"], s["cap"]
                    for ti in range(nt):
                        h2t = h2ring.next()
                        P.dma("sp", h2t[:], s["H2"][ti * 128:(ti + 1) * 128, :], writes=[h2t])
                        sel = selr.next()
                        k.ts("dve", sel[:, 0:cap], iota_f[:, 0:cap], s["PMtokG"][:, ti, s["gi"] * NE + e_:s["gi"] * NE + e_ + 1], ALU.is_equal, [iota_f, s["PMtokG"]], [sel])
                        for dc in range(8):
                            pg = ps_ga[dc // 2]
                            k.mm(pg, pg[:, (dc % 2) * 256:(dc % 2) * 256 + cap], h2t[:, dc * 128:(dc + 1) * 128], sel[:, 0:cap], ti == 0 and dc % 2 == 0, ti == nt - 1 and dc % 2 == 1, [h2t, sel])
                    for dc in range(8):
                        pg = ps_ga[dc // 2]
                        k.evac(s["xsT"][:, dc, s["gi"] * cap:(s["gi"] + 1) * cap], pg[:, (dc % 2) * 256:(dc % 2) * 256 + cap], [pg], [s["xsT"]])
                for fb in range(4):
                    ensure(pidx + 2 + 12)
                    wg_, wu_ = loaded[(e_, "g", fb)], loaded[(e_, "u", fb)]
                    pidx += 2
                    for grp in groups:
                        s = grp[0]
                        ncap = s["ncap"]
                        for f2 in range(2):
                            pg_ = ps_up.next()
                            for kc in range(8):
                                k.mm(pg_, pg_[:, 0:ncap], wg_[:, kc, f2 * 128:(f2 + 1) * 128], s["xsT"][:, kc, :], kc == 0, kc == 7, [wg_, s["xsT"]])
                            pu = ps_up.next()
                            for kc in range(8):
                                k.mm(pu, pu[:, 0:ncap], wu_[:, kc, f2 * 128:(f2 + 1) * 128], s["xsT"][:, kc, :], kc == 0, kc == 7, [wu_, s["xsT"]])
                            sg = sgr.next()
                            k.act(sg[:, 0:ncap], pg_[:, 0:ncap], AF.Silu, [pg_], [sg])
                            k.tt("dve", s["hidT"][:, fb * 2 + f2, :], sg[:, 0:ncap], pu[:, 0:ncap], ALU.mult, [sg, pu], [s["hidT"]])
                ensure(pidx + 4 + 12)
                wd_ = [loaded[(e_, "d", i_)] for i_ in range(4)]
                pidx += 4
                for s in act_seqs:
                    cap = s["cap"]
                    M = min(128, cap)
                    ncc = cap // M
                    ys = ysr.next()
                    for cc in range(ncc):
                        for hf in range(2):
                            pd = ps_dn.next()
                            for fc in range(8):
                                wv = wd_[fc // 2][:].rearrange("p a b -> p (a b)").rearrange("p (f d) -> p f d", f=2)
                                k.mm(pd, pd[0:M, :], s["hidT"][:, fc, s["gi"] * cap + cc * M:s["gi"] * cap + (cc + 1) * M], wv[:, fc % 2, hf * 512:(hf + 1) * 512], fc == 0, fc == 7, [s["hidT"], wd_[fc // 2]])
                            k.evac(ys[0:M, cc, hf * 512:(hf + 1) * 512], pd[0:M, :], [pd], [ys])
                    P.dma("sp", s["YE"][e_].rearrange("(cc p) d -> p cc d", p=M), ys[0:M, 0:ncc, :], reads=[ys])
            P.barrier()
            PA.reset(mark_E)

            G5 = [load_bc(f"G5_{kd}", MODS[l, 5, kd]) for kd in range(3)]
            if last:
                fng = load_bc("fng", final_norm_g)
            yer = [PA.alloc(f"yer{e_}", [2, D], BF16) for e_ in range(NE)]
            xr = Ring([PA.alloc(f"xt{i}", [D], F32) for i in range(2)])
            gmr = Ring([PA.alloc(f"gm{i}", [512], BF16) for i in range(3)])
            selT = [[PA.alloc(f"selT{e_}_{cc}", [512], BF16) for cc in range(2)] for e_ in range(NE)]
            tmpr = Ring([PA.alloc(f"tmp{i}", [D], F32) for i in range(2)])
            xnr = Ring([PA.alloc(f"xn{i}", [D], F32) for i in range(2)])
            junk = PA.alloc("junk", [D], F32)
            ssr = Ring([PA.alloc(f"ss{i}", [2], F32) for i in range(3)])
            ps_bc = Ring(PS[0:4])
            ps_oo = Ring(PS[4:8])
            for s in act_seqs:
                T, nt, cap, kd = s["T"], s["nt"], s["cap"], s["kind"]
                M = min(128, cap)
                ncc = cap // M
                NBt = min(512, T)
                for e_ in range(NE):
                    P.dma("sp", yer[e_][0:M, 0:ncc, :], s["YE"][e_].rearrange("(cc p) d -> p cc d", p=M), writes=[yer[e_]])
                for blk in range(T // NBt):
                    c0 = blk * NBt
                    for e_ in range(NE):
                        pbm = ps_bc.next()
                        k.mm(pbm, pbm[:, 0:NBt], oh32[:, s["gi"] * NE + e_, :], s["PGg"][:, 0, c0:c0 + NBt], True, True, [oh32, s["PGg"]])
                        pbg = ps_bc.next()
                        k.mm(pbg, pbg[:, 0:NBt], oh32[:, s["gi"] * NE + e_, :], s["PGg"][:, 1, c0:c0 + NBt], True, True, [oh32, s["PGg"]])
                        gm = gmr.next()
                        k.copy("act", gm[:, 0:NBt], pbg[:, 0:NBt], [pbg], [gm])
                        for cc in range(ncc):
                            st_ = selT[e_][cc]
                            k.stt("dve", st_[0:M, 0:NBt], pbm[0:M, 0:NBt], (iota_p if cc == 0 else iota_p128)[0:M, 0:1], gm[0:M, 0:NBt], ALU.is_equal, ALU.mult, [pbm, gm, iota_p, iota_p128], [st_])
                    for ti in range(NBt // 128):
                        t0 = c0 + ti * 128
                        xt = xr.next()
                        P.dma("sp", xt[:], s["XS"][t0:t0 + 128, :], writes=[xt])
                        po = [ps_oo.next(), ps_oo.next()]
                        for hf in range(2):
                            for e_ in range(NE):
                                for cc in range(ncc):
                                    k.mm(po[hf], po[hf][:], selT[e_][cc][0:M, ti * 128:(ti + 1) * 128], yer[e_][0:M, cc, hf * 512:(hf + 1) * 512], e_ == 0 and cc == 0, e_ == NE - 1 and cc == ncc - 1, [selT[e_][cc], yer[e_]])
                        tmp = tmpr.next()
                        for hf in range(2):
                            k.tt("dve", tmp[:, hf * 512:(hf + 1) * 512], po[hf][:], G5[kd][:, hf * 512:(hf + 1) * 512], ALU.mult, [po[hf], G5[kd]], [tmp])
                        xn = xnr.next()
                        k.tt("dve", xn[:], tmp[:], xt[:], ALU.add, [tmp, xt], [xn])
                        if not last:
                            P.dma("pool", s["XS"][t0:t0 + 128, :], xn[:], reads=[xn])
                        else:
                            ss = ssr.next()
                            k.act(junk[:], xn[:], AF.Square, [xn], [junk, ss], accum=ss[:, 0:1])
                            k.act(ss[:, 1:2], ss[:, 0:1], AF.Ln, [ss], [ss], scale=1.0 / D, bias=EPS)
                            k.act(ss[:, 1:2], ss[:, 1:2], AF.Exp, [ss], [ss], scale=-0.5)
                            tmp2 = tmpr.next()
                            k.stt("dve", tmp2[:], xn[:], ss[:, 1:2], fng[:], ALU.mult, ALU.mult, [xn, ss, fng], [tmp2])
                            P.dma("pool", out_ap[s["b"], t0:t0 + 128, :], tmp2[:], reads=[tmp2])
            P.barrier()
            PA.reset()
            if dbg:
                for s in SEQS:
                    if s["ctx"] and last:
                        continue
                    tp = dbg_tap(f"dbg_x{l}_{s['name']}", [s["T"], D])
                    P.dma("sp", tp, s["XS"])
                P.barrier()
            if dbg and dbg == "l0":
                break
        P.emit()
    return nc, list(dbg_outs.keys()), len(P.ops)


_CACHE = {}


def kernel(**inputs):
    if "nc" not in _CACHE:
        _CACHE["nc"] = build(False)
    nc, _, nops = _CACHE["nc"]
    f32 = lambda a: np.ascontiguousarray(np.asarray(a, dtype=np.float32))
    shared = {}
    for name in ("c_ctx", "ada_w", "ada_b", "norm_mix_g", "norm_ffn_g", "final_norm_g", "mlstm_w_in", "mlstm_conv_w",
                 "mlstm_conv_b", "mlstm_head_norm_g", "mlstm_w_out", "ret_w_in", "ret_group_norm_g", "ret_w_out",
                 "moe_router", "moe_w_gate", "moe_w_up", "moe_w_down"):
        shared[name] = f32(inputs[name])
    for name in ("mlstm_igate_b", "mlstm_fgate_b", "ret_decay_logit"):
        shared[name] = f32(inputs[name]).reshape(2, 16)
    x = f32(inputs["x"])
    c = f32(inputs["c"])
    ctx = f32(inputs["ctx"])
    in_maps = []
    for i in range(NCORES):
        m = dict(shared)
        m["x"] = x[i * BPC:(i + 1) * BPC]
        m["c"] = c[i * BPC:(i + 1) * BPC]
        m["ctx"] = ctx[i * BPC:(i + 1) * BPC]
        in_maps.append(m)
    res = run_bass_kernel_spmd(nc, in_maps, core_ids=list(range(NCORES)))
    out = np.concatenate([np.asarray(r["out"], dtype=np.float32) for r in res.results], axis=0)
    return out
```

```python
import numpy as np
from contextlib import ExitStack
import concourse.bass as bass
import concourse.mybir as mybir
from concourse.bass_utils import run_bass_kernel_spmd

F32 = mybir.dt.float32
BF16 = mybir.dt.bfloat16
I32 = mybir.dt.int32
ALU = mybir.AluOpType
AF = mybir.ActivationFunctionType
AX = mybir.AxisListType

ENGS = ("sp", "pe", "act", "dve", "pool")
NDMASEM = 24

D = 1024
SEQ = 2048
CTX = 256
DEPTH = 4
NE = 16
EPS = 1e-6
NCORES = 8
BPC = 2


class Tile:
    __slots__ = ("ap", "name", "last_w", "readers")

    def __init__(self, ap, name):
        self.ap = ap
        self.name = name
        self.last_w = None
        self.readers = []

    def __getitem__(self, k):
        return self.ap[k]


class Op:
    __slots__ = ("idx", "eng", "fn", "deps", "signal", "is_dma", "semi", "semval", "pos", "kind", "nconsumers")

    def __init__(self, idx, eng, fn, is_dma=False, kind="op"):
        self.idx = idx
        self.eng = eng
        self.fn = fn
        self.deps = []
        self.signal = False
        self.is_dma = is_dma
        self.semi = None
        self.semval = None
        self.pos = None
        self.kind = kind
        self.nconsumers = 0


class Prog:
    def __init__(self, nc):
        self.nc = nc
        self.ops = []
        self.streams = {e: [] for e in ENGS}
        self.waited = {e: {f: -1 for f in ENGS} for e in ENGS}
        self.waited_dma = {e: {} for e in ENGS}
        self.dma_rr = 0
        self.dma_last = [None] * NDMASEM
        self.dma_count = [0] * NDMASEM
        self.unconsumed_dma = []
        self.last_op = {e: None for e in ENGS}
        self.nbar = 0

    def _add_dep(self, op, prod):
        if prod is None or prod is op:
            return
        e = op.eng
        if prod.is_dma:
            w = self.waited_dma[e].get(prod.semi, 0)
            if w >= prod.semval:
                return
            self.waited_dma[e][prod.semi] = prod.semval
            op.deps.append(prod)
            prod.nconsumers += 1
        else:
            f = prod.eng
            if e == "pe" and f == "pe":
                return
            if self.waited[e][f] >= prod.pos:
                return
            self.waited[e][f] = prod.pos
            prod.signal = True
            op.deps.append(prod)

    def _track(self, op, reads, writes):
        for t in reads:
            self._add_dep(op, t.last_w)
        for t in writes:
            self._add_dep(op, t.last_w)
            for r in t.readers:
                self._add_dep(op, r)
        for t in reads:
            t.readers.append(op)
        for t in writes:
            t.last_w = op
            t.readers = []

    def _push(self, op):
        op.pos = len(self.streams[op.eng])
        self.streams[op.eng].append(op)
        self.ops.append(op)
        if not op.is_dma:
            self.last_op[op.eng] = op

    def op(self, eng, fn, reads=(), writes=()):
        o = Op(len(self.ops), eng, fn)
        self._push(o)
        self._track(o, reads, writes)
        return o

    def dma(self, queue, out_ap, in_ap, reads=(), writes=(), **kw):
        o = Op(len(self.ops), queue, None, is_dma=True)
        k = self.dma_rr
        self.dma_rr = (self.dma_rr + 1) % NDMASEM
        o.semi = k
        self.dma_count[k] += 1
        o.semval = 16 * self.dma_count[k]
        o.fn = lambda eng: eng.dma_start(out=out_ap, in_=in_ap, **kw)
        self._push(o)
        prev = self.dma_last[k]
        if prev is not None:
            self._add_dep(o, prev)
        self.dma_last[k] = o
        self._track(o, reads, writes)
        self.unconsumed_dma.append(o)
        return o

    def barrier(self):
        self.nbar += 1
        n = self.nbar
        b = Op(len(self.ops), "sp", None, kind="bar_inc")
        b.semval = n
        self._push(b)
        for e in ENGS:
            if e != "sp":
                self._add_dep(b, self.last_op[e])
        for d in self.unconsumed_dma:
            self._add_dep(b, d)
        self.unconsumed_dma = []
        for e in ENGS:
            if e == "sp":
                continue
            w = Op(len(self.ops), e, None, kind="bar_wait")
            w.semval = n
            self._push(w)

    def emit(self):
        nc = self.nc
        with ExitStack() as es:
            esem = {e: es.enter_context(nc.semaphore(f"es_{e}")) for e in ENGS}
            dsem = [es.enter_context(nc.semaphore(f"ds_{i}")) for i in range(NDMASEM)]
            bsem = es.enter_context(nc.semaphore("barsem"))
            for e in ENGS:
                c = 0
                for o in self.streams[e]:
                    if o.is_dma or o.kind != "op":
                        continue
                    if o.signal:
                        c += 1
                        o.semval = c
                        o.semi = e
            block = es.enter_context(nc.Block())

            def run(e, eng):
                for o in self.streams[e]:
                    for p in o.deps:
                        if p.is_dma:
                            eng.wait_ge(dsem[p.semi], p.semval)
                        else:
                            eng.wait_ge(esem[p.eng], p.semval)
                    if o.kind == "bar_inc":
                        eng.sem_inc(bsem, 1)
                        continue
                    if o.kind == "bar_wait":
                        eng.wait_ge(bsem, o.semval)
                        continue
                    ins = o.fn(eng)
                    if o.is_dma:
                        ins.then_inc(dsem[o.semi], 16)
                    elif o.signal:
                        ins.then_inc(esem[e], 1)

            @block.sync
            def _(eng):
                run("sp", eng)

            @block.tensor
            def _(eng):
                run("pe", eng)

            @block.scalar
            def _(eng):
                run("act", eng)

            @block.vector
            def _(eng):
                run("dve", eng)

            @block.gpsimd
            def _(eng):
                run("pool", eng)


class Arena:
    def __init__(self, t, ncols):
        self.t = t
        self.n = ncols
        self.off = 0

    def alloc(self, name, free, dt, parts=128):
        free = list(free)
        n = int(np.prod(free))
        cols = n * (2 if dt in (F32, I32) else 1)
        cols = (cols + 1) // 2 * 2
        assert self.off + cols <= self.n, f"arena overflow {name} {self.off}+{cols}>{self.n}"
        ap = self.t[0:parts, self.off:self.off + cols]
        self.off += cols
        if dt != BF16:
            ap = ap.bitcast(dt)
        if n * (2 if dt in (F32, I32) else 1) != cols:
            ap = ap[:, 0:n]
        if len(free) == 2:
            ap = ap.rearrange("p (a b) -> p a b", a=free[0])
        elif len(free) == 3:
            ap = ap.rearrange("p (a b c) -> p a b c", a=free[0], b=free[1])
        return Tile(ap, name)

    def reset(self, to=0):
        self.off = to


class Ring:
    def __init__(self, tiles):
        self.tiles = tiles
        self.i = 0

    def next(self):
        t = self.tiles[self.i % len(self.tiles)]
        self.i += 1
        return t


class K:
    def __init__(self, P):
        self.P = P
        self.flip = 0

    def mm(self, ps, out, lhsT, rhs, start, stop, rd):
        self.P.op("pe", lambda e: e.matmul(out, lhsT=lhsT, rhs=rhs, start=start, stop=stop), reads=rd, writes=[ps])

    def tr(self, ps, out, in_, ident, rd):
        self.P.op("pe", lambda e: e.transpose(out, in_, ident), reads=rd, writes=[ps])

    def act(self, out, in_, func, rd, wr, bias=None, scale=None, accum=None):
        kw = {}
        if bias is not None:
            kw["bias"] = bias
        if scale is not None:
            kw["scale"] = scale
        if accum is not None:
            kw["accum_out"] = accum
        self.P.op("act", lambda e: e.activation(out=out, in_=in_, func=func, **kw), reads=rd, writes=wr)

    def tt(self, eng, out, in0, in1, op, rd, wr):
        self.P.op(eng, lambda e: e.tensor_tensor(out=out, in0=in0, in1=in1, op=op), reads=rd, writes=wr)

    def ts(self, eng, out, in0, s1, op0, rd, wr, s2=None, op1=None, accum=None):
        kw = {}
        if op1 is not None:
            kw["op1"] = op1
        if accum is not None:
            kw["accum_out"] = accum
        self.P.op(eng, lambda e: e.tensor_scalar(out=out, in0=in0, scalar1=s1, scalar2=s2, op0=op0, **kw), reads=rd, writes=wr)

    def stt(self, eng, out, in0, scalar, in1, op0, op1, rd, wr):
        self.P.op("dve", lambda e: e.scalar_tensor_tensor(out=out, in0=in0, scalar=scalar, in1=in1, op0=op0, op1=op1), reads=rd, writes=wr)

    def copy(self, eng, out, in_, rd, wr):
        if eng == "act":
            self.P.op("act", lambda e: e.copy(out=out, in_=in_), reads=rd, writes=wr)
        else:
            self.P.op(eng, lambda e: e.tensor_copy(out=out, in_=in_), reads=rd, writes=wr)

    def evac(self, out, in_, rd, wr):
        self.flip ^= 1
        self.copy("act" if self.flip else "dve", out, in_, rd, wr)

    def memset(self, eng, ap, val, wr):
        self.P.op(eng, lambda e: e.memset(ap, val), writes=wr)


def build(dbg=False):
    nc = bass.Bass("TRN2", target_bir_lowering=False)

    def din(name, shape):
        return nc.dram_tensor(name, list(shape), F32, kind="ExternalInput").ap()

    x_in = din("x", [BPC, SEQ, D])
    c_in = din("c", [BPC, D])
    ctx_in = din("ctx", [BPC, CTX, D])
    cctx_in = din("c_ctx", [D])
    ada_w = din("ada_w", [DEPTH, D, 6 * D])
    ada_b = din("ada_b", [DEPTH, 6 * D])
    norm_mix_g = din("norm_mix_g", [DEPTH, D])
    norm_ffn_g = din("norm_ffn_g", [DEPTH, D])
    final_norm_g = din("final_norm_g", [D])
    ml_w_in = din("mlstm_w_in", [2, D, 3104])
    ml_conv_w = din("mlstm_conv_w", [2, 5, 1024])
    ml_conv_b = din("mlstm_conv_b", [2, 1024])
    ml_ig_b = din("mlstm_igate_b", [2, 16])
    ml_fg_b = din("mlstm_fgate_b", [2, 16])
    ml_hn_g = din("mlstm_head_norm_g", [2, 1024])
    ml_w_out = din("mlstm_w_out", [2, 1024, D])
    rt_w_in = din("ret_w_in", [2, D, 4096])
    rt_decay = din("ret_decay_logit", [2, 16])
    rt_gn_g = din("ret_group_norm_g", [2, 1024])
    rt_w_out = din("ret_w_out", [2, 1024, D])
    moe_router = din("moe_router", [DEPTH, D, NE])
    moe_wg = din("moe_w_gate", [DEPTH, NE, D, D])
    moe_wu = din("moe_w_up", [DEPTH, NE, D, D])
    moe_wd = din("moe_w_down", [DEPTH, NE, D, D])
    out_ap = nc.dram_tensor("out", [BPC, SEQ, D], F32, kind="ExternalOutput").ap()

    def scr(name, shape, dt=F32):
        return nc.dram_tensor(name, list(shape), dt, kind="Internal").ap()

    SEQS = []
    for b in range(BPC):
        SEQS.append(dict(name=f"c{b}", b=b, ctx=True, T=CTX, kind=2, xin=ctx_in[b]))
        SEQS.append(dict(name=f"l{b}", b=b, ctx=False, T=SEQ, kind=b, xin=x_in[b]))
    for s in SEQS:
        T = s["T"]
        n = s["name"]
        s["XS"] = scr(f"XS_{n}", [T, D])
        s["FM"] = scr(f"FM_{n}", [2048, T])
        s["TM"] = scr(f"TM_{n}", [T, 2080])
        s["QK"] = scr(f"QK_{n}", [2048, T], BF16)
        s["KT"] = scr(f"KT_{n}", [T, 1024], BF16)
        s["Y"] = scr(f"Y_{n}", [T, D])
        s["H2"] = scr(f"H2_{n}", [T, D], BF16)
        s["AFFT"] = scr(f"AFFT_{n}", [NE, T])
        s["YE"] = scr(f"YE_{n}", [NE, T // 8, D], BF16)
        s["nt"] = T // 128
        s["cap"] = T // 8
    MODS = scr("MODS", [DEPTH, 6, 3, D])

    dbg_outs = {}

    def dbg_tap(name, shape):
        dbg_outs[name] = nc.dram_tensor(name, list(shape), F32, kind="ExternalOutput").ap()
        return dbg_outs[name]

    P = Prog(nc)
    k = K(P)
    with ExitStack() as es:
        CA_COLS = 20 * 1024
        PA_COLS = 83 * 1024
        ca_t = es.enter_context(nc.sbuf_tensor("carena", [128, CA_COLS], BF16))
        pa_t = es.enter_context(nc.sbuf_tensor("parena", [128, PA_COLS], BF16))
        CA = Arena(ca_t, CA_COLS)
        PA = Arena(pa_t, PA_COLS)
        PS = [Tile(es.enter_context(nc.psum_tensor(f"psb{i}", [128, 512], F32)), f"psb{i}") for i in range(8)]

        def ps_bf(t):
            return t.ap.bitcast(BF16)

        iota_p = CA.alloc("iota_p", [1], F32)
        iota_p128 = CA.alloc("iota_p128", [1], F32)
        iota_f = CA.alloc("iota_f", [2048], F32)
        ident32 = CA.alloc("ident32", [128], F32)
        identbf = CA.alloc("identbf", [128], BF16)
        Mfwd = CA.alloc("Mfwd", [128], F32)
        Mbwd = CA.alloc("Mbwd", [128], F32)
        ones32 = CA.alloc("ones32", [128], F32)
        oh32 = CA.alloc("oh32", [32, 128], BF16, parts=32)
        Psw = CA.alloc("Psw", [128], F32)
        cosT = CA.alloc("cosT", [2048], F32)
        sinT = CA.alloc("sinT", [2048], F32)
        P.op("pool", lambda e: e.iota(iota_p[:], pattern=[[0, 1]], base=0, channel_multiplier=1, allow_small_or_imprecise_dtypes=True), writes=[iota_p])
        P.op("pool", lambda e: e.iota(iota_p128[:], pattern=[[0, 1]], base=128, channel_multiplier=1, allow_small_or_imprecise_dtypes=True), writes=[iota_p128])
        P.op("pool", lambda e: e.iota(iota_f[:], pattern=[[1, 2048]], base=0, channel_multiplier=0, allow_small_or_imprecise_dtypes=True), writes=[iota_f])
        k.ts("dve", ident32[:], iota_f[:, 0:128], iota_p[:, 0:1], ALU.is_equal, [iota_f, iota_p], [ident32])
        k.copy("dve", identbf[:], ident32[:], [ident32], [identbf])
        k.ts("dve", Mfwd[:], iota_f[:, 0:128], iota_p[:, 0:1], ALU.is_ge, [iota_f, iota_p], [Mfwd])
        k.ts("dve", Mbwd[:], iota_f[:, 0:128], iota_p[:, 0:1], ALU.is_le, [iota_f, iota_p], [Mbwd])
        k.memset("dve", ones32[:], 1.0, [ones32])
        ohf = PA.alloc("ohf", [32, 128], F32, parts=32)
        for e_ in range(32):
            k.ts("dve", ohf[:, e_, :], ones32[0:32, :], float(e_), ALU.mult, [ones32], [ohf])
        k.ts("dve", oh32[:].rearrange("p a b -> p (a b)"), ohf[:].rearrange("p a b -> p (a b)"), iota_p[0:32, 0:1], ALU.is_equal, [ohf, iota_p], [oh32])
        tA = PA.alloc("tA", [128], F32)
        tB = PA.alloc("tB", [128], F32)
        tC = PA.alloc("tC", [128], F32)
        pm32 = PA.alloc("pm32", [2], F32)
        k.ts("dve", pm32[:, 0:1], iota_p[:, 0:1], -32.0, ALU.add, [iota_p], [pm32])
        k.ts("dve", pm32[:, 1:2], iota_p[:, 0:1], 32.0, ALU.add, [iota_p], [pm32])
        k.ts("dve", tA[:], iota_f[:, 0:128], pm32[:, 0:1], ALU.is_equal, [iota_f, pm32], [tA])
        k.ts("dve", tB[:], iota_f[:, 0:128], pm32[:, 1:2], ALU.is_equal, [iota_f, pm32], [tB])
        tD = PA.alloc("tD", [128], F32)
        k.ts("dve", tC[:], iota_f[:, 0:128], 64.0, ALU.is_ge, [iota_f], [tC], s2=None)
        k.ts("dve", tD[:], iota_f[:, 0:128], 96.0, ALU.is_lt, [iota_f], [tD])
        k.tt("dve", tC[:], tC[:], tD[:], ALU.mult, [tC, tD], [tC])
        k.ts("dve", tD[:], iota_f[:, 0:128], 32.0, ALU.is_lt, [iota_f], [tD])
        k.tt("dve", tC[:], tC[:], tD[:], ALU.add, [tC, tD], [tC])
        k.tt("dve", tA[:], tA[:], tC[:], ALU.mult, [tA, tC], [tA])
        k.ts("dve", tC[:], tC[:], -1.0, ALU.mult, [tC], [tC], s2=1.0, op1=ALU.add)
        k.tt("dve", tB[:], tB[:], tC[:], ALU.mult, [tB, tC], [tB])
        k.tt("dve", Psw[:], tB[:], tA[:], ALU.subtract, [tA, tB], [Psw])
        inv = PA.alloc("inv", [1], F32)
        posT = PA.alloc("posT", [2048], F32)
        ang = PA.alloc("ang", [2048], F32)
        inv2 = PA.alloc("inv2", [1], F32)
        k.copy("dve", inv[:], iota_p[:, 0:1], [iota_p], [inv])
        for thr in (32.0, 64.0, 96.0):
            k.ts("dve", inv2[:], iota_p[:, 0:1], thr, ALU.is_ge, [iota_p], [inv2], s2=-32.0, op1=ALU.mult)
            k.tt("dve", inv[:], inv[:], inv2[:], ALU.add, [inv, inv2], [inv])
        k.act(inv[:], inv[:], AF.Exp, [inv], [inv], scale=-float(np.log(10000.0) / 32.0))
        P.op("pool", lambda e: e.iota(posT[64:128, :], pattern=[[0, 32], [1, 64]], base=0, channel_multiplier=0, allow_small_or_imprecise_dtypes=True), writes=[posT])
        P.op("pool", lambda e: e.iota(posT[0:64, :], pattern=[[1, 32], [0, 64]], base=0, channel_multiplier=0, allow_small_or_imprecise_dtypes=True), writes=[posT])
        k.ts("dve", ang[:], posT[:], inv[:, 0:1], ALU.mult, [posT, inv], [ang])
        PI = float(np.pi)
        red = PA.alloc("red", [2048], F32)
        cmpt = PA.alloc("cmpt", [2048], F32)
        for (dst, shift) in ((sinT, 0.0), (cosT, 0.5 * PI)):
            k.ts("dve", red[:], ang[:], shift, ALU.add, [ang], [red])
            for kk in range(1, 12):
                k.ts("dve", cmpt[:], ang[:], 2 * PI * kk - PI - shift, ALU.is_ge, [ang], [cmpt], s2=-2 * PI, op1=ALU.mult)
                k.tt("dve", red[:], red[:], cmpt[:], ALU.add, [red, cmpt], [red])
            k.ts("dve", red[:], red[:], -3.141592, ALU.max, [red], [red], s2=3.141592, op1=ALU.min)
            k.act(dst[:], red[:], AF.Sin, [red], [dst])

        cT = CA.alloc("cT", [8, 3], F32)
        for b in range(BPC):
            P.dma("sp", cT[:, :, b], c_in[b].rearrange("(kc p) -> p kc", p=128), writes=[cT], allow_slow_non_contiguous=True)
        P.dma("sp", cT[:, :, 2], cctx_in.rearrange("(kc p) -> p kc", p=128), writes=[cT], allow_slow_non_contiguous=True)
        k.act(cT[:], cT[:], AF.Silu, [cT], [cT])

        def ada_alloc():
            return dict(awr=Ring([PA.alloc(f"aw{i}", [8, 512], F32) for i in range(2)]),
                        abr=Ring([PA.alloc(f"ab{i}", [512], F32, parts=3) for i in range(2)]),
                        gr=Ring([PA.alloc(f"gg{i}", [512], F32, parts=3) for i in range(2)]),
                        mrow=Ring([PA.alloc(f"mrow{i}", [512], F32, parts=3) for i in range(3)]))

        def ada_block(R_, l_, nb, psr):
            m, hf = nb // 2, nb % 2
            aw = R_["awr"].next()
            P.dma("sp", aw[:], ada_w[l_].rearrange("(kc p) n -> p kc n", p=128)[:, :, nb * 512:(nb + 1) * 512], writes=[aw])
            ab = R_["abr"].next()
            P.dma("sp", ab[:], ada_b[l_, nb * 512:(nb + 1) * 512].partition_broadcast(3), writes=[ab])
            ps = psr.next()
            for kc in range(8):
                k.mm(ps, ps[0:3, :], cT[:, kc, :], aw[:, kc, :], kc == 0, kc == 7, [cT, aw])
            mr = R_["mrow"].next()
            k.tt("dve", mr[:], ps[0:3, :], ab[:], ALU.add, [ps, ab], [mr])
            if m in (1, 4):
                g = R_["gr"].next()
                gsrc = norm_mix_g if m == 1 else norm_ffn_g
                P.dma("sp", g[:], gsrc[l_, hf * 512:(hf + 1) * 512].partition_broadcast(3), writes=[g])
                k.stt("dve", mr[:], mr[:], 1.0, g[:], ALU.add, ALU.mult, [mr, g], [mr])
            slot = {1: 0, 0: 1, 2: 2, 4: 3, 3: 4, 5: 5}[m]
            P.dma("pool", MODS[l_, slot, :, hf * 512:(hf + 1) * 512], mr[:], reads=[mr])

        R0 = ada_alloc()
        psr0 = Ring(PS[0:2])
        for nb in range(12):
            ada_block(R0, 0, nb, psr0)
        P.barrier()
        PA.reset()

        def load_bc(name, src_row):
            t = PA.alloc(name, [D], F32)
            P.dma("sp", t[:], src_row.partition_broadcast(128), writes=[t])
            return t

        def rstd_from_ssq(ssq, n, scr2):
            raise NotImplementedError

        for l in range(DEPTH):
            last = l == DEPTH - 1
            is_ml = (l % 2 == 0)
            j = l // 2
            if is_ml:
                w_in, NP, NFM, NHDK = ml_w_in[j], 3104, 8, 512
                w_out, hn_g = ml_w_out[j], ml_hn_g[j]
                dk, VC0, OC0 = 64, 0, 1024
            else:
                w_in, NP, NFM, NHDK = rt_w_in[j], 4096, 16, 1024
                w_out, hn_g = rt_w_out[j], rt_gn_g[j]
                dk, VC0, OC0 = 128, 0, 1024
            NTM = NP - NFM * 128

            def xsrc(s):
                return s["xin"] if l == 0 else s["XS"]

            Win = PA.alloc("Win", [8, NP], BF16)
            for kc in range(8):
                P.dma("pool", Win[:, kc, :], w_in[kc * 128:(kc + 1) * 128, :], writes=[Win])
            A1 = [load_bc(f"A1_{kd}", MODS[l, 0, kd]) for kd in range(3)]
            B1 = [load_bc(f"B1_{kd}", MODS[l, 1, kd]) for kd in range(3)]
            xr = Ring([PA.alloc(f"xt{i}", [D], F32) for i in range(3)])
            junk = PA.alloc("junk", [D], F32)
            ssr = Ring([PA.alloc(f"ss{i}", [2], F32) for i in range(3)])
            tmpr = Ring([PA.alloc(f"tmp{i}", [D], F32) for i in range(2)])
            hbr = Ring([PA.alloc(f"hb{i}", [D], BF16) for i in range(2)])
            hTr = Ring([PA.alloc(f"hT{i}", [8, 512], BF16) for i in range(2)])
            fmr = Ring([PA.alloc(f"fms{i}", [512], F32) for i in range(3)])
            tmr = Ring([PA.alloc(f"tms{i}", [NTM], F32) for i in range(2)])
            ps_tr = Ring(PS[0:2])
            ps_mm = Ring(PS[2:8])
            for s in SEQS:
                T, kd = s["T"], s["kind"]
                NB = min(512, T)
                for blk in range(T // NB):
                    hT = hTr.next()
                    for ti in range(NB // 128):
                        t0 = blk * NB + ti * 128
                        xt = xr.next()
                        P.dma("sp", xt[:], xsrc(s)[t0:t0 + 128, :], writes=[xt])
                        ss = ssr.next()
                        k.act(junk[:], xt[:], AF.Square, [xt], [junk, ss], accum=ss[:, 0:1])
                        k.act(ss[:, 1:2], ss[:, 0:1], AF.Ln, [ss], [ss], scale=1.0 / D, bias=EPS)
                        k.act(ss[:, 1:2], ss[:, 1:2], AF.Exp, [ss], [ss], scale=-0.5)
                        tmp = tmpr.next()
                        k.stt("dve", tmp[:], xt[:], ss[:, 1:2], A1[kd][:], ALU.mult, ALU.mult, [xt, ss, A1[kd]], [tmp])
                        hb = hbr.next()
                        k.tt("dve", hb[:], tmp[:], B1[kd][:], ALU.add, [tmp, B1[kd]], [hb])
                        pt = ps_tr.next()
                        for kc in range(8):
                            k.tr(pt, ps_bf(pt)[:, kc * 128:(kc + 1) * 128], hb[:, kc * 128:(kc + 1) * 128], identbf[:], [hb, identbf])
                        k.evac(hT[:, :, ti * 128:(ti + 1) * 128], ps_bf(pt).rearrange("p (a b) -> p a b", a=8), [pt], [hT])
                    for fc in range(NFM):
                        ps = ps_mm.next()
                        for kc in range(8):
                            k.mm(ps, ps[:, 0:NB], Win[:, kc, fc * 128:(fc + 1) * 128], hT[:, kc, 0:NB], kc == 0, kc == 7, [Win, hT])
                        st = fmr.next()
                        k.evac(st[:, 0:NB], ps[:, 0:NB], [ps], [st])
                        P.dma("pool", s["FM"][fc * 128:(fc + 1) * 128, blk * NB:(blk + 1) * NB], st[:, 0:NB], reads=[st])
                    for ti in range(NB // 128):
                        t0 = blk * NB + ti * 128
                        st = tmr.next()
                        c0 = 0
                        while c0 < NTM:
                            cw = min(512, NTM - c0)
                            ps = ps_mm.next()
                            for kc in range(8):
                                k.mm(ps, ps[:, 0:cw], hT[:, kc, ti * 128:(ti + 1) * 128], Win[:, kc, NFM * 128 + c0:NFM * 128 + c0 + cw], kc == 0, kc == 7, [Win, hT])
                            k.evac(st[:, c0:c0 + cw], ps[:, 0:cw], [ps], [st])
                            c0 += cw
                        P.dma("pool", s["TM"][t0:t0 + 128, 0:NTM], st[:], reads=[st])
            P.barrier()
            PA.reset()

            QOFF, KOFF = 0, NHDK
            if is_ml:
                cwt = PA.alloc("cwt", [8, 5], F32)
                cbt = PA.alloc("cbt", [8], F32)
                for jj in range(5):
                    P.dma("sp", cwt[:, :, jj], ml_conv_w[j, jj].rearrange("(fc p) -> p fc", p=128), writes=[cwt], allow_slow_non_contiguous=True)
                P.dma("sp", cbt[:], ml_conv_b[j].rearrange("(fc p) -> p fc", p=128), writes=[cbt], allow_slow_non_contiguous=True)
            xpr = {T_: Ring([PA.alloc(f"xp{T_}_{i}", [T_ + 4], F32) for i in range(2)]) for T_ in (CTX, SEQ)}
            for T_ in (CTX, SEQ):
                for t_ in xpr[T_].tiles:
                    k.memset("pool", t_[:, 0:2], 0.0, [t_])
                    k.memset("pool", t_[:, T_ + 2:T_ + 4], 0.0, [t_])
            accr = Ring([PA.alloc(f"acc{i}", [SEQ], F32) for i in range(2)])
            t2r = Ring([PA.alloc(f"t2{i}", [SEQ], F32) for i in range(2)])
            qbr = Ring([PA.alloc(f"qb{i}", [SEQ], BF16) for i in range(3)])
            ktr = Ring([PA.alloc(f"kts{i}", [16, 128], BF16) for i in range(2)])
            ps_tr = Ring(PS[0:2])
            ps_sw = Ring(PS[2:6])
            kscale = float(128.0 ** -0.5)
            ada_todo = []
            if l + 1 < DEPTH:
                R_ada = ada_alloc()
                ps_ada = Ring(PS[6:8])
                ada_todo = list(range(12))
            for s in SEQS:
                T, nt = s["T"], s["nt"]
                for fc in range(NFM):
                    if ada_todo:
                        ada_block(R_ada, l + 1, ada_todo.pop(0), ps_ada)
                    is_k = fc >= NFM // 2
                    xp = xpr[T].next()
                    P.dma("sp", xp[:, 2:T + 2], s["FM"][fc * 128:(fc + 1) * 128, 0:T], writes=[xp])
                    qb = qbr.next()
                    if is_ml:
                        acc = accr.next()
                        k.ts("dve", acc[:, 0:T], xp[:, 0:T], cwt[:, fc, 0:1], ALU.mult, [xp, cwt, cbt], [acc], s2=cbt[:, fc:fc + 1], op1=ALU.add)
                        for jj in range(1, 5):
                            eng = "pool" if jj in (2, 4) else "dve"
                            k.stt(eng, acc[:, 0:T], xp[:, jj:jj + T], cwt[:, fc, jj:jj + 1], acc[:, 0:T], ALU.mult, ALU.add, [xp, cwt, acc], [acc])
                        if is_k:
                            k.act(qb[:, 0:T], acc[:, 0:T], AF.Silu, [acc], [qb])
                        else:
                            t2 = t2r.next()
                            k.act(t2[:, 0:T], acc[:, 0:T], AF.Silu, [acc], [t2])
                            k.ts("dve", qb[:, 0:T], t2[:, 0:T], 0.125, ALU.mult, [t2], [qb])
                    else:
                        sc_ = kscale if is_k else 1.0
                        if s["ctx"]:
                            k.ts("dve", qb[:, 0:T], xp[:, 2:T + 2], sc_, ALU.mult, [xp], [qb])
                        else:
                            acc = accr.next()
                            t2 = t2r.next()
                            for b4 in range(T // 512):
                                ps = ps_sw.next()
                                k.mm(ps, ps[:], Psw[:], xp[:, 2 + b4 * 512:2 + (b4 + 1) * 512], True, True, [Psw, xp])
                                k.stt("dve", t2[:, b4 * 512:(b4 + 1) * 512], ps[:], sc_, sinT[:, b4 * 512:(b4 + 1) * 512], ALU.mult, ALU.mult, [ps, sinT], [t2])
                            k.stt("pool", acc[:, 0:T], xp[:, 2:T + 2], sc_, cosT[:, 0:T], ALU.mult, ALU.mult, [xp, cosT], [acc])
                            k.tt("dve", qb[:, 0:T], acc[:, 0:T], t2[:, 0:T], ALU.add, [acc, t2], [qb])
                    P.dma("pool", s["QK"][fc * 128:(fc + 1) * 128, 0:T], qb[:, 0:T], reads=[qb])
                    if is_k:
                        kt = ktr.next()
                        for g0 in range(0, nt, 8):
                            pt = ps_tr.next()
                            ng = min(8, nt - g0)
                            for ti in range(ng):
                                k.tr(pt, ps_bf(pt)[:, ti * 128:(ti + 1) * 128], qb[:, (g0 + ti) * 128:(g0 + ti + 1) * 128], identbf[:], [qb, identbf])
                            k.evac(kt[:, g0:g0 + ng, :], ps_bf(pt)[:, 0:ng * 128].rearrange("p (a b) -> p a b", a=ng), [pt], [kt])
                        kc0 = (fc - NFM // 2) * 128
                        P.dma("pool", s["KT"].rearrange("(n p) f -> p n f", p=128)[:, :, kc0:kc0 + 128], kt[:, 0:nt, :], reads=[kt])
            P.barrier()
            PA.reset()

            NTT = 18
            NS = NTT - 1
            ORD = [list(range(NTT)), [1, 0] + list(range(17, 1, -1))]
            gbias = PA.alloc("gbias", [32], F32)
            if is_ml:
                P.dma("sp", gbias[:, 0:16], ml_ig_b[j].partition_broadcast(128), writes=[gbias])
                P.dma("sp", gbias[:, 16:32], ml_fg_b[j].partition_broadcast(128), writes=[gbias])
            else:
                P.dma("sp", gbias[:, 16:32], rt_decay[j].partition_broadcast(128), writes=[gbias])
            Gt = PA.alloc("Gt", [NTT, 32], F32)
            igb = PA.alloc("igb", [NTT, 16], F32)
            spt = PA.alloc("spt", [NTT, 16], F32)
            chf = PA.alloc("chf", [NTT, 16], F32)
            gsets = [dict(wt=PA.alloc(f"wt{i}", [NTT, 16], F32), rowf=PA.alloc(f"rowf{i}", [NTT, 16], F32),
                          fS=[PA.alloc(f"fS{i}_{d}", [8, NTT + 1], F32) for d in range(2)]) for i in range(2)]
            hdr = [dict(
                qT=PA.alloc(f"qT{i}", [NTT * 128], BF16),
                kT=PA.alloc(f"kT{i}", [NTT * 128], BF16),
                ktok=PA.alloc(f"ktok{i}", [NTT, dk], BF16),
                vaug=PA.alloc(f"vaug{i}", [NTT, 129], BF16),
                vp=[PA.alloc(f"vp{i}_{d}", [NTT, 129], BF16) for d in range(2)],
                atm=[PA.alloc(f"atm{i}_{d}", [NTT, 128], BF16) for d in range(2)],
                cbf=[PA.alloc(f"cbf{i}_{d}", [NS, 129], BF16) for d in range(2)],
                Y=PA.alloc(f"Y{i}", [NTT, 128], F32),
            ) for i in range(2)]
            for hb_ in hdr:
                k.memset("pool", hb_["vaug"][:, :, 128:129], 1.0, [hb_["vaug"]])
            dCall = [PA.alloc(f"dCall{d}", [129, NS], F32) for d in range(2)]
            Fb = [PA.alloc(f"Fb{d}", [129, NS], F32) for d in range(2)]
            ats = Ring([PA.alloc(f"ats{i}", [128], F32) for i in range(3)])
            resr = Ring([PA.alloc(f"res{i}", [129], F32) for i in range(6)])
            ddr = Ring([PA.alloc(f"dd{i}", [2], F32) for i in range(6)])
            ps_g = PS[0]
            ps_at = Ring(PS[1:3])
            ps_dc = Ring(PS[3:5])
            ps_out = Ring(PS[5:8])

            def gate_prep(b, GS):
                sc, sl = SEQS[2 * b], SEQS[2 * b + 1]
                parts = [(sc, 0, 2), (sl, 2, 16)]
                wt, rowf, fS = GS["wt"], GS["rowf"], GS["fS"]
                if is_ml:
                    for (s, o0, n_) in parts:
                        P.dma("sp", Gt[:, o0:o0 + n_, :], s["TM"].rearrange("(n p) c -> p n c", p=128)[:, :, 2048:2080], writes=[Gt])
                    gb_ig = gbias[:, 0:16].unsqueeze(1).to_broadcast([128, NTT, 16])
                    gb_fg = gbias[:, 16:32].unsqueeze(1).to_broadcast([128, NTT, 16])
                    k.tt("dve", igb[:], Gt[:, :, 0:16], gb_ig, ALU.add, [Gt, gbias], [igb])
                    k.tt("dve", spt[:], Gt[:, :, 16:32], gb_fg, ALU.add, [Gt, gbias], [spt])
                else:
                    k.memset("dve", igb[:], 0.0, [igb])
                    k.copy("dve", spt[:], gbias[:, 16:32].unsqueeze(1).to_broadcast([128, NTT, 16]), [gbias], [spt])
                k.act(spt[:], spt[:], AF.Exp, [spt], [spt], scale=-1.0)
                k.act(spt[:], spt[:], AF.Ln, [spt], [spt], bias=1.0)
                for t_ in range(NTT):
                    k.mm(ps_g, ps_g[:, t_ * 16:t_ * 16 + 8], Mfwd[:], spt[:, t_, 0:8], True, True, [Mfwd, spt])
                    k.mm(ps_g, ps_g[:, t_ * 16 + 8:t_ * 16 + 16], Mbwd[:], spt[:, t_, 8:16], True, True, [Mbwd, spt])
                pt_ = ps_at.next()
                k.mm(pt_, pt_[:, 0:NTT * 16], ones32[:], spt[:].rearrange("p a b -> p (a b)"), True, True, [ones32, spt])
                cum = ps_g[:, 0:NTT * 16]
                tot = pt_[:, 0:NTT * 16]
                k.tt("dve", wt[:].rearrange("p a b -> p (a b)"), igb[:].rearrange("p a b -> p (a b)"), cum, ALU.add, [igb, ps_g], [wt])
                k.act(wt[:], wt[:], AF.Exp, [wt], [wt])
                k.act(rowf[:].rearrange("p a b -> p (a b)"), cum, AF.Exp, [ps_g], [rowf], scale=-1.0)
                k.act(chf[:].rearrange("p a b -> p (a b)"), tot, AF.Exp, [pt_], [chf], scale=-1.0)
                for d in range(2):
                    k.memset("pool", fS[d][:, :, 0:1], 0.0, [fS[d]])
                k.copy("pool", fS[0][:, :, 1:NTT + 1], chf[:, :, 0:8].rearrange("p t h -> p h t"), [chf], [fS[0]])
                for s_ in range(NTT):
                    k.copy("pool", fS[1][:, :, s_ + 1], chf[:, ORD[1][s_], 8:16], [chf], [fS[1]])

            def head_pre(b, h, H, GS):
                sc, sl = SEQS[2 * b], SEQS[2 * b + 1]
                parts = [(sc, 0, 2), (sl, 2, 16)]
                wt, fS = GS["wt"], GS["fS"]
                qT, kT, ktok, vaug = H["qT"], H["kT"], H["ktok"], H["vaug"]
                for (s, o0, n_) in parts:
                    P.dma("sp", qT[0:dk, o0 * 128:(o0 + n_) * 128], s["QK"][QOFF + h * dk:QOFF + (h + 1) * dk, :], writes=[qT])
                    P.dma("sp", kT[0:dk, o0 * 128:(o0 + n_) * 128], s["QK"][KOFF + h * dk:KOFF + (h + 1) * dk, :], writes=[kT])
                    P.dma("sp", ktok[:, o0:o0 + n_, :], s["KT"].rearrange("(n p) f -> p n f", p=128)[:, :, h * dk:(h + 1) * dk], writes=[ktok])
                    P.dma("pool", vaug[:, o0:o0 + n_, 0:128], s["TM"].rearrange("(n p) c -> p n c", p=128)[:, :, VC0 + h * 128:VC0 + (h + 1) * 128], writes=[vaug])
                for d in range(2):
                    col = d * 8 + h
                    k.tt("pool", H["vp"][d][:], vaug[:], wt[:, :, col:col + 1].to_broadcast([128, NTT, 129]), ALU.mult, [vaug, wt], [H["vp"][d]])
                for t_ in range(NTT):
                    pa = ps_at.next()
                    k.mm(pa, pa[:, 0:128], kT[0:dk, t_ * 128:(t_ + 1) * 128], qT[0:dk, t_ * 128:(t_ + 1) * 128], True, True, [kT, qT])
                    a_ = ats.next()
                    k.copy("act", a_[:], pa[:, 0:128], [pa], [a_])
                    k.tt("dve", H["atm"][0][:, t_, :], a_[:], Mfwd[:], ALU.mult, [a_, Mfwd], [H["atm"][0]])
                    k.tt("pool", H["atm"][1][:, t_, :], a_[:], Mbwd[:], ALU.mult, [a_, Mbwd], [H["atm"][1]])
                yield
                for d in range(2):
                    vp = H["vp"][d]
                    for s0 in range(0, NS, 3):
                        ng = min(3, NS - s0)
                        pd = ps_dc.next()
                        for i_ in range(ng):
                            t_ = ORD[d][s0 + i_]
                            k.mm(pd, pd[0:dk, i_ * 129:(i_ + 1) * 129], ktok[:, t_, :], vp[:, t_, :], True, True, [ktok, vp])
                        k.evac(dCall[d][0:dk, :, s0:s0 + ng], pd[0:dk, 0:ng * 129].rearrange("p (s c) -> p c s", s=ng), [pd], [dCall[d]])
                yield
                for d in range(2):
                    k.copy("pool", Fb[d][0:dk, :, :], fS[d][0:dk, h, 0:NS].unsqueeze(1).to_broadcast([dk, 129, NS]), [fS[d]], [Fb[d]])
                    dflat = dCall[d][0:dk].rearrange("p c s -> p (c s)")
                    fflat = Fb[d][0:dk].rearrange("p c s -> p (c s)")
                    P.op("dve", lambda e, dflat=dflat, fflat=fflat: e.tensor_tensor_scan(out=dflat, data0=fflat, data1=dflat, initial=0.0, op0=ALU.mult, op1=ALU.add),
                         reads=[Fb[d], dCall[d]], writes=[dCall[d]])
                    k.tt("dve", H["cbf"][d][0:dk].rearrange("p s c -> p c s"), dCall[d][0:dk],
                         fS[d][0:dk, h, 1:NS + 1].unsqueeze(1).to_broadcast([dk, 129, NS]), ALU.mult, [dCall[d], fS[d]], [H["cbf"][d]])

            def head_out(b, h, H, GS):
                sc, sl = SEQS[2 * b], SEQS[2 * b + 1]
                parts = [(sc, 0, 2), (sl, 2, 16)]
                rowf = GS["rowf"]
                qT = H["qT"]
                for st_ in range(NTT):
                    if st_ in (6, 12):
                        yield
                    for d in range(2):
                        t_ = ORD[d][st_]
                        col = d * 8 + h
                        vp, atm, Yd, cbf = H["vp"][d], H["atm"][d], H["Y"], H["cbf"][d]
                        first = (d == 0) == (ORD[0].index(t_) <= ORD[1].index(t_))
                        po = ps_out.next()
                        k.mm(po, po[:, 0:129], atm[:, t_, :], vp[:, t_, :], True, st_ == 0, [atm, vp])
                        if st_ > 0:
                            k.mm(po, po[:, 0:129], qT[0:dk, t_ * 128:(t_ + 1) * 128], cbf[0:dk, st_ - 1, :], False, True, [qT, cbf])
                        if is_ml:
                            res = resr.next()
                            k.act(res[:], po[:, 0:129], AF.Copy, [po, rowf], [res], scale=rowf[:, t_, col:col + 1])
                            dd = ddr.next()
                            k.act(dd[:, 0:1], res[:, 128:129], AF.Abs, [res], [dd])
                            k.ts("pool", dd[:, 0:1], dd[:, 0:1], 1.0, ALU.max, [dd], [dd])
                            P.op("dve", lambda e, dd=dd: e.reciprocal(out=dd[:, 1:2], in_=dd[:, 0:1]), reads=[dd], writes=[dd])
                            if first:
                                k.ts("dve", Yd[:, t_, :], res[:, 0:128], dd[:, 1:2], ALU.mult, [res, dd], [Yd])
                            else:
                                k.stt("dve", Yd[:, t_, :], res[:, 0:128], dd[:, 1:2], Yd[:, t_, :], ALU.mult, ALU.add, [res, dd, Yd], [Yd])
                        else:
                            if first:
                                k.act(Yd[:, t_, :], po[:, 0:128], AF.Copy, [po, rowf], [Yd], scale=rowf[:, t_, col:col + 1])
                            else:
                                k.stt("dve", Yd[:, t_, :], po[:, 0:128], rowf[:, t_, col:col + 1], Yd[:, t_, :], ALU.mult, ALU.add, [po, rowf, Yd], [Yd])
                for (s, o0, n_) in parts:
                    if s["ctx"] and last:
                        continue
                    P.dma("sp", s["Y"].rearrange("(n p) f -> p n f", p=128)[:, :, h * 128:(h + 1) * 128], H["Y"][:, o0:o0 + n_, :], reads=[H["Y"]])

            heads = [(b, h) for b in range(BPC) for h in range(8)]
            for i_h in range(len(heads) + 1):
                gens = []
                if i_h < len(heads):
                    b, h = heads[i_h]
                    if h == 0:
                        gate_prep(b, gsets[b % 2])
                    gens.append(head_pre(b, h, hdr[i_h % 2], gsets[b % 2]))
                if i_h >= 1:
                    b, h = heads[i_h - 1]
                    gens.append(head_out(b, h, hdr[(i_h - 1) % 2], gsets[b % 2]))
                while gens:
                    for g_ in list(gens):
                        try:
                            next(g_)
                        except StopIteration:
                            gens.remove(g_)
            P.barrier()
            PA.reset()

            Wout = PA.alloc("Wout", [8, D], BF16)
            P.dma("pool", Wout[:], w_out.rearrange("(kc p) n -> p kc n", p=128), writes=[Wout])
            rtr = PA.alloc("rtr", [8, NE], F32)
            P.dma("sp", rtr[:], moe_router[l].rearrange("(kc p) e -> p kc e", p=128), writes=[rtr])
            hng = load_bc("hng", hn_g)
            G2 = [load_bc(f"G2_{kd}", MODS[l, 2, kd]) for kd in range(3)]
            A2 = [load_bc(f"A2_{kd}", MODS[l, 3, kd]) for kd in range(3)]
            B2 = [load_bc(f"B2_{kd}", MODS[l, 4, kd]) for kd in range(3)]
            yr = Ring([PA.alloc(f"yt{i}", [8, 128], F32) for i in range(3)])
            orr = Ring([PA.alloc(f"ot{i}", [D], F32) for i in range(3)])
            xr = Ring([PA.alloc(f"xt{i}", [D], F32) for i in range(3)])
            sqr = Ring([PA.alloc(f"sq{i}", [8, 128], F32) for i in range(2)])
            st8 = Ring([PA.alloc(f"st8{i}", [4, 8], F32) for i in range(2)])
            ybr = Ring([PA.alloc(f"yb{i}", [D], BF16) for i in range(2)])
            ybT = Ring([PA.alloc(f"ybT{i}", [8, 128], BF16) for i in range(2)])
            xnr = Ring([PA.alloc(f"xn{i}", [D], F32) for i in range(2)])
            tmpr = Ring([PA.alloc(f"tmp{i}", [D], F32) for i in range(2)])
            h2r = Ring([PA.alloc(f"h2{i}", [D], F32) for i in range(2)])
            h2br = Ring([PA.alloc(f"h2b{i}", [D], BF16) for i in range(2)])
            h2Tr = Ring([PA.alloc(f"h2T{i}", [8, 128], F32) for i in range(2)])
            junk = PA.alloc("junk", [D], F32)
            ssr = Ring([PA.alloc(f"ss{i}", [4], F32) for i in range(3)])
            smr = Ring([PA.alloc(f"sm{i}", [4], F32) for i in range(2)])
            affr = Ring([PA.alloc(f"aff{i}", [NE], F32) for i in range(2)])
            aftr = Ring([PA.alloc(f"aft{i}", [128], F32, parts=NE) for i in range(2)])
            ps_tr = Ring(PS[0:2])
            ps_o = Ring(PS[2:6])
            ps_t32 = Ring(PS[6:8])
            for s in SEQS:
                if s["ctx"] and last:
                    continue
                T, kd, nt = s["T"], s["kind"], s["nt"]
                for ti in range(nt):
                    t0 = ti * 128
                    yt = yr.next()
                    P.dma("sp", yt[:], s["Y"][t0:t0 + 128, :].rearrange("p (h f) -> p h f", h=8), writes=[yt])
                    ot = orr.next()
                    P.dma("sp", ot[:], s["TM"][t0:t0 + 128, OC0:OC0 + 1024], writes=[ot])
                    xt = xr.next()
                    P.dma("sp", xt[:], xsrc(s)[t0:t0 + 128, :], writes=[xt])
                    sq = sqr.next()
                    s8 = st8.next()
                    if not is_ml:
                        k.P.op("dve", lambda e, s8=s8, yt=yt: e.tensor_reduce(out=s8[:, 0, :], in_=yt[:], axis=AX.X, op=ALU.add), reads=[yt], writes=[s8])
                        k.ts("dve", s8[:, 0, :], s8[:, 0, :], -1.0 / 128.0, ALU.mult, [s8], [s8])
                        k.tt("dve", yt[:], yt[:], s8[:, 0, :].unsqueeze(2).to_broadcast([128, 8, 128]), ALU.add, [yt, s8], [yt])
                    k.tt("dve", sq[:], yt[:], yt[:], ALU.mult, [yt], [sq])
                    k.P.op("dve", lambda e, s8=s8, sq=sq: e.tensor_reduce(out=s8[:, 1, :], in_=sq[:], axis=AX.X, op=ALU.add), reads=[sq], writes=[s8])
                    k.act(s8[:, 2, :], s8[:, 1, :], AF.Ln, [s8], [s8], scale=1.0 / 128.0, bias=EPS)
                    k.act(s8[:, 3, :], s8[:, 2, :], AF.Exp, [s8], [s8], scale=-0.5)
                    k.tt("dve", sq[:], yt[:], s8[:, 3, :].unsqueeze(2).to_broadcast([128, 8, 128]), ALU.mult, [yt, s8], [sq])
                    k.act(ot[:], ot[:], AF.Sigmoid if is_ml else AF.Silu, [ot], [ot])
                    k.tt("dve", sq[:].rearrange("p a b -> p (a b)"), sq[:].rearrange("p a b -> p (a b)"), hng[:], ALU.mult, [sq, hng], [sq])
                    yb = ybr.next()
                    k.tt("dve", yb[:], sq[:].rearrange("p a b -> p (a b)"), ot[:], ALU.mult, [sq, ot], [yb])
                    pt = ps_tr.next()
                    for kc in range(8):
                        k.tr(pt, ps_bf(pt)[:, kc * 128:(kc + 1) * 128], yb[:, kc * 128:(kc + 1) * 128], identbf[:], [yb, identbf])
                    yT = ybT.next()
                    k.evac(yT[:], ps_bf(pt).rearrange("p (a b) -> p a b", a=8), [pt], [yT])
                    xn = xnr.next()
                    tmp = tmpr.next()
                    for hf in range(2):
                        po = ps_o.next()
                        for kc in range(8):
                            k.mm(po, po[:], yT[:, kc, :], Wout[:, kc, hf * 512:(hf + 1) * 512], kc == 0, kc == 7, [yT, Wout])
                        k.tt("dve", tmp[:, hf * 512:(hf + 1) * 512], po[:], G2[kd][:, hf * 512:(hf + 1) * 512], ALU.mult, [po, G2[kd]], [tmp])
                    k.tt("dve", xn[:], tmp[:], xt[:], ALU.add, [tmp, xt], [xn])
                    P.dma("pool", s["XS"][t0:t0 + 128, :], xn[:], reads=[xn])
                    ss = ssr.next()
                    k.act(junk[:], xn[:], AF.Square, [xn], [junk, ss], accum=ss[:, 0:1])
                    k.act(ss[:, 1:2], ss[:, 0:1], AF.Ln, [ss], [ss], scale=1.0 / D, bias=EPS)
                    k.act(ss[:, 1:2], ss[:, 1:2], AF.Exp, [ss], [ss], scale=-0.5)
                    tmp2 = tmpr.next()
                    k.stt("dve", tmp2[:], xn[:], ss[:, 1:2], A2[kd][:], ALU.mult, ALU.mult, [xn, ss, A2[kd]], [tmp2])
                    h2 = h2r.next()
                    k.tt("dve", h2[:], tmp2[:], B2[kd][:], ALU.add, [tmp2, B2[kd]], [h2])
                    h2b = h2br.next()
                    k.copy("act", h2b[:], h2[:], [h2], [h2b])
                    P.dma("pool", s["H2"][t0:t0 + 128, :], h2b[:], reads=[h2b])
                    h2T = h2Tr.next()
                    for half in range(2):
                        p32 = ps_t32.next()
                        for kc in range(4):
                            kk = half * 4 + kc
                            k.tr(p32, p32[:, kc * 128:(kc + 1) * 128], h2[:, kk * 128:(kk + 1) * 128], ident32[:], [h2, ident32])
                        k.evac(h2T[:, half * 4:(half + 1) * 4, :], p32[:].rearrange("p (a b) -> p a b", a=4), [p32], [h2T])
                    pl = ps_o.next()
                    for kc in range(8):
                        k.mm(pl, pl[:, 0:NE], h2T[:, kc, :], rtr[:, kc, :], kc == 0, kc == 7, [h2T, rtr])
                    sm = smr.next()
                    k.P.op("dve", lambda e, sm=sm, pl=pl: e.tensor_reduce(out=sm[:, 0:1], in_=pl[:, 0:NE], axis=AX.X, op=ALU.max), reads=[pl], writes=[sm])
                    k.ts("dve", sm[:, 1:2], sm[:, 0:1], -1.0, ALU.mult, [sm], [sm])
                    aff = affr.next()
                    k.act(aff[:], pl[:, 0:NE], AF.Exp, [pl, sm], [aff, sm], bias=sm[:, 1:2], accum=sm[:, 2:3])
                    P.op("dve", lambda e, sm=sm: e.reciprocal(out=sm[:, 3:4], in_=sm[:, 2:3]), reads=[sm], writes=[sm])
                    k.ts("dve", aff[:], aff[:], sm[:, 3:4], ALU.mult, [aff, sm], [aff])
                    pa_ = ps_t32.next()
                    k.tr(pa_, pa_[0:NE, 0:128], aff[:], ident32[:], [aff, ident32])
                    aft = aftr.next()
                    k.evac(aft[:], pa_[0:NE, 0:128], [pa_], [aft])
                    P.dma("pool", s["AFFT"][:, t0:t0 + 128], aft[:], reads=[aft])
            P.barrier()
            PA.reset()
            if dbg and l == 0:
                for s in SEQS:
                    tp = dbg_tap(f"dbg_xmix_{s['name']}", [s["T"], D])
                    P.dma("sp", tp, s["XS"], )
                    tp = dbg_tap(f"dbg_afft_{s['name']}", [NE, s["T"]])
                    P.dma("sp", tp, s["AFFT"])
                    tp = dbg_tap(f"dbg_y_{s['name']}", [s["T"], D])
                    P.dma("sp", tp, s["Y"])
                    tp = dbg_tap(f"dbg_tm_{s['name']}", [s["T"], 2080])
                    P.dma("sp", tp, s["TM"])
                    tp = dbg_tap(f"dbg_fm_{s['name']}", [2048, s["T"]])
                    P.dma("sp", tp, s["FM"])
                P.barrier()

            act_seqs = [s for s in SEQS if not (s["ctx"] and last)]
            groups = [[s for s in act_seqs if not s["ctx"]]]
            if not last:
                groups.append([s for s in act_seqs if s["ctx"]])
            for grp in groups:
                T, nt = grp[0]["T"], grp[0]["nt"]
                PMtokG = PA.alloc(f"PMtokG{T}", [nt, 32], F32)
                PGg = PA.alloc(f"PGg{T}", [2, T], BF16, parts=32)
                for gi, s in enumerate(grp):
                    s["gi"], s["PMtokG"], s["PGg"] = gi, PMtokG, PGg
            mark_E = PA.off
            aff_F = PA.alloc("aff_", [SEQ], F32, parts=32)
            work_F = PA.alloc("work", [SEQ], F32, parts=32)
            onesr_F = PA.alloc("onesr", [SEQ], F32, parts=32)
            cum_F = PA.alloc("cum_", [SEQ], F32, parts=32)
            m8 = PA.alloc("m8", [8], F32, parts=32)
            k.memset("pool", onesr_F[:], 1.0, [onesr_F])
            for grp in groups:
                T, nt, cap = grp[0]["T"], grp[0]["nt"], grp[0]["cap"]
                aff_ = Tile(aff_F[:, 0:T], "aff_v")
                work = Tile(work_F[:, 0:T], "work_v")
                onesr = Tile(onesr_F[:, 0:T], "ones_v")
                cum_ = Tile(cum_F[:, 0:T], "cum_v")
                P.barrier()
                for gi, s in enumerate(grp):
                    P.dma("sp", aff_[gi * NE:(gi + 1) * NE, :], s["AFFT"], writes=[aff_])
                k.copy("dve", work[:], aff_[:], [aff_], [work])
                for it in range(cap // 8):
                    P.op("dve", lambda e, m8=m8, work=work: e.max(out=m8[:], in_=work[:]), reads=[work], writes=[m8])
                    P.op("dve", lambda e, m8=m8, work=work: e.match_replace(out=work[:], in_to_replace=m8[:], in_values=work[:], imm_value=0.0), reads=[m8, work], writes=[work])
                k.tt("dve", work[:], aff_[:], work[:], ALU.subtract, [aff_, work], [work])
                k.ts("dve", aff_[:], work[:], 0.0, ALU.is_gt, [work], [aff_])
                P.op("dve", lambda e, cum_=cum_, onesr=onesr, aff_=aff_: e.tensor_tensor_scan(out=cum_[:], data0=onesr[:], data1=aff_[:], initial=0.0, op0=ALU.mult, op1=ALU.add), reads=[onesr, aff_], writes=[cum_])
                k.tt("dve", cum_[:], cum_[:], aff_[:], ALU.mult, [cum_, aff_], [cum_])
                k.ts("dve", cum_[:], cum_[:], -1.0, ALU.add, [cum_], [cum_])
                PGg = grp[0]["PGg"]
                k.copy("dve", PGg[:, 0, :], cum_[:], [cum_], [PGg])
                k.copy("dve", PGg[:, 1, :], work[:], [work], [PGg])
                pt = PS[0]
                for ti in range(nt):
                    k.tr(pt, pt[:, ti * 32:(ti + 1) * 32], cum_[:, ti * 128:(ti + 1) * 128], ident32[0:32, 0:32], [cum_, ident32])
                k.copy("dve", grp[0]["PMtokG"][:].rearrange("p a b -> p (a b)"), pt[:, 0:nt * 32], [pt], [grp[0]["PMtokG"]])
            P.barrier()
            PA.reset(mark_E)

            NRING = 16
            wring = Ring([PA.alloc(f"wr{i}", [8, 256], BF16) for i in range(NRING)])
            h2ring = Ring([PA.alloc(f"h2t{i}", [D], BF16) for i in range(6)])
            selr = Ring([PA.alloc(f"sel{i}", [256], BF16) for i in range(6)])
            for grp in groups:
                ncap = len(grp) * grp[0]["cap"]
                xs_g = PA.alloc(f"xsT_g{ncap}", [8, ncap], BF16)
                hid_g = PA.alloc(f"hidT_g{ncap}", [8, ncap], BF16)
                for s in grp:
                    s["xsT"], s["hidT"], s["ncap"] = xs_g, hid_g, ncap
            sgr = Ring([PA.alloc(f"sg{i}", [512], F32) for i in range(2)])
            ysr = Ring([PA.alloc(f"ys{i}", [2, D], BF16) for i in range(2)])
            pieces = []
            for e_ in range(NE):
                for fb in range(4):
                    pieces.append((e_, "g", fb))
                    pieces.append((e_, "u", fb))
                for i_ in range(4):
                    pieces.append((e_, "d", i_))
            loaded = {}
            nload = [0]

            def ensure(n_upto):
                while nload[0] < min(n_upto, len(pieces)):
                    e_, kind_, i_ = pieces[nload[0]]
                    wt_ = wring.next()
                    if kind_ == "d":
                        src = moe_wd[l, e_].rearrange("(fc p) d -> p fc d", p=128)[:, 2 * i_:2 * i_ + 2, :]
                        dst = wt_[:].rearrange("p a b -> p (a b)").rearrange("p (f d) -> p f d", f=2)
                    else:
                        wsrc = moe_wg if kind_ == "g" else moe_wu
                        src = wsrc[l, e_].rearrange("(kc p) n -> p kc n", p=128)[:, :, i_ * 256:(i_ + 1) * 256]
                        dst = wt_[:]
                    P.dma("pool", dst, src, writes=[wt_])
                    loaded[pieces[nload[0]]] = wt_
                    nload[0] += 1

            ps_ga = PS[0:4]
            ps_up = Ring(PS[4:8])
            ps_dn = Ring(PS[0:4])
            pidx = 0
            for e_ in range(NE):
                for s in act_seqs:
                    nt, cap = s["nt"], s["cap"]
                    for ti in range(nt):
                        h2t = h2ring.next()
                        P.dma("sp", h2t[:], s["H2"][ti * 128:(ti + 1) * 128, :], writes=[h2t])
                        sel = selr.next()
                        k.ts("dve", sel[:, 0:cap], iota_f[:, 0:cap], s["PMtokG"][:, ti, s["gi"] * NE + e_:s["gi"] * NE + e_ + 1], ALU.is_equal, [iota_f, s["PMtokG"]], [sel])
                        for dc in range(8):
                            pg = ps_ga[dc // 2]
                            k.mm(pg, pg[:, (dc % 2) * 256:(dc % 2) * 256 + cap], h2t[:, dc * 128:(dc + 1) * 128], sel[:, 0:cap], ti == 0 and dc % 2 == 0, ti == nt - 1 and dc % 2 == 1, [h2t, sel])
                    for dc in range(8):
                        pg = ps_ga[dc // 2]
                        k.evac(s["xsT"][:, dc, s["gi"] * cap:(s["gi"] + 1) * cap], pg[:, (dc % 2) * 256:(dc % 2) * 256 + cap], [pg], [s["xsT"]])
                for fb in range(4):
                    ensure(pidx + 2 + 12)
                    wg_, wu_ = loaded[(e_, "g", fb)], loaded[(e_, "u", fb)]
                    pidx += 2
                    for grp in groups:
                        s = grp[0]
                        ncap = s["ncap"]
                        for f2 in range(2):
                            pg_ = ps_up.next()
                            for kc in range(8):
                                k.mm(pg_, pg_[:, 0:ncap], wg_[:, kc, f2 * 128:(f2 + 1) * 128], s["xsT"][:, kc, :], kc == 0, kc == 7, [wg_, s["xsT"]])
                            pu = ps_up.next()
                            for kc in range(8):
                                k.mm(pu, pu[:, 0:ncap], wu_[:, kc, f2 * 128:(f2 + 1) * 128], s["xsT"][:, kc, :], kc == 0, kc == 7, [wu_, s["xsT"]])
                            sg = sgr.next()
                            k.act(sg[:, 0:ncap], pg_[:, 0:ncap], AF.Silu, [pg_], [sg])
                            k.tt("dve", s["hidT"][:, fb * 2 + f2, :], sg[:, 0:ncap], pu[:, 0:ncap], ALU.mult, [sg, pu], [s["hidT"]])
                ensure(pidx + 4 + 12)
                wd_ = [loaded[(e_, "d", i_)] for i_ in range(4)]
                pidx += 4
                for s in act_seqs:
                    cap = s["cap"]
                    M = min(128, cap)
                    ncc = cap // M
                    ys = ysr.next()
                    for cc in range(ncc):
                        for hf in range(2):
                            pd = ps_dn.next()
                            for fc in range(8):
                                wv = wd_[fc // 2][:].rearrange("p a b -> p (a b)").rearrange("p (f d) -> p f d", f=2)
                                k.mm(pd, pd[0:M, :], s["hidT"][:, fc, s["gi"] * cap + cc * M:s["gi"] * cap + (cc + 1) * M], wv[:, fc % 2, hf * 512:(hf + 1) * 512], fc == 0, fc == 7, [s["hidT"], wd_[fc // 2]])
                            k.evac(ys[0:M, cc, hf * 512:(hf + 1) * 512], pd[0:M, :], [pd], [ys])
                    P.dma("sp", s["YE"][e_].rearrange("(cc p) d -> p cc d", p=M), ys[0:M, 0:ncc, :], reads=[ys])
            P.barrier()
            PA.reset(mark_E)

            G5 = [load_bc(f"G5_{kd}", MODS[l, 5, kd]) for kd in range(3)]
            if last:
                fng = load_bc("fng", final_norm_g)
            yer = [PA.alloc(f"yer{e_}", [2, D], BF16) for e_ in range(NE)]
            xr = Ring([PA.alloc(f"xt{i}", [D], F32) for i in range(2)])
            gmr = Ring([PA.alloc(f"gm{i}", [512], BF16) for i in range(3)])
            selT = [[PA.alloc(f"selT{e_}_{cc}", [512], BF16) for cc in range(2)] for e_ in range(NE)]
            tmpr = Ring([PA.alloc(f"tmp{i}", [D], F32) for i in range(2)])
            xnr = Ring([PA.alloc(f"xn{i}", [D], F32) for i in range(2)])
            junk = PA.alloc("junk", [D], F32)
            ssr = Ring([PA.alloc(f"ss{i}", [2], F32) for i in range(3)])
            ps_bc = Ring(PS[0:4])
            ps_oo = Ring(PS[4:8])
            for s in act_seqs:
                T, nt, cap, kd = s["T"], s["nt"], s["cap"], s["kind"]
                M = min(128, cap)
                ncc = cap // M
                NBt = min(512, T)
                for e_ in range(NE):
                    P.dma("sp", yer[e_][0:M, 0:ncc, :], s["YE"][e_].rearrange("(cc p) d -> p cc d", p=M), writes=[yer[e_]])
                for blk in range(T // NBt):
                    c0 = blk * NBt
                    for e_ in range(NE):
                        pbm = ps_bc.next()
                        k.mm(pbm, pbm[:, 0:NBt], oh32[:, s["gi"] * NE + e_, :], s["PGg"][:, 0, c0:c0 + NBt], True, True, [oh32, s["PGg"]])
                        pbg = ps_bc.next()
                        k.mm(pbg, pbg[:, 0:NBt], oh32[:, s["gi"] * NE + e_, :], s["PGg"][:, 1, c0:c0 + NBt], True, True, [oh32, s["PGg"]])
                        gm = gmr.next()
                        k.copy("act", gm[:, 0:NBt], pbg[:, 0:NBt], [pbg], [gm])
                        for cc in range(ncc):
                            st_ = selT[e_][cc]
                            k.stt("dve", st_[0:M, 0:NBt], pbm[0:M, 0:NBt], (iota_p if cc == 0 else iota_p128)[0:M, 0:1], gm[0:M, 0:NBt], ALU.is_equal, ALU.mult, [pbm, gm, iota_p, iota_p128], [st_])
                    for ti in range(NBt // 128):
                        t0 = c0 + ti * 128
                        xt = xr.next()
                        P.dma("sp", xt[:], s["XS"][t0:t0 + 128, :], writes=[xt])
                        po = [ps_oo.next(), ps_oo.next()]
                        for hf in range(2):
                            for e_ in range(NE):
                                for cc in range(ncc):
                                    k.mm(po[hf], po[hf][:], selT[e_][cc][0:M, ti * 128:(ti + 1) * 128], yer[e_][0:M, cc, hf * 512:(hf + 1) * 512], e_ == 0 and cc == 0, e_ == NE - 1 and cc == ncc - 1, [selT[e_][cc], yer[e_]])
                        tmp = tmpr.next()
                        for hf in range(2):
                            k.tt("dve", tmp[:, hf * 512:(hf + 1) * 512], po[hf][:], G5[kd][:, hf * 512:(hf + 1) * 512], ALU.mult, [po[hf], G5[kd]], [tmp])
                        xn = xnr.next()
                        k.tt("dve", xn[:], tmp[:], xt[:], ALU.add, [tmp, xt], [xn])
                        if not last:
                            P.dma("pool", s["XS"][t0:t0 + 128, :], xn[:], reads=[xn])
                        else:
                            ss = ssr.next()
                            k.act(junk[:], xn[:], AF.Square, [xn], [junk, ss], accum=ss[:, 0:1])
                            k.act(ss[:, 1:2], ss[:, 0:1], AF.Ln, [ss], [ss], scale=1.0 / D, bias=EPS)
                            k.act(ss[:, 1:2], ss[:, 1:2], AF.Exp, [ss], [ss], scale=-0.5)
                            tmp2 = tmpr.next()
                            k.stt("dve", tmp2[:], xn[:], ss[:, 1:2], fng[:], ALU.mult, ALU.mult, [xn, ss, fng], [tmp2])
                            P.dma("pool", out_ap[s["b"], t0:t0 + 128, :], tmp2[:], reads=[tmp2])
            P.barrier()
            PA.reset()
            if dbg:
                for s in SEQS:
                    if s["ctx"] and last:
                        continue
                    tp = dbg_tap(f"dbg_x{l}_{s['name']}", [s["T"], D])
                    P.dma("sp", tp, s["XS"])
                P.barrier()
            if dbg and dbg == "l0":
                break
        P.emit()
    return nc, list(dbg_outs.keys()), len(P.ops)


_CACHE = {}


def kernel(**inputs):
    if "nc" not in _CACHE:
        _CACHE["nc"] = build(False)
    nc, _, nops = _CACHE["nc"]
    f32 = lambda a: np.ascontiguousarray(np.asarray(a, dtype=np.float32))
    shared = {}
    for name in ("c_ctx", "ada_w", "ada_b", "norm_mix_g", "norm_ffn_g", "final_norm_g", "mlstm_w_in", "mlstm_conv_w",
                 "mlstm_conv_b", "mlstm_head_norm_g", "mlstm_w_out", "ret_w_in", "ret_group_norm_g", "ret_w_out",
                 "moe_router", "moe_w_gate", "moe_w_up", "moe_w_down"):
        shared[name] = f32(inputs[name])
    for name in ("mlstm_igate_b", "mlstm_fgate_b", "ret_decay_logit"):
        shared[name] = f32(inputs[name]).reshape(2, 16)
    x = f32(inputs["x"])
    c = f32(inputs["c"])
    ctx = f32(inputs["ctx"])
    in_maps = []
    for i in range(NCORES):
        m = dict(shared)
        m["x"] = x[i * BPC:(i + 1) * BPC]
        m["c"] = c[i * BPC:(i + 1) * BPC]
        m["ctx"] = ctx[i * BPC:(i + 1) * BPC]
        in_maps.append(m)
    res = run_bass_kernel_spmd(nc, in_maps, core_ids=list(range(NCORES)))
    out = np.concatenate([np.asarray(r["out"], dtype=np.float32) for r in res.results], axis=0)
    return out
```

```python
import numpy as np
from contextlib import ExitStack
import concourse.bass as bass
import concourse.mybir as mybir
from concourse.bass_utils import run_bass_kernel_spmd

F32 = mybir.dt.float32
BF16 = mybir.dt.bfloat16
I32 = mybir.dt.int32
ALU = mybir.AluOpType
AF = mybir.ActivationFunctionType
AX = mybir.AxisListType

ENGS = ("sp", "pe", "act", "dve", "pool")
NDMASEM = 24

D = 1024
SEQ = 2048
CTX = 256
DEPTH = 4
NE = 16
EPS = 1e-6
NCORES = 8
BPC = 2


class Tile:
    __slots__ = ("ap", "name", "last_w", "readers")

    def __init__(self, ap, name):
        self.ap = ap
        self.name = name
        self.last_w = None
        self.readers = []

    def __getitem__(self, k):
        return self.ap[k]


class Op:
    __slots__ = ("idx", "eng", "fn", "deps", "signal", "is_dma", "semi", "semval", "pos", "kind", "nconsumers")

    def __init__(self, idx, eng, fn, is_dma=False, kind="op"):
        self.idx = idx
        self.eng = eng
        self.fn = fn
        self.deps = []
        self.signal = False
        self.is_dma = is_dma
        self.semi = None
        self.semval = None
        self.pos = None
        self.kind = kind
        self.nconsumers = 0


class Prog:
    def __init__(self, nc):
        self.nc = nc
        self.ops = []
        self.streams = {e: [] for e in ENGS}
        self.waited = {e: {f: -1 for f in ENGS} for e in ENGS}
        self.waited_dma = {e: {} for e in ENGS}
        self.dma_rr = 0
        self.dma_last = [None] * NDMASEM
        self.dma_count = [0] * NDMASEM
        self.unconsumed_dma = []
        self.last_op = {e: None for e in ENGS}
        self.nbar = 0

    def _add_dep(self, op, prod):
        if prod is None or prod is op:
            return
        e = op.eng
        if prod.is_dma:
            w = self.waited_dma[e].get(prod.semi, 0)
            if w >= prod.semval:
                return
            self.waited_dma[e][prod.semi] = prod.semval
            op.deps.append(prod)
            prod.nconsumers += 1
        else:
            f = prod.eng
            if e == "pe" and f == "pe":
                return
            if self.waited[e][f] >= prod.pos:
                return
            self.waited[e][f] = prod.pos
            prod.signal = True
            op.deps.append(prod)

    def _track(self, op, reads, writes):
        for t in reads:
            self._add_dep(op, t.last_w)
        for t in writes:
            self._add_dep(op, t.last_w)
            for r in t.readers:
                self._add_dep(op, r)
        for t in reads:
            t.readers.append(op)
        for t in writes:
            t.last_w = op
            t.readers = []

    def _push(self, op):
        op.pos = len(self.streams[op.eng])
        self.streams[op.eng].append(op)
        self.ops.append(op)
        if not op.is_dma:
            self.last_op[op.eng] = op

    def op(self, eng, fn, reads=(), writes=()):
        o = Op(len(self.ops), eng, fn)
        self._push(o)
        self._track(o, reads, writes)
        return o

    def dma(self, queue, out_ap, in_ap, reads=(), writes=(), **kw):
        o = Op(len(self.ops), queue, None, is_dma=True)
        k = self.dma_rr
        self.dma_rr = (self.dma_rr + 1) % NDMASEM
        o.semi = k
        self.dma_count[k] += 1
        o.semval = 16 * self.dma_count[k]
        o.fn = lambda eng: eng.dma_start(out=out_ap, in_=in_ap, **kw)
        self._push(o)
        prev = self.dma_last[k]
        if prev is not None:
            self._add_dep(o, prev)
        self.dma_last[k] = o
        self._track(o, reads, writes)
        self.unconsumed_dma.append(o)
        return o

    def barrier(self):
        self.nbar += 1
        n = self.nbar
        b = Op(len(self.ops), "sp", None, kind="bar_inc")
        b.semval = n
        self._push(b)
        for e in ENGS:
            if e != "sp":
                self._add_dep(b, self.last_op[e])
        for d in self.unconsumed_dma:
            self._add_dep(b, d)
        self.unconsumed_dma = []
        for e in ENGS:
            if e == "sp":
                continue
            w = Op(len(self.ops), e, None, kind="bar_wait")
            w.semval = n
            self._push(w)

    def emit(self):
        nc = self.nc
        with ExitStack() as es:
            esem = {e: es.enter_context(nc.semaphore(f"es_{e}")) for e in ENGS}
            dsem = [es.enter_context(nc.semaphore(f"ds_{i}")) for i in range(NDMASEM)]
            bsem = es.enter_context(nc.semaphore("barsem"))
            for e in ENGS:
                c = 0
                for o in self.streams[e]:
                    if o.is_dma or o.kind != "op":
                        continue
                    if o.signal:
                        c += 1
                        o.semval = c
                        o.semi = e
            block = es.enter_context(nc.Block())

            def run(e, eng):
                for o in self.streams[e]:
                    for p in o.deps:
                        if p.is_dma:
                            eng.wait_ge(dsem[p.semi], p.semval)
                        else:
                            eng.wait_ge(esem[p.eng], p.semval)
                    if o.kind == "bar_inc":
                        eng.sem_inc(bsem, 1)
                        continue
                    if o.kind == "bar_wait":
                        eng.wait_ge(bsem, o.semval)
                        continue
                    ins = o.fn(eng)
                    if o.is_dma:
                        ins.then_inc(dsem[o.semi], 16)
                    elif o.signal:
                        ins.then_inc(esem[e], 1)

            @block.sync
            def _(eng):
                run("sp", eng)

            @block.tensor
            def _(eng):
                run("pe", eng)

            @block.scalar
            def _(eng):
                run("act", eng)

            @block.vector
            def _(eng):
                run("dve", eng)

            @block.gpsimd
            def _(eng):
                run("pool", eng)


class Arena:
    def __init__(self, t, ncols):
        self.t = t
        self.n = ncols
        self.off = 0

    def alloc(self, name, free, dt, parts=128):
        free = list(free)
        n = int(np.prod(free))
        cols = n * (2 if dt in (F32, I32) else 1)
        cols = (cols + 1) // 2 * 2
        assert self.off + cols <= self.n, f"arena overflow {name} {self.off}+{cols}>{self.n}"
        ap = self.t[0:parts, self.off:self.off + cols]
        self.off += cols
        if dt != BF16:
            ap = ap.bitcast(dt)
        if n * (2 if dt in (F32, I32) else 1) != cols:
            ap = ap[:, 0:n]
        if len(free) == 2:
            ap = ap.rearrange("p (a b) -> p a b", a=free[0])
        elif len(free) == 3:
            ap = ap.rearrange("p (a b c) -> p a b c", a=free[0], b=free[1])
        return Tile(ap, name)

    def reset(self, to=0):
        self.off = to


class Ring:
    def __init__(self, tiles):
        self.tiles = tiles
        self.i = 0

    def next(self):
        t = self.tiles[self.i % len(self.tiles)]
        self.i += 1
        return t


class K:
    def __init__(self, P):
        self.P = P
        self.flip = 0

    def mm(self, ps, out, lhsT, rhs, start, stop, rd):
        self.P.op("pe", lambda e: e.matmul(out, lhsT=lhsT, rhs=rhs, start=start, stop=stop), reads=rd, writes=[ps])

    def tr(self, ps, out, in_, ident, rd):
        self.P.op("pe", lambda e: e.transpose(out, in_, ident), reads=rd, writes=[ps])

    def act(self, out, in_, func, rd, wr, bias=None, scale=None, accum=None):
        kw = {}
        if bias is not None:
            kw["bias"] = bias
        if scale is not None:
            kw["scale"] = scale
        if accum is not None:
            kw["accum_out"] = accum
        self.P.op("act", lambda e: e.activation(out=out, in_=in_, func=func, **kw), reads=rd, writes=wr)

    def tt(self, eng, out, in0, in1, op, rd, wr):
        self.P.op(eng, lambda e: e.tensor_tensor(out=out, in0=in0, in1=in1, op=op), reads=rd, writes=wr)

    def ts(self, eng, out, in0, s1, op0, rd, wr, s2=None, op1=None, accum=None):
        kw = {}
        if op1 is not None:
            kw["op1"] = op1
        if accum is not None:
            kw["accum_out"] = accum
        self.P.op(eng, lambda e: e.tensor_scalar(out=out, in0=in0, scalar1=s1, scalar2=s2, op0=op0, **kw), reads=rd, writes=wr)

    def stt(self, eng, out, in0, scalar, in1, op0, op1, rd, wr):
        self.P.op("dve", lambda e: e.scalar_tensor_tensor(out=out, in0=in0, scalar=scalar, in1=in1, op0=op0, op1=op1), reads=rd, writes=wr)

    def copy(self, eng, out, in_, rd, wr):
        if eng == "act":
            self.P.op("act", lambda e: e.copy(out=out, in_=in_), reads=rd, writes=wr)
        else:
            self.P.op(eng, lambda e: e.tensor_copy(out=out, in_=in_), reads=rd, writes=wr)

    def evac(self, out, in_, rd, wr):
        self.flip ^= 1
        self.copy("act" if self.flip else "dve", out, in_, rd, wr)

    def memset(self, eng, ap, val, wr):
        self.P.op(eng, lambda e: e.memset(ap, val), writes=wr)


def build(dbg=False):
    nc = bass.Bass("TRN2", target_bir_lowering=False)

    def din(name, shape):
        return nc.dram_tensor(name, list(shape), F32, kind="ExternalInput").ap()

    x_in = din("x", [BPC, SEQ, D])
    c_in = din("c", [BPC, D])
    ctx_in = din("ctx", [BPC, CTX, D])
    cctx_in = din("c_ctx", [D])
    ada_w = din("ada_w", [DEPTH, D, 6 * D])
    ada_b = din("ada_b", [DEPTH, 6 * D])
    norm_mix_g = din("norm_mix_g", [DEPTH, D])
    norm_ffn_g = din("norm_ffn_g", [DEPTH, D])
    final_norm_g = din("final_norm_g", [D])
    ml_w_in = din("mlstm_w_in", [2, D, 3104])
    ml_conv_w = din("mlstm_conv_w", [2, 5, 1024])
    ml_conv_b = din("mlstm_conv_b", [2, 1024])
    ml_ig_b = din("mlstm_igate_b", [2, 16])
    ml_fg_b = din("mlstm_fgate_b", [2, 16])
    ml_hn_g = din("mlstm_head_norm_g", [2, 1024])
    ml_w_out = din("mlstm_w_out", [2, 1024, D])
    rt_w_in = din("ret_w_in", [2, D, 4096])
    rt_decay = din("ret_decay_logit", [2, 16])
    rt_gn_g = din("ret_group_norm_g", [2, 1024])
    rt_w_out = din("ret_w_out", [2, 1024, D])
    moe_router = din("moe_router", [DEPTH, D, NE])
    moe_wg = din("moe_w_gate", [DEPTH, NE, D, D])
    moe_wu = din("moe_w_up", [DEPTH, NE, D, D])
    moe_wd = din("moe_w_down", [DEPTH, NE, D, D])
    out_ap = nc.dram_tensor("out", [BPC, SEQ, D], F32, kind="ExternalOutput").ap()

    def scr(name, shape, dt=F32):
        return nc.dram_tensor(name, list(shape), dt, kind="Internal").ap()

    SEQS = []
    for b in range(BPC):
        SEQS.append(dict(name=f"c{b}", b=b, ctx=True, T=CTX, kind=2, xin=ctx_in[b]))
        SEQS.append(dict(name=f"l{b}", b=b, ctx=False, T=SEQ, kind=b, xin=x_in[b]))
    for s in SEQS:
        T = s["T"]
        n = s["name"]
        s["XS"] = scr(f"XS_{n}", [T, D])
        s["FM"] = scr(f"FM_{n}", [2048, T])
        s["TM"] = scr(f"TM_{n}", [T, 2080])
        s["QK"] = scr(f"QK_{n}", [2048, T], BF16)
        s["KT"] = scr(f"KT_{n}", [T, 1024], BF16)
        s["Y"] = scr(f"Y_{n}", [T, D])
        s["H2"] = scr(f"H2_{n}", [T, D], BF16)
        s["AFFT"] = scr(f"AFFT_{n}", [NE, T])
        s["YE"] = scr(f"YE_{n}", [NE, T // 8, D], BF16)
        s["nt"] = T // 128
        s["cap"] = T // 8
    MODS = scr("MODS", [DEPTH, 6, 3, D])

    dbg_outs = {}

    def dbg_tap(name, shape):
        dbg_outs[name] = nc.dram_tensor(name, list(shape), F32, kind="ExternalOutput").ap()
        return dbg_outs[name]

    P = Prog(nc)
    k = K(P)
    with ExitStack() as es:
        CA_COLS = 20 * 1024
        PA_COLS = 83 * 1024
        ca_t = es.enter_context(nc.sbuf_tensor("carena", [128, CA_COLS], BF16))
        pa_t = es.enter_context(nc.sbuf_tensor("parena", [128, PA_COLS], BF16))
        CA = Arena(ca_t, CA_COLS)
        PA = Arena(pa_t, PA_COLS)
        PS = [Tile(es.enter_context(nc.psum_tensor(f"psb{i}", [128, 512], F32)), f"psb{i}") for i in range(8)]

        def ps_bf(t):
            return t.ap.bitcast(BF16)

        iota_p = CA.alloc("iota_p", [1], F32)
        iota_p128 = CA.alloc("iota_p128", [1], F32)
        iota_f = CA.alloc("iota_f", [2048], F32)
        ident32 = CA.alloc("ident32", [128], F32)
        identbf = CA.alloc("identbf", [128], BF16)
        Mfwd = CA.alloc("Mfwd", [128], F32)
        Mbwd = CA.alloc("Mbwd", [128], F32)
        ones32 = CA.alloc("ones32", [128], F32)
        oh32 = CA.alloc("oh32", [32, 128], BF16, parts=32)
        Psw = CA.alloc("Psw", [128], F32)
        cosT = CA.alloc("cosT", [2048], F32)
        sinT = CA.alloc("sinT", [2048], F32)
        P.op("pool", lambda e: e.iota(iota_p[:], pattern=[[0, 1]], base=0, channel_multiplier=1, allow_small_or_imprecise_dtypes=True), writes=[iota_p])
        P.op("pool", lambda e: e.iota(iota_p128[:], pattern=[[0, 1]], base=128, channel_multiplier=1, allow_small_or_imprecise_dtypes=True), writes=[iota_p128])
        P.op("pool", lambda e: e.iota(iota_f[:], pattern=[[1, 2048]], base=0, channel_multiplier=0, allow_small_or_imprecise_dtypes=True), writes=[iota_f])
        k.ts("dve", ident32[:], iota_f[:, 0:128], iota_p[:, 0:1], ALU.is_equal, [iota_f, iota_p], [ident32])
        k.copy("dve", identbf[:], ident32[:], [ident32], [identbf])
        k.ts("dve", Mfwd[:], iota_f[:, 0:128], iota_p[:, 0:1], ALU.is_ge, [iota_f, iota_p], [Mfwd])
        k.ts("dve", Mbwd[:], iota_f[:, 0:128], iota_p[:, 0:1], ALU.is_le, [iota_f, iota_p], [Mbwd])
        k.memset("dve", ones32[:], 1.0, [ones32])
        ohf = PA.alloc("ohf", [32, 128], F32, parts=32)
        for e_ in range(32):
            k.ts("dve", ohf[:, e_, :], ones32[0:32, :], float(e_), ALU.mult, [ones32], [ohf])
        k.ts("dve", oh32[:].rearrange("p a b -> p (a b)"), ohf[:].rearrange("p a b -> p (a b)"), iota_p[0:32, 0:1], ALU.is_equal, [ohf, iota_p], [oh32])
        tA = PA.alloc("tA", [128], F32)
        tB = PA.alloc("tB", [128], F32)
        tC = PA.alloc("tC", [128], F32)
        pm32 = PA.alloc("pm32", [2], F32)
        k.ts("dve", pm32[:, 0:1], iota_p[:, 0:1], -32.0, ALU.add, [iota_p], [pm32])
        k.ts("dve", pm32[:, 1:2], iota_p[:, 0:1], 32.0, ALU.add, [iota_p], [pm32])
        k.ts("dve", tA[:], iota_f[:, 0:128], pm32[:, 0:1], ALU.is_equal, [iota_f, pm32], [tA])
        k.ts("dve", tB[:], iota_f[:, 0:128], pm32[:, 1:2], ALU.is_equal, [iota_f, pm32], [tB])
        tD = PA.alloc("tD", [128], F32)
        k.ts("dve", tC[:], iota_f[:, 0:128], 64.0, ALU.is_ge, [iota_f], [tC], s2=None)
        k.ts("dve", tD[:], iota_f[:, 0:128], 96.0, ALU.is_lt, [iota_f], [tD])
        k.tt("dve", tC[:], tC[:], tD[:], ALU.mult, [tC, tD], [tC])
        k.ts("dve", tD[:], iota_f[:, 0:128], 32.0, ALU.is_lt, [iota_f], [tD])
        k.tt("dve", tC[:], tC[:], tD[:], ALU.add, [tC, tD], [tC])
        k.tt("dve", tA[:], tA[:], tC[:], ALU.mult, [tA, tC], [tA])
        k.ts("dve", tC[:], tC[:], -1.0, ALU.mult, [tC], [tC], s2=1.0, op1=ALU.add)
        k.tt("dve", tB[:], tB[:], tC[:], ALU.mult, [tB, tC], [tB])
        k.tt("dve", Psw[:], tB[:], tA[:], ALU.subtract, [tA, tB], [Psw])
        inv = PA.alloc("inv", [1], F32)
        posT = PA.alloc("posT", [2048], F32)
        ang = PA.alloc("ang", [2048], F32)
        inv2 = PA.alloc("inv2", [1], F32)
        k.copy("dve", inv[:], iota_p[:, 0:1], [iota_p], [inv])
        for thr in (32.0, 64.0, 96.0):
            k.ts("dve", inv2[:], iota_p[:, 0:1], thr, ALU.is_ge, [iota_p], [inv2], s2=-32.0, op1=ALU.mult)
            k.tt("dve", inv[:], inv[:], inv2[:], ALU.add, [inv, inv2], [inv])
        k.act(inv[:], inv[:], AF.Exp, [inv], [inv], scale=-float(np.log(10000.0) / 32.0))
        P.op("pool", lambda e: e.iota(posT[64:128, :], pattern=[[0, 32], [1, 64]], base=0, channel_multiplier=0, allow_small_or_imprecise_dtypes=True), writes=[posT])
        P.op("pool", lambda e: e.iota(posT[0:64, :], pattern=[[1, 32], [0, 64]], base=0, channel_multiplier=0, allow_small_or_imprecise_dtypes=True), writes=[posT])
        k.ts("dve", ang[:], posT[:], inv[:, 0:1], ALU.mult, [posT, inv], [ang])
        PI = float(np.pi)
        red = PA.alloc("red", [2048], F32)
        cmpt = PA.alloc("cmpt", [2048], F32)
        for (dst, shift) in ((sinT, 0.0), (cosT, 0.5 * PI)):
            k.ts("dve", red[:], ang[:], shift, ALU.add, [ang], [red])
            for kk in range(1, 12):
                k.ts("dve", cmpt[:], ang[:], 2 * PI * kk - PI - shift, ALU.is_ge, [ang], [cmpt], s2=-2 * PI, op1=ALU.mult)
                k.tt("dve", red[:], red[:], cmpt[:], ALU.add, [red, cmpt], [red])
            k.ts("dve", red[:], red[:], -3.141592, ALU.max, [red], [red], s2=3.141592, op1=ALU.min)
            k.act(dst[:], red[:], AF.Sin, [red], [dst])

        cT = CA.alloc("cT", [8, 3], F32)
        for b in range(BPC):
            P.dma("sp", cT[:, :, b], c_in[b].rearrange("(kc p) -> p kc", p=128), writes=[cT], allow_slow_non_contiguous=True)
        P.dma("sp", cT[:, :, 2], cctx_in.rearrange("(kc p) -> p kc", p=128), writes=[cT], allow_slow_non_contiguous=True)
        k.act(cT[:], cT[:], AF.Silu, [cT], [cT])

        def ada_alloc():
            return dict(awr=Ring([PA.alloc(f"aw{i}", [8, 512], F32) for i in range(2)]),
                        abr=Ring([PA.alloc(f"ab{i}", [512], F32, parts=3) for i in range(2)]),
                        gr=Ring([PA.alloc(f"gg{i}", [512], F32, parts=3) for i in range(2)]),
                        mrow=Ring([PA.alloc(f"mrow{i}", [512], F32, parts=3) for i in range(3)]))

        def ada_block(R_, l_, nb, psr):
            m, hf = nb // 2, nb % 2
            aw = R_["awr"].next()
            P.dma("sp", aw[:], ada_w[l_].rearrange("(kc p) n -> p kc n", p=128)[:, :, nb * 512:(nb + 1) * 512], writes=[aw])
            ab = R_["abr"].next()
            P.dma("sp", ab[:], ada_b[l_, nb * 512:(nb + 1) * 512].partition_broadcast(3), writes=[ab])
            ps = psr.next()
            for kc in range(8):
                k.mm(ps, ps[0:3, :], cT[:, kc, :], aw[:, kc, :], kc == 0, kc == 7, [cT, aw])
            mr = R_["mrow"].next()
            k.tt("dve", mr[:], ps[0:3, :], ab[:], ALU.add, [ps, ab], [mr])
            if m in (1, 4):
                g = R_["gr"].next()
                gsrc = norm_mix_g if m == 1 else norm_ffn_g
                P.dma("sp", g[:], gsrc[l_, hf * 512:(hf + 1) * 512].partition_broadcast(3), writes=[g])
                k.stt("dve", mr[:], mr[:], 1.0, g[:], ALU.add, ALU.mult, [mr, g], [mr])
            slot = {1: 0, 0: 1, 2: 2, 4: 3, 3: 4, 5: 5}[m]
            P.dma("pool", MODS[l_, slot, :, hf * 512:(hf + 1) * 512], mr[:], reads=[mr])

        R0 = ada_alloc()
        psr0 = Ring(PS[0:2])
        for nb in range(12):
            ada_block(R0, 0, nb, psr0)
        P.barrier()
        PA.reset()

        def load_bc(name, src_row):
            t = PA.alloc(name, [D], F32)
            P.dma("sp", t[:], src_row.partition_broadcast(128), writes=[t])
            return t

        def rstd_from_ssq(ssq, n, scr2):
            raise NotImplementedError

        for l in range(DEPTH):
            last = l == DEPTH - 1
            is_ml = (l % 2 == 0)
            j = l // 2
            if is_ml:
                w_in, NP, NFM, NHDK = ml_w_in[j], 3104, 8, 512
                w_out, hn_g = ml_w_out[j], ml_hn_g[j]
                dk, VC0, OC0 = 64, 0, 1024
            else:
                w_in, NP, NFM, NHDK = rt_w_in[j], 4096, 16, 1024
                w_out, hn_g = rt_w_out[j], rt_gn_g[j]
                dk, VC0, OC0 = 128, 0, 1024
            NTM = NP - NFM * 128

            def xsrc(s):
                return s["xin"] if l == 0 else s["XS"]

            Win = PA.alloc("Win", [8, NP], BF16)
            for kc in range(8):
                P.dma("pool", Win[:, kc, :], w_in[kc * 128:(kc + 1) * 128, :], writes=[Win])
            A1 = [load_bc(f"A1_{kd}", MODS[l, 0, kd]) for kd in range(3)]
            B1 = [load_bc(f"B1_{kd}", MODS[l, 1, kd]) for kd in range(3)]
            xr = Ring([PA.alloc(f"xt{i}", [D], F32) for i in range(3)])
            junk = PA.alloc("junk", [D], F32)
            ssr = Ring([PA.alloc(f"ss{i}", [2], F32) for i in range(3)])
            tmpr = Ring([PA.alloc(f"tmp{i}", [D], F32) for i in range(2)])
            hbr = Ring([PA.alloc(f"hb{i}", [D], BF16) for i in range(2)])
            hTr = Ring([PA.alloc(f"hT{i}", [8, 512], BF16) for i in range(2)])
            fmr = Ring([PA.alloc(f"fms{i}", [512], F32) for i in range(3)])
            tmr = Ring([PA.alloc(f"tms{i}", [NTM], F32) for i in range(2)])
            ps_tr = Ring(PS[0:2])
            ps_mm = Ring(PS[2:8])
            for s in SEQS:
                T, kd = s["T"], s["kind"]
                NB = min(512, T)
                for blk in range(T // NB):
                    hT = hTr.next()
                    for ti in range(NB // 128):
                        t0 = blk * NB + ti * 128
                        xt = xr.next()
                        P.dma("sp", xt[:], xsrc(s)[t0:t0 + 128, :], writes=[xt])
                        ss = ssr.next()
                        k.act(junk[:], xt[:], AF.Square, [xt], [junk, ss], accum=ss[:, 0:1])
                        k.act(ss[:, 1:2], ss[:, 0:1], AF.Ln, [ss], [ss], scale=1.0 / D, bias=EPS)
                        k.act(ss[:, 1:2], ss[:, 1:2], AF.Exp, [ss], [ss], scale=-0.5)
                        tmp = tmpr.next()
                        k.stt("dve", tmp[:], xt[:], ss[:, 1:2], A1[kd][:], ALU.mult, ALU.mult, [xt, ss, A1[kd]], [tmp])
                        hb = hbr.next()
                        k.tt("dve", hb[:], tmp[:], B1[kd][:], ALU.add, [tmp, B1[kd]], [hb])
                        pt = ps_tr.next()
                        for kc in range(8):
                            k.tr(pt, ps_bf(pt)[:, kc * 128:(kc + 1) * 128], hb[:, kc * 128:(kc + 1) * 128], identbf[:], [hb, identbf])
                        k.evac(hT[:, :, ti * 128:(ti + 1) * 128], ps_bf(pt).rearrange("p (a b) -> p a b", a=8), [pt], [hT])
                    for fc in range(NFM):
                        ps = ps_mm.next()
                        for kc in range(8):
                            k.mm(ps, ps[:, 0:NB], Win[:, kc, fc * 128:(fc + 1) * 128], hT[:, kc, 0:NB], kc == 0, kc == 7, [Win, hT])
                        st = fmr.next()
                        k.evac(st[:, 0:NB], ps[:, 0:NB], [ps], [st])
                        P.dma("pool", s["FM"][fc * 128:(fc + 1) * 128, blk * NB:(blk + 1) * NB], st[:, 0:NB], reads=[st])
                    for ti in range(NB // 128):
                        t0 = blk * NB + ti * 128
                        st = tmr.next()
                        c0 = 0
                        while c0 < NTM:
                            cw = min(512, NTM - c0)
                            ps = ps_mm.next()
                            for kc in range(8):
                                k.mm(ps, ps[:, 0:cw], hT[:, kc, ti * 128:(ti + 1) * 128], Win[:, kc, NFM * 128 + c0:NFM * 128 + c0 + cw], kc == 0, kc == 7, [Win, hT])
                            k.evac(st[:, c0:c0 + cw], ps[:, 0:cw], [ps], [st])
                            c0 += cw
                        P.dma("pool", s["TM"][t0:t0 + 128, 0:NTM], st[:], reads=[st])
            P.barrier()
            PA.reset()

            QOFF, KOFF = 0, NHDK
            if is_ml:
                cwt = PA.alloc("cwt", [8, 5], F32)
                cbt = PA.alloc("cbt", [8], F32)
                for jj in range(5):
                    P.dma("sp", cwt[:, :, jj], ml_conv_w[j, jj].rearrange("(fc p) -> p fc", p=128), writes=[cwt], allow_slow_non_contiguous=True)
                P.dma("sp", cbt[:], ml_conv_b[j].rearrange("(fc p) -> p fc", p=128), writes=[cbt], allow_slow_non_contiguous=True)
            xpr = {T_: Ring([PA.alloc(f"xp{T_}_{i}", [T_ + 4], F32) for i in range(2)]) for T_ in (CTX, SEQ)}
            for T_ in (CTX, SEQ):
                for t_ in xpr[T_].tiles:
                    k.memset("pool", t_[:, 0:2], 0.0, [t_])
                    k.memset("pool", t_[:, T_ + 2:T_ + 4], 0.0, [t_])
            accr = Ring([PA.alloc(f"acc{i}", [SEQ], F32) for i in range(2)])
            t2r = Ring([PA.alloc(f"t2{i}", [SEQ], F32) for i in range(2)])
            qbr = Ring([PA.alloc(f"qb{i}", [SEQ], BF16) for i in range(3)])
            ktr = Ring([PA.alloc(f"kts{i}", [16, 128], BF16) for i in range(2)])
            ps_tr = Ring(PS[0:2])
            ps_sw = Ring(PS[2:6])
            kscale = float(128.0 ** -0.5)
            ada_todo = []
            if l + 1 < DEPTH:
                R_ada = ada_alloc()
                ps_ada = Ring(PS[6:8])
                ada_todo = list(range(12))
            for s in SEQS:
                T, nt = s["T"], s["nt"]
                for fc in range(NFM):
                    if ada_todo:
                        ada_block(R_ada, l + 1, ada_todo.pop(0), ps_ada)
                    is_k = fc >= NFM // 2
                    xp = xpr[T].next()
                    P.dma("sp", xp[:, 2:T + 2], s["FM"][fc * 128:(fc + 1) * 128, 0:T], writes=[xp])
                    qb = qbr.next()
                    if is_ml:
                        acc = accr.next()
                        k.ts("dve", acc[:, 0:T], xp[:, 0:T], cwt[:, fc, 0:1], ALU.mult, [xp, cwt, cbt], [acc], s2=cbt[:, fc:fc + 1], op1=ALU.add)
                        for jj in range(1, 5):
                            eng = "pool" if jj in (2, 4) else "dve"
                            k.stt(eng, acc[:, 0:T], xp[:, jj:jj + T], cwt[:, fc, jj:jj + 1], acc[:, 0:T], ALU.mult, ALU.add, [xp, cwt, acc], [acc])
                        if is_k:
                            k.act(qb[:, 0:T], acc[:, 0:T], AF.Silu, [acc], [qb])
                        else:
                            t2 = t2r.next()
                            k.act(t2[:, 0:T], acc[:, 0:T], AF.Silu, [acc], [t2])
                            k.ts("dve", qb[:, 0:T], t2[:, 0:T], 0.125, ALU.mult, [t2], [qb])
                    else:
                        sc_ = kscale if is_k else 1.0
                        if s["ctx"]:
                            k.ts("dve", qb[:, 0:T], xp[:, 2:T + 2], sc_, ALU.mult, [xp], [qb])
                        else:
                            acc = accr.next()
                            t2 = t2r.next()
                            for b4 in range(T // 512):
                                ps = ps_sw.next()
                                k.mm(ps, ps[:], Psw[:], xp[:, 2 + b4 * 512:2 + (b4 + 1) * 512], True, True, [Psw, xp])
                                k.stt("dve", t2[:, b4 * 512:(b4 + 1) * 512], ps[:], sc_, sinT[:, b4 * 512:(b4 + 1) * 512], ALU.mult, ALU.mult, [ps, sinT], [t2])
                            k.stt("pool", acc[:, 0:T], xp[:, 2:T + 2], sc_, cosT[:, 0:T], ALU.mult, ALU.mult, [xp, cosT], [acc])
                            k.tt("dve", qb[:, 0:T], acc[:, 0:T], t2[:, 0:T], ALU.add, [acc, t2], [qb])
                    P.dma("pool", s["QK"][fc * 128:(fc + 1) * 128, 0:T], qb[:, 0:T], reads=[qb])
                    if is_k:
                        kt = ktr.next()
                        for g0 in range(0, nt, 8):
                            pt = ps_tr.next()
                            ng = min(8, nt - g0)
                            for ti in range(ng):
                                k.tr(pt, ps_bf(pt)[:, ti * 128:(ti + 1) * 128], qb[:, (g0 + ti) * 128:(g0 + ti + 1) * 128], identbf[:], [qb, identbf])
                            k.evac(kt[:, g0:g0 + ng, :], ps_bf(pt)[:, 0:ng * 128].rearrange("p (a b) -> p a b", a=ng), [pt], [kt])
                        kc0 = (fc - NFM // 2) * 128
                        P.dma("pool", s["KT"].rearrange("(n p) f -> p n f", p=128)[:, :, kc0:kc0 + 128], kt[:, 0:nt, :], reads=[kt])
            P.barrier()
            PA.reset()

            NTT = 18
            NS = NTT - 1
            ORD = [list(range(NTT)), [1, 0] + list(range(17, 1, -1))]
            gbias = PA.alloc("gbias", [32], F32)
            if is_ml:
                P.dma("sp", gbias[:, 0:16], ml_ig_b[j].partition_broadcast(128), writes=[gbias])
                P.dma("sp", gbias[:, 16:32], ml_fg_b[j].partition_broadcast(128), writes=[gbias])
            else:
                P.dma("sp", gbias[:, 16:32], rt_decay[j].partition_broadcast(128), writes=[gbias])
            Gt = PA.alloc("Gt", [NTT, 32], F32)
            igb = PA.alloc("igb", [NTT, 16], F32)
            spt = PA.alloc("spt", [NTT, 16], F32)
            chf = PA.alloc("chf", [NTT, 16], F32)
            gsets = [dict(wt=PA.alloc(f"wt{i}", [NTT, 16], F32), rowf=PA.alloc(f"rowf{i}", [NTT, 16], F32),
                          fS=[PA.alloc(f"fS{i}_{d}", [8, NTT + 1], F32) for d in range(2)]) for i in range(2)]
            hdr = [dict(
                qT=PA.alloc(f"qT{i}", [NTT * 128], BF16),
                kT=PA.alloc(f"kT{i}", [NTT * 128], BF16),
                ktok=PA.alloc(f"ktok{i}", [NTT, dk], BF16),
                vaug=PA.alloc(f"vaug{i}", [NTT, 129], BF16),
                vp=[PA.alloc(f"vp{i}_{d}", [NTT, 129], BF16) for d in range(2)],
                atm=[PA.alloc(f"atm{i}_{d}", [NTT, 128], BF16) for d in range(2)],
                cbf=[PA.alloc(f"cbf{i}_{d}", [NS, 129], BF16) for d in range(2)],
                Y=PA.alloc(f"Y{i}", [NTT, 128], F32),
            ) for i in range(2)]
            for hb_ in hdr:
                k.memset("pool", hb_["vaug"][:, :, 128:129], 1.0, [hb_["vaug"]])
            dCall = [PA.alloc(f"dCall{d}", [129, NS], F32) for d in range(2)]
            Fb = [PA.alloc(f"Fb{d}", [129, NS], F32) for d in range(2)]
            ats = Ring([PA.alloc(f"ats{i}", [128], F32) for i in range(3)])
            resr = Ring([PA.alloc(f"res{i}", [129], F32) for i in range(6)])
            ddr = Ring([PA.alloc(f"dd{i}", [2], F32) for i in range(6)])
            ps_g = PS[0]
            ps_at = Ring(PS[1:3])
            ps_dc = Ring(PS[3:5])
            ps_out = Ring(PS[5:8])

            def gate_prep(b, GS):
                sc, sl = SEQS[2 * b], SEQS[2 * b + 1]
                parts = [(sc, 0, 2), (sl, 2, 16)]
                wt, rowf, fS = GS["wt"], GS["rowf"], GS["fS"]
                if is_ml:
                    for (s, o0, n_) in parts:
                        P.dma("sp", Gt[:, o0:o0 + n_, :], s["TM"].rearrange("(n p) c -> p n c", p=128)[:, :, 2048:2080], writes=[Gt])
                    gb_ig = gbias[:, 0:16].unsqueeze(1).to_broadcast([128, NTT, 16])
                    gb_fg = gbias[:, 16:32].unsqueeze(1).to_broadcast([128, NTT, 16])
                    k.tt("dve", igb[:], Gt[:, :, 0:16], gb_ig, ALU.add, [Gt, gbias], [igb])
                    k.tt("dve", spt[:], Gt[:, :, 16:32], gb_fg, ALU.add, [Gt, gbias], [spt])
                else:
                    k.memset("dve", igb[:], 0.0, [igb])
                    k.copy("dve", spt[:], gbias[:, 16:32].unsqueeze(1).to_broadcast([128, NTT, 16]), [gbias], [spt])
                k.act(spt[:], spt[:], AF.Exp, [spt], [spt], scale=-1.0)
                k.act(spt[:], spt[:], AF.Ln, [spt], [spt], bias=1.0)
                for t_ in range(NTT):
                    k.mm(ps_g, ps_g[:, t_ * 16:t_ * 16 + 8], Mfwd[:], spt[:, t_, 0:8], True, True, [Mfwd, spt])
                    k.mm(ps_g, ps_g[:, t_ * 16 + 8:t_ * 16 + 16], Mbwd[:], spt[:, t_, 8:16], True, True, [Mbwd, spt])
                pt_ = ps_at.next()
                k.mm(pt_, pt_[:, 0:NTT * 16], ones32[:], spt[:].rearrange("p a b -> p (a b)"), True, True, [ones32, spt])
                cum = ps_g[:, 0:NTT * 16]
                tot = pt_[:, 0:NTT * 16]
                k.tt("dve", wt[:].rearrange("p a b -> p (a b)"), igb[:].rearrange("p a b -> p (a b)"), cum, ALU.add, [igb, ps_g], [wt])
                k.act(wt[:], wt[:], AF.Exp, [wt], [wt])
                k.act(rowf[:].rearrange("p a b -> p (a b)"), cum, AF.Exp, [ps_g], [rowf], scale=-1.0)
                k.act(chf[:].rearrange("p a b -> p (a b)"), tot, AF.Exp, [pt_], [chf], scale=-1.0)
                for d in range(2):
                    k.memset("pool", fS[d][:, :, 0:1], 0.0, [fS[d]])
                k.copy("pool", fS[0][:, :, 1:NTT + 1], chf[:, :, 0:8].rearrange("p t h -> p h t"), [chf], [fS[0]])
                for s_ in range(NTT):
                    k.copy("pool", fS[1][:, :, s_ + 1], chf[:, ORD[1][s_], 8:16], [chf], [fS[1]])

            def head_pre(b, h, H, GS):
                sc, sl = SEQS[2 * b], SEQS[2 * b + 1]
                parts = [(sc, 0, 2), (sl, 2, 16)]
                wt, fS = GS["wt"], GS["fS"]
                qT, kT, ktok, vaug = H["qT"], H["kT"], H["ktok"], H["vaug"]
                for (s, o0, n_) in parts:
                    P.dma("sp", qT[0:dk, o0 * 128:(o0 + n_) * 128], s["QK"][QOFF + h * dk:QOFF + (h + 1) * dk, :], writes=[qT])
                    P.dma("sp", kT[0:dk, o0 * 128:(o0 + n_) * 128], s["QK"][KOFF + h * dk:KOFF + (h + 1) * dk, :], writes=[kT])
                    P.dma("sp", ktok[:, o0:o0 + n_, :], s["KT"].rearrange("(n p) f -> p n f", p=128)[:, :, h * dk:(h + 1) * dk], writes=[ktok])
                    P.dma("pool", vaug[:, o0:o0 + n_, 0:128], s["TM"].rearrange("(n p) c -> p n c", p=128)[:, :, VC0 + h * 128:VC0 + (h + 1) * 128], writes=[vaug])
                for d in range(2):
                    col = d * 8 + h
                    k.tt("pool", H["vp"][d][:], vaug[:], wt[:, :, col:col + 1].to_broadcast([128, NTT, 129]), ALU.mult, [vaug, wt], [H["vp"][d]])
                for t_ in range(NTT):
                    if t_ == 9:
                        yield
                    pa = ps_at.next()
                    k.mm(pa, pa[:, 0:128], kT[0:dk, t_ * 128:(t_ + 1) * 128], qT[0:dk, t_ * 128:(t_ + 1) * 128], True, True, [kT, qT])
                    a_ = ats.next()
                    k.copy("act", a_[:], pa[:, 0:128], [pa], [a_])
                    k.tt("dve", H["atm"][0][:, t_, :], a_[:], Mfwd[:], ALU.mult, [a_, Mfwd], [H["atm"][0]])
                    k.tt("pool", H["atm"][1][:, t_, :], a_[:], Mbwd[:], ALU.mult, [a_, Mbwd], [H["atm"][1]])
                for d in range(2):
                    yield
                    vp = H["vp"][d]
                    for s0 in range(0, NS, 3):
                        ng = min(3, NS - s0)
                        pd = ps_dc.next()
                        for i_ in range(ng):
                            t_ = ORD[d][s0 + i_]
                            k.mm(pd, pd[0:dk, i_ * 129:(i_ + 1) * 129], ktok[:, t_, :], vp[:, t_, :], True, True, [ktok, vp])
                        k.evac(dCall[d][0:dk, :, s0:s0 + ng], pd[0:dk, 0:ng * 129].rearrange("p (s c) -> p c s", s=ng), [pd], [dCall[d]])
                yield
                for d in range(2):
                    k.copy("pool", Fb[d][0:dk, :, :], fS[d][0:dk, h, 0:NS].unsqueeze(1).to_broadcast([dk, 129, NS]), [fS[d]], [Fb[d]])
                    dflat = dCall[d][0:dk].rearrange("p c s -> p (c s)")
                    fflat = Fb[d][0:dk].rearrange("p c s -> p (c s)")
                    P.op("dve", lambda e, dflat=dflat, fflat=fflat: e.tensor_tensor_scan(out=dflat, data0=fflat, data1=dflat, initial=0.0, op0=ALU.mult, op1=ALU.add),
                         reads=[Fb[d], dCall[d]], writes=[dCall[d]])
                    k.tt("dve", H["cbf"][d][0:dk].rearrange("p s c -> p c s"), dCall[d][0:dk],
                         fS[d][0:dk, h, 1:NS + 1].unsqueeze(1).to_broadcast([dk, 129, NS]), ALU.mult, [dCall[d], fS[d]], [H["cbf"][d]])

            def head_out(b, h, H, GS):
                sc, sl = SEQS[2 * b], SEQS[2 * b + 1]
                parts = [(sc, 0, 2), (sl, 2, 16)]
                rowf = GS["rowf"]
                qT = H["qT"]
                for st_ in range(NTT):
                    if st_ in (3, 6, 9, 12, 15):
                        yield
                    for d in range(2):
                        t_ = ORD[d][st_]
                        col = d * 8 + h
                        vp, atm, Yd, cbf = H["vp"][d], H["atm"][d], H["Y"], H["cbf"][d]
                        first = (d == 0) == (ORD[0].index(t_) <= ORD[1].index(t_))
                        po = ps_out.next()
                        k.mm(po, po[:, 0:129], atm[:, t_, :], vp[:, t_, :], True, st_ == 0, [atm, vp])
                        if st_ > 0:
                            k.mm(po, po[:, 0:129], qT[0:dk, t_ * 128:(t_ + 1) * 128], cbf[0:dk, st_ - 1, :], False, True, [qT, cbf])
                        if is_ml:
                            res = resr.next()
                            k.act(res[:], po[:, 0:129], AF.Copy, [po, rowf], [res], scale=rowf[:, t_, col:col + 1])
                            dd = ddr.next()
                            k.act(dd[:, 0:1], res[:, 128:129], AF.Abs, [res], [dd])
                            k.ts("pool", dd[:, 0:1], dd[:, 0:1], 1.0, ALU.max, [dd], [dd])
                            P.op("dve", lambda e, dd=dd: e.reciprocal(out=dd[:, 1:2], in_=dd[:, 0:1]), reads=[dd], writes=[dd])
                            if first:
                                k.ts("dve", Yd[:, t_, :], res[:, 0:128], dd[:, 1:2], ALU.mult, [res, dd], [Yd])
                            else:
                                k.stt("dve", Yd[:, t_, :], res[:, 0:128], dd[:, 1:2], Yd[:, t_, :], ALU.mult, ALU.add, [res, dd, Yd], [Yd])
                        else:
                            if first:
                                k.act(Yd[:, t_, :], po[:, 0:128], AF.Copy, [po, rowf], [Yd], scale=rowf[:, t_, col:col + 1])
                            else:
                                k.stt("dve", Yd[:, t_, :], po[:, 0:128], rowf[:, t_, col:col + 1], Yd[:, t_, :], ALU.mult, ALU.add, [po, rowf, Yd], [Yd])
                for (s, o0, n_) in parts:
                    if s["ctx"] and last:
                        continue
                    P.dma("sp", s["Y"].rearrange("(n p) f -> p n f", p=128)[:, :, h * 128:(h + 1) * 128], H["Y"][:, o0:o0 + n_, :], reads=[H["Y"]])

            heads = [(b, h) for b in range(BPC) for h in range(8)]
            for i_h in range(len(heads) + 1):
                gens = []
                if i_h < len(heads):
                    b, h = heads[i_h]
                    if h == 0:
                        gate_prep(b, gsets[b % 2])
                    gens.append(head_pre(b, h, hdr[i_h % 2], gsets[b % 2]))
                if i_h >= 1:
                    b, h = heads[i_h - 1]
                    gens.append(head_out(b, h, hdr[(i_h - 1) % 2], gsets[b % 2]))
                while gens:
                    for g_ in list(gens):
                        try:
                            next(g_)
                        except StopIteration:
                            gens.remove(g_)
            P.barrier()
            PA.reset()

            Wout = PA.alloc("Wout", [8, D], BF16)
            P.dma("pool", Wout[:], w_out.rearrange("(kc p) n -> p kc n", p=128), writes=[Wout])
            rtr = PA.alloc("rtr", [8, NE], F32)
            P.dma("sp", rtr[:], moe_router[l].rearrange("(kc p) e -> p kc e", p=128), writes=[rtr])
            hng = load_bc("hng", hn_g)
            G2 = [load_bc(f"G2_{kd}", MODS[l, 2, kd]) for kd in range(3)]
            A2 = [load_bc(f"A2_{kd}", MODS[l, 3, kd]) for kd in range(3)]
            B2 = [load_bc(f"B2_{kd}", MODS[l, 4, kd]) for kd in range(3)]
            yr = Ring([PA.alloc(f"yt{i}", [8, 128], F32) for i in range(3)])
            orr = Ring([PA.alloc(f"ot{i}", [D], F32) for i in range(3)])
            xr = Ring([PA.alloc(f"xt{i}", [D], F32) for i in range(3)])
            sqr = Ring([PA.alloc(f"sq{i}", [8, 128], F32) for i in range(2)])
            st8 = Ring([PA.alloc(f"st8{i}", [4, 8], F32) for i in range(2)])
            ybr = Ring([PA.alloc(f"yb{i}", [D], BF16) for i in range(2)])
            ybT = Ring([PA.alloc(f"ybT{i}", [8, 128], BF16) for i in range(2)])
            xnr = Ring([PA.alloc(f"xn{i}", [D], F32) for i in range(2)])
            tmpr = Ring([PA.alloc(f"tmp{i}", [D], F32) for i in range(2)])
            h2r = Ring([PA.alloc(f"h2{i}", [D], F32) for i in range(2)])
            h2br = Ring([PA.alloc(f"h2b{i}", [D], BF16) for i in range(2)])
            h2Tr = Ring([PA.alloc(f"h2T{i}", [8, 128], F32) for i in range(2)])
            junk = PA.alloc("junk", [D], F32)
            ssr = Ring([PA.alloc(f"ss{i}", [4], F32) for i in range(3)])
            smr = Ring([PA.alloc(f"sm{i}", [4], F32) for i in range(2)])
            affr = Ring([PA.alloc(f"aff{i}", [NE], F32) for i in range(2)])
            aftr = Ring([PA.alloc(f"aft{i}", [128], F32, parts=NE) for i in range(2)])
            ps_tr = Ring(PS[0:2])
            ps_o = Ring(PS[2:6])
            ps_t32 = Ring(PS[6:8])
            for s in SEQS:
                if s["ctx"] and last:
                    continue
                T, kd, nt = s["T"], s["kind"], s["nt"]
                for ti in range(nt):
                    t0 = ti * 128
                    yt = yr.next()
                    P.dma("sp", yt[:], s["Y"][t0:t0 + 128, :].rearrange("p (h f) -> p h f", h=8), writes=[yt])
                    ot = orr.next()
                    P.dma("sp", ot[:], s["TM"][t0:t0 + 128, OC0:OC0 + 1024], writes=[ot])
                    xt = xr.next()
                    P.dma("sp", xt[:], xsrc(s)[t0:t0 + 128, :], writes=[xt])
                    sq = sqr.next()
                    s8 = st8.next()
                    if not is_ml:
                        k.P.op("dve", lambda e, s8=s8, yt=yt: e.tensor_reduce(out=s8[:, 0, :], in_=yt[:], axis=AX.X, op=ALU.add), reads=[yt], writes=[s8])
                        k.ts("dve", s8[:, 0, :], s8[:, 0, :], -1.0 / 128.0, ALU.mult, [s8], [s8])
                        k.tt("dve", yt[:], yt[:], s8[:, 0, :].unsqueeze(2).to_broadcast([128, 8, 128]), ALU.add, [yt, s8], [yt])
                    k.tt("dve", sq[:], yt[:], yt[:], ALU.mult, [yt], [sq])
                    k.P.op("dve", lambda e, s8=s8, sq=sq: e.tensor_reduce(out=s8[:, 1, :], in_=sq[:], axis=AX.X, op=ALU.add), reads=[sq], writes=[s8])
                    k.act(s8[:, 2, :], s8[:, 1, :], AF.Ln, [s8], [s8], scale=1.0 / 128.0, bias=EPS)
                    k.act(s8[:, 3, :], s8[:, 2, :], AF.Exp, [s8], [s8], scale=-0.5)
                    k.tt("dve", sq[:], yt[:], s8[:, 3, :].unsqueeze(2).to_broadcast([128, 8, 128]), ALU.mult, [yt, s8], [sq])
                    k.act(ot[:], ot[:], AF.Sigmoid if is_ml else AF.Silu, [ot], [ot])
                    k.tt("dve", sq[:].rearrange("p a b -> p (a b)"), sq[:].rearrange("p a b -> p (a b)"), hng[:], ALU.mult, [sq, hng], [sq])
                    yb = ybr.next()
                    k.tt("dve", yb[:], sq[:].rearrange("p a b -> p (a b)"), ot[:], ALU.mult, [sq, ot], [yb])
                    pt = ps_tr.next()
                    for kc in range(8):
                        k.tr(pt, ps_bf(pt)[:, kc * 128:(kc + 1) * 128], yb[:, kc * 128:(kc + 1) * 128], identbf[:], [yb, identbf])
                    yT = ybT.next()
                    k.evac(yT[:], ps_bf(pt).rearrange("p (a b) -> p a b", a=8), [pt], [yT])
                    xn = xnr.next()
                    tmp = tmpr.next()
                    for hf in range(2):
                        po = ps_o.next()
                        for kc in range(8):
                            k.mm(po, po[:], yT[:, kc, :], Wout[:, kc, hf * 512:(hf + 1) * 512], kc == 0, kc == 7, [yT, Wout])
                        k.tt("dve", tmp[:, hf * 512:(hf + 1) * 512], po[:], G2[kd][:, hf * 512:(hf + 1) * 512], ALU.mult, [po, G2[kd]], [tmp])
                    k.tt("dve", xn[:], tmp[:], xt[:], ALU.add, [tmp, xt], [xn])
                    P.dma("pool", s["XS"][t0:t0 + 128, :], xn[:], reads=[xn])
                    ss = ssr.next()
                    k.act(junk[:], xn[:], AF.Square, [xn], [junk, ss], accum=ss[:, 0:1])
                    k.act(ss[:, 1:2], ss[:, 0:1], AF.Ln, [ss], [ss], scale=1.0 / D, bias=EPS)
                    k.act(ss[:, 1:2], ss[:, 1:2], AF.Exp, [ss], [ss], scale=-0.5)
                    tmp2 = tmpr.next()
                    k.stt("dve", tmp2[:], xn[:], ss[:, 1:2], A2[kd][:], ALU.mult, ALU.mult, [xn, ss, A2[kd]], [tmp2])
                    h2 = h2r.next()
                    k.tt("dve", h2[:], tmp2[:], B2[kd][:], ALU.add, [tmp2, B2[kd]], [h2])
                    h2b = h2br.next()
                    k.copy("act", h2b[:], h2[:], [h2], [h2b])
                    P.dma("pool", s["H2"][t0:t0 + 128, :], h2b[:], reads=[h2b])
                    h2T = h2Tr.next()
                    for half in range(2):
                        p32 = ps_t32.next()
                        for kc in range(4):
                            kk = half * 4 + kc
                            k.tr(p32, p32[:, kc * 128:(kc + 1) * 128], h2[:, kk * 128:(kk + 1) * 128], ident32[:], [h2, ident32])
                        k.evac(h2T[:, half * 4:(half + 1) * 4, :], p32[:].rearrange("p (a b) -> p a b", a=4), [p32], [h2T])
                    pl = ps_o.next()
                    for kc in range(8):
                        k.mm(pl, pl[:, 0:NE], h2T[:, kc, :], rtr[:, kc, :], kc == 0, kc == 7, [h2T, rtr])
                    sm = smr.next()
                    k.P.op("dve", lambda e, sm=sm, pl=pl: e.tensor_reduce(out=sm[:, 0:1], in_=pl[:, 0:NE], axis=AX.X, op=ALU.max), reads=[pl], writes=[sm])
                    k.ts("dve", sm[:, 1:2], sm[:, 0:1], -1.0, ALU.mult, [sm], [sm])
                    aff = affr.next()
                    k.act(aff[:], pl[:, 0:NE], AF.Exp, [pl, sm], [aff, sm], bias=sm[:, 1:2], accum=sm[:, 2:3])
                    P.op("dve", lambda e, sm=sm: e.reciprocal(out=sm[:, 3:4], in_=sm[:, 2:3]), reads=[sm], writes=[sm])
                    k.ts("dve", aff[:], aff[:], sm[:, 3:4], ALU.mult, [aff, sm], [aff])
                    pa_ = ps_t32.next()
                    k.tr(pa_, pa_[0:NE, 0:128], aff[:], ident32[:], [aff, ident32])
                    aft = aftr.next()
                    k.evac(aft[:], pa_[0:NE, 0:128], [pa_], [aft])
                    P.dma("pool", s["AFFT"][:, t0:t0 + 128], aft[:], reads=[aft])
            P.barrier()
            PA.reset()
            if dbg and l == 0:
                for s in SEQS:
                    tp = dbg_tap(f"dbg_xmix_{s['name']}", [s["T"], D])
                    P.dma("sp", tp, s["XS"], )
                    tp = dbg_tap(f"dbg_afft_{s['name']}", [NE, s["T"]])
                    P.dma("sp", tp, s["AFFT"])
                    tp = dbg_tap(f"dbg_y_{s['name']}", [s["T"], D])
                    P.dma("sp", tp, s["Y"])
                    tp = dbg_tap(f"dbg_tm_{s['name']}", [s["T"], 2080])
                    P.dma("sp", tp, s["TM"])
                    tp = dbg_tap(f"dbg_fm_{s['name']}", [2048, s["T"]])
                    P.dma("sp", tp, s["FM"])
                P.barrier()

            act_seqs = [s for s in SEQS if not (s["ctx"] and last)]
            groups = [[s for s in act_seqs if not s["ctx"]]]
            if not last:
                groups.append([s for s in act_seqs if s["ctx"]])
            for grp in groups:
                T, nt = grp[0]["T"], grp[0]["nt"]
                PMtokG = PA.alloc(f"PMtokG{T}", [nt, 32], F32)
                PGg = PA.alloc(f"PGg{T}", [2, T], BF16, parts=32)
                for gi, s in enumerate(grp):
                    s["gi"], s["PMtokG"], s["PGg"] = gi, PMtokG, PGg
            mark_E = PA.off
            aff_F = PA.alloc("aff_", [SEQ], F32, parts=32)
            work_F = PA.alloc("work", [SEQ], F32, parts=32)
            onesr_F = PA.alloc("onesr", [SEQ], F32, parts=32)
            cum_F = PA.alloc("cum_", [SEQ], F32, parts=32)
            m8 = PA.alloc("m8", [8], F32, parts=32)
            k.memset("pool", onesr_F[:], 1.0, [onesr_F])
            for grp in groups:
                T, nt, cap = grp[0]["T"], grp[0]["nt"], grp[0]["cap"]
                aff_ = Tile(aff_F[:, 0:T], "aff_v")
                work = Tile(work_F[:, 0:T], "work_v")
                onesr = Tile(onesr_F[:, 0:T], "ones_v")
                cum_ = Tile(cum_F[:, 0:T], "cum_v")
                P.barrier()
                for gi, s in enumerate(grp):
                    P.dma("sp", aff_[gi * NE:(gi + 1) * NE, :], s["AFFT"], writes=[aff_])
                k.copy("dve", work[:], aff_[:], [aff_], [work])
                for it in range(cap // 8):
                    P.op("dve", lambda e, m8=m8, work=work: e.max(out=m8[:], in_=work[:]), reads=[work], writes=[m8])
                    P.op("dve", lambda e, m8=m8, work=work: e.match_replace(out=work[:], in_to_replace=m8[:], in_values=work[:], imm_value=0.0), reads=[m8, work], writes=[work])
                k.tt("dve", work[:], aff_[:], work[:], ALU.subtract, [aff_, work], [work])
                k.ts("dve", aff_[:], work[:], 0.0, ALU.is_gt, [work], [aff_])
                P.op("dve", lambda e, cum_=cum_, onesr=onesr, aff_=aff_: e.tensor_tensor_scan(out=cum_[:], data0=onesr[:], data1=aff_[:], initial=0.0, op0=ALU.mult, op1=ALU.add), reads=[onesr, aff_], writes=[cum_])
                k.tt("dve", cum_[:], cum_[:], aff_[:], ALU.mult, [cum_, aff_], [cum_])
                k.ts("dve", cum_[:], cum_[:], -1.0, ALU.add, [cum_], [cum_])
                PGg = grp[0]["PGg"]
                k.copy("dve", PGg[:, 0, :], cum_[:], [cum_], [PGg])
                k.copy("dve", PGg[:, 1, :], work[:], [work], [PGg])
                pt = PS[0]
                for ti in range(nt):
                    k.tr(pt, pt[:, ti * 32:(ti + 1) * 32], cum_[:, ti * 128:(ti + 1) * 128], ident32[0:32, 0:32], [cum_, ident32])
                k.copy("dve", grp[0]["PMtokG"][:].rearrange("p a b -> p (a b)"), pt[:, 0:nt * 32], [pt], [grp[0]["PMtokG"]])
            P.barrier()
            PA.reset(mark_E)

            NRING = 16
            wring = Ring([PA.alloc(f"wr{i}", [8, 256], BF16) for i in range(NRING)])
            h2ring = Ring([PA.alloc(f"h2t{i}", [D], BF16) for i in range(6)])
            selr = Ring([PA.alloc(f"sel{i}", [256], BF16) for i in range(6)])
            for grp in groups:
                ncap = len(grp) * grp[0]["cap"]
                xs_g = PA.alloc(f"xsT_g{ncap}", [8, ncap], BF16)
                hid_g = PA.alloc(f"hidT_g{ncap}", [8, ncap], BF16)
                for s in grp:
                    s["xsT"], s["hidT"], s["ncap"] = xs_g, hid_g, ncap
            sgr = Ring([PA.alloc(f"sg{i}", [512], F32) for i in range(2)])
            ysr = Ring([PA.alloc(f"ys{i}", [2, D], BF16) for i in range(2)])
            pieces = []
            for e_ in range(NE):
                for fb in range(4):
                    pieces.append((e_, "g", fb))
                    pieces.append((e_, "u", fb))
                for i_ in range(4):
                    pieces.append((e_, "d", i_))
            loaded = {}
            nload = [0]

            def ensure(n_upto):
                while nload[0] < min(n_upto, len(pieces)):
                    e_, kind_, i_ = pieces[nload[0]]
                    wt_ = wring.next()
                    if kind_ == "d":
                        src = moe_wd[l, e_].rearrange("(fc p) d -> p fc d", p=128)[:, 2 * i_:2 * i_ + 2, :]
                        dst = wt_[:].rearrange("p a b -> p (a b)").rearrange("p (f d) -> p f d", f=2)
                    else:
                        wsrc = moe_wg if kind_ == "g" else moe_wu
                        src = wsrc[l, e_].rearrange("(kc p) n -> p kc n", p=128)[:, :, i_ * 256:(i_ + 1) * 256]
                        dst = wt_[:]
                    P.dma("pool", dst, src, writes=[wt_])
                    loaded[pieces[nload[0]]] = wt_
                    nload[0] += 1

            ps_ga = PS[0:4]
            ps_up = Ring(PS[4:8])
            ps_dn = Ring(PS[0:4])
            pidx = 0
            for e_ in range(NE):
                for s in act_seqs:
                    nt, cap = s["nt"], s["cap"]
                    for ti in range(nt):
                        h2t = h2ring.next()
                        P.dma("sp", h2t[:], s["H2"][ti * 128:(ti + 1) * 128, :], writes=[h2t])
                        sel = selr.next()
                        k.ts("dve", sel[:, 0:cap], iota_f[:, 0:cap], s["PMtokG"][:, ti, s["gi"] * NE + e_:s["gi"] * NE + e_ + 1], ALU.is_equal, [iota_f, s["PMtokG"]], [sel])
                        for dc in range(8):
                            pg = ps_ga[dc // 2]
                            k.mm(pg, pg[:, (dc % 2) * 256:(dc % 2) * 256 + cap], h2t[:, dc * 128:(dc + 1) * 128], sel[:, 0:cap], ti == 0 and dc % 2 == 0, ti == nt - 1 and dc % 2 == 1, [h2t, sel])
                    for dc in range(8):
                        pg = ps_ga[dc // 2]
                        k.evac(s["xsT"][:, dc, s["gi"] * cap:(s["gi"] + 1) * cap], pg[:, (dc % 2) * 256:(dc % 2) * 256 + cap], [pg], [s["xsT"]])
                for fb in range(4):
                    ensure(pidx + 2 + 12)
                    wg_, wu_ = loaded[(e_, "g", fb)], loaded[(e_, "u", fb)]
                    pidx += 2
                    for grp in groups:
                        s = grp[0]
                        ncap = s["ncap"]
                        for f2 in range(2):
                            pg_ = ps_up.next()
                            for kc in range(8):
                                k.mm(pg_, pg_[:, 0:ncap], wg_[:, kc, f2 * 128:(f2 + 1) * 128], s["xsT"][:, kc, :], kc == 0, kc == 7, [wg_, s["xsT"]])
                            pu = ps_up.next()
                            for kc in range(8):
                                k.mm(pu, pu[:, 0:ncap], wu_[:, kc, f2 * 128:(f2 + 1) * 128], s["xsT"][:, kc, :], kc == 0, kc == 7, [wu_, s["xsT"]])
                            sg = sgr.next()
                            k.act(sg[:, 0:ncap], pg_[:, 0:ncap], AF.Silu, [pg_], [sg])
                            k.tt("dve", s["hidT"][:, fb * 2 + f2, :], sg[:, 0:ncap], pu[:, 0:ncap], ALU.mult, [sg, pu], [s["hidT"]])
                ensure(pidx + 4 + 12)
                wd_ = [loaded[(e_, "d", i_)] for i_ in range(4)]
                pidx += 4
                for s in act_seqs:
                    cap = s["cap"]
                    M = min(128, cap)
                    ncc = cap // M
                    ys = ysr.next()
                    for cc in range(ncc):
                        for hf in range(2):
                            pd = ps_dn.next()
                            for fc in range(8):
                                wv = wd_[fc // 2][:].rearrange("p a b -> p (a b)").rearrange("p (f d) -> p f d", f=2)
                                k.mm(pd, pd[0:M, :], s["hidT"][:, fc, s["gi"] * cap + cc * M:s["gi"] * cap + (cc + 1) * M], wv[:, fc % 2, hf * 512:(hf + 1) * 512], fc == 0, fc == 7, [s["hidT"], wd_[fc // 2]])
                            k.evac(ys[0:M, cc, hf * 512:(hf + 1) * 512], pd[0:M, :], [pd], [ys])
                    P.dma("sp", s["YE"][e_].rearrange("(cc p) d -> p cc d", p=M), ys[0:M, 0:ncc, :], reads=[ys])
            P.barrier()
            PA.reset(mark_E)

            G5 = [load_bc(f"G5_{kd}", MODS[l, 5, kd]) for kd in range(3)]
            if last:
                fng = load_bc("fng", final_norm_g)
            yer = [PA.alloc(f"yer{e_}", [2, D], BF16) for e_ in range(NE)]
            xr = Ring([PA.alloc(f"xt{i}", [D], F32) for i in range(2)])
            gmr = Ring([PA.alloc(f"gm{i}", [512], BF16) for i in range(3)])
            selT = [[PA.alloc(f"selT{e_}_{cc}", [512], BF16) for cc in range(2)] for e_ in range(NE)]
            tmpr = Ring([PA.alloc(f"tmp{i}", [D], F32) for i in range(2)])
            xnr = Ring([PA.alloc(f"xn{i}", [D], F32) for i in range(2)])
            junk = PA.alloc("junk", [D], F32)
            ssr = Ring([PA.alloc(f"ss{i}", [2], F32) for i in range(3)])
            ps_bc = Ring(PS[0:4])
            ps_oo = Ring(PS[4:8])
            for s in act_seqs:
                T, nt, cap, kd = s["T"], s["nt"], s["cap"], s["kind"]
                M = min(128, cap)
                ncc = cap // M
                NBt = min(512, T)
                for e_ in range(NE):
                    P.dma("sp", yer[e_][0:M, 0:ncc, :], s["YE"][e_].rearrange("(cc p) d -> p cc d", p=M), writes=[yer[e_]])
                for blk in range(T // NBt):
                    c0 = blk * NBt
                    for e_ in range(NE):
                        pbm = ps_bc.next()
                        k.mm(pbm, pbm[:, 0:NBt], oh32[:, s["gi"] * NE + e_, :], s["PGg"][:, 0, c0:c0 + NBt], True, True, [oh32, s["PGg"]])
                        pbg = ps_bc.next()
                        k.mm(pbg, pbg[:, 0:NBt], oh32[:, s["gi"] * NE + e_, :], s["PGg"][:, 1, c0:c0 + NBt], True, True, [oh32, s["PGg"]])
                        gm = gmr.next()
                        k.copy("act", gm[:, 0:NBt], pbg[:, 0:NBt], [pbg], [gm])
                        for cc in range(ncc):
                            st_ = selT[e_][cc]
                            k.stt("dve", st_[0:M, 0:NBt], pbm[0:M, 0:NBt], (iota_p if cc == 0 else iota_p128)[0:M, 0:1], gm[0:M, 0:NBt], ALU.is_equal, ALU.mult, [pbm, gm, iota_p, iota_p128], [st_])
                    for ti in range(NBt // 128):
                        t0 = c0 + ti * 128
                        xt = xr.next()
                        P.dma("sp", xt[:], s["XS"][t0:t0 + 128, :], writes=[xt])
                        po = [ps_oo.next(), ps_oo.next()]
                        for hf in range(2):
                            for e_ in range(NE):
                                for cc in range(ncc):
                                    k.mm(po[hf], po[hf][:], selT[e_][cc][0:M, ti * 128:(ti + 1) * 128], yer[e_][0:M, cc, hf * 512:(hf + 1) * 512], e_ == 0 and cc == 0, e_ == NE - 1 and cc == ncc - 1, [selT[e_][cc], yer[e_]])
                        tmp = tmpr.next()
                        for hf in range(2):
                            k.tt("dve", tmp[:, hf * 512:(hf + 1) * 512], po[hf][:], G5[kd][:, hf * 512:(hf + 1) * 512], ALU.mult, [po[hf], G5[kd]], [tmp])
                        xn = xnr.next()
                        k.tt("dve", xn[:], tmp[:], xt[:], ALU.add, [tmp, xt], [xn])
                        if not last:
                            P.dma("pool", s["XS"][t0:t0 + 128, :], xn[:], reads=[xn])
                        else:
                            ss = ssr.next()
                            k.act(junk[:], xn[:], AF.Square, [xn], [junk, ss], accum=ss[:, 0:1])
                            k.act(ss[:, 1:2], ss[:, 0:1], AF.Ln, [ss], [ss], scale=1.0 / D, bias=EPS)
                            k.act(ss[:, 1:2], ss[:, 1:2], AF.Exp, [ss], [ss], scale=-0.5)
                            tmp2 = tmpr.next()
                            k.stt("dve", tmp2[:], xn[:], ss[:, 1:2], fng[:], ALU.mult, ALU.mult, [xn, ss, fng], [tmp2])
                            P.dma("pool", out_ap[s["b"], t0:t0 + 128, :], tmp2[:], reads=[tmp2])
            P.barrier()
            PA.reset()
            if dbg:
                for s in SEQS:
                    if s["ctx"] and last:
                        continue
                    tp = dbg_tap(f"dbg_x{l}_{s['name']}", [s["T"], D])
                    P.dma("sp", tp, s["XS"])
                P.barrier()
            if dbg and dbg == "l0":
                break
        P.emit()
    return nc, list(dbg_outs.keys()), len(P.ops)


_CACHE = {}


def kernel(**inputs):
    if "nc" not in _CACHE:
        _CACHE["nc"] = build(False)
    nc, _, nops = _CACHE["nc"]
    f32 = lambda a: np.ascontiguousarray(np.asarray(a, dtype=np.float32))
    shared = {}
    for name in ("c_ctx", "ada_w", "ada_b", "norm_mix_g", "norm_ffn_g", "final_norm_g", "mlstm_w_in", "mlstm_conv_w",
                 "mlstm_conv_b", "mlstm_head_norm_g", "mlstm_w_out", "ret_w_in", "ret_group_norm_g", "ret_w_out",
                 "moe_router", "moe_w_gate", "moe_w_up", "moe_w_down"):
        shared[name] = f32(inputs[name])
    for name in ("mlstm_igate_b", "mlstm_fgate_b", "ret_decay_logit"):
        shared[name] = f32(inputs[name]).reshape(2, 16)
    x = f32(inputs["x"])
    c = f32(inputs["c"])
    ctx = f32(inputs["ctx"])
    in_maps = []
    for i in range(NCORES):
        m = dict(shared)
        m["x"] = x[i * BPC:(i + 1) * BPC]
        m["c"] = c[i * BPC:(i + 1) * BPC]
        m["ctx"] = ctx[i * BPC:(i + 1) * BPC]
        in_maps.append(m)
    res = run_bass_kernel_spmd(nc, in_maps, core_ids=list(range(NCORES)))
    out = np.concatenate([np.asarray(r["out"], dtype=np.float32) for r in res.results], axis=0)
    return out
```
